# Optimizing a Trainium2 kernel written in Bass

```python
import jax, jax.numpy as jnp
from jax import lax
import numpy as np

D_MODEL = 1024
BATCH = 8
SEQ = 4096
DEPTH = 1

HEAD_DIM = 64
ROPE_THETA = 10000.0
BLOCK = 128
RMS_EPS = 1e-6
DIL_GROUPS = ((128, 1), (512, 4), (2048, 16))
N_DIL_GROUPS = len(DIL_GROUPS)
A_HEADS = D_MODEL // (2 * HEAD_DIM)
A_GROUP_WIDTH = A_HEADS * HEAD_DIM
A_QKV_WIDTH = 3 * N_DIL_GROUPS * A_GROUP_WIDTH
B_Q_HEADS = D_MODEL // HEAD_DIM
B_KV_HEADS = max(1, B_Q_HEADS // 8)
B_GROUP = B_Q_HEADS // B_KV_HEADS
B_WINDOW = 128
B_Q_WIDTH = B_Q_HEADS * HEAD_DIM
B_KV_WIDTH = B_KV_HEADS * HEAD_DIM
B_QKV_WIDTH = B_Q_WIDTH + 2 * B_KV_WIDTH
N_BRANCHES = 2
GATE_WIDTH = N_BRANCHES * D_MODEL
IN_WIDTH = A_QKV_WIDTH + B_QKV_WIDTH + GATE_WIDTH
N_EXPERT_GROUPS = 4
EXPERTS_PER_GROUP = 8
N_EXPERTS = N_EXPERT_GROUPS * EXPERTS_PER_GROUP
TOP_K_IN_GROUP = 2
D_EXPERT = D_MODEL // 4

kernel_name = "hybrid_dilated_swa_sink_hmoe"


def rmsnorm(x, g):
    xf = x.astype(jnp.float32)
    y = xf * lax.rsqrt(jnp.mean(xf * xf, axis=-1, keepdims=True) + RMS_EPS)
    return (y * g.astype(jnp.float32)).astype(x.dtype)


def rope(t, cos, sin):
    half = t.shape[-1] // 2
    tf = t.astype(jnp.float32)
    t1, t2 = tf[..., :half], tf[..., half:]
    c, s = cos[:, None, :], sin[:, None, :]
    return jnp.concatenate([t1 * c - t2 * s, t2 * c + t1 * s], axis=-1).astype(t.dtype)


def banded_attention(q, k, v, max_dist, sink=None):
    n, hk, g, L, hd = q.shape
    nb = -(-L // BLOCK)
    lp = nb * BLOCK
    qb = jnp.pad(q, ((0, 0), (0, 0), (0, 0), (0, lp - L), (0, 0))).reshape(n, hk, g, nb, BLOCK, hd)
    kv_pad = ((0, 0), (0, 0), (BLOCK, lp - L), (0, 0))
    kp = jnp.pad(k, kv_pad).reshape(n, hk, nb + 1, BLOCK, hd)
    vp = jnp.pad(v, kv_pad).reshape(n, hk, nb + 1, BLOCK, hd)
    kb = jnp.concatenate([kp[:, :, :-1], kp[:, :, 1:]], axis=3)
    vb = jnp.concatenate([vp[:, :, :-1], vp[:, :, 1:]], axis=3)
    s = jnp.einsum('nhgiqd,nhikd->nhgiqk', qb, kb).astype(jnp.float32) * (hd ** -0.5)
    r = jnp.arange(BLOCK)[:, None]
    c = jnp.arange(2 * BLOCK)[None, :]
    dist = r - c + BLOCK
    kpos = jnp.arange(nb)[:, None, None] * BLOCK - BLOCK + c[None]
    mask = (dist >= 0)[None] & (dist <= max_dist)[None] & (kpos >= 0)
    s = jnp.where(mask, s, -jnp.inf)
    m = jnp.max(s, axis=-1)
    if sink is not None:
        sink_b = sink.astype(jnp.float32)[None, :, :, None, None]
        m = jnp.maximum(m, sink_b)
    p = jnp.exp(s - m[..., None])
    denom = jnp.sum(p, axis=-1)
    if sink is not None:
        denom = denom + jnp.exp(sink_b - m)
    o = jnp.einsum('nhgiqk,nhikd->nhgiqd', p.astype(v.dtype), vb).astype(jnp.float32) / denom[..., None]
    lse = m + jnp.log(denom)
    o = o.astype(q.dtype).reshape(n, hk, g, lp, hd)[:, :, :, :L]
    lse = lse.reshape(n, hk, g, lp)[:, :, :, :L]
    return o, lse


def dilated_group(q, k, v, window, dilation):
    b, s, h, hd = q.shape
    ls = s // dilation

    def to_sub(t):
        return t.reshape(b, ls, dilation, h, hd).transpose(0, 2, 3, 1, 4).reshape(b * dilation, h, ls, hd)

    o, lse = banded_attention(to_sub(q)[:, :, None], to_sub(k), to_sub(v), window // dilation)
    o = o[:, :, 0].reshape(b, dilation, h, ls, hd).transpose(0, 3, 1, 2, 4).reshape(b, s, h, hd)
    lse = lse[:, :, 0].reshape(b, dilation, h, ls).transpose(0, 3, 1, 2).reshape(b, s, h)
    return o, lse


def hier_moe(t, w_rg, b_rg, w_re, b_re, w_eg, w_eu, w_ed):
    n_tok = t.shape[0]
    lg = (t @ w_rg).astype(jnp.float32) + b_rg.astype(jnp.float32)
    pg = jax.nn.softmax(lg, axis=-1)
    gsel = jnp.argmax(lg, axis=-1)
    pg_sel = jnp.take_along_axis(pg, gsel[:, None], axis=-1)[:, 0]
    le = ((t @ w_re).astype(jnp.float32) + b_re.astype(jnp.float32)).reshape(n_tok, N_EXPERT_GROUPS, EXPERTS_PER_GROUP)
    le_sel = jnp.take_along_axis(le, gsel[:, None, None], axis=1)[:, 0]
    pe = jax.nn.softmax(le_sel, axis=-1)
    top_p, top_i = lax.top_k(pe, TOP_K_IN_GROUP)
    wts = pg_sel[:, None] * top_p / jnp.sum(top_p, axis=-1, keepdims=True)
    eidx = gsel[:, None] * EXPERTS_PER_GROUP + top_i
    combine = jnp.sum(jax.nn.one_hot(eidx, N_EXPERTS, dtype=jnp.float32) * wts[..., None], axis=1)
    y = jnp.zeros((n_tok, t.shape[1]), jnp.float32)
    for gi in range(N_EXPERT_GROUPS):
        sl = slice(gi * EXPERTS_PER_GROUP, (gi + 1) * EXPERTS_PER_GROUP)
        hg = jnp.einsum('td,edf->etf', t, w_eg[sl])
        hu = jnp.einsum('td,edf->etf', t, w_eu[sl])
        a = jax.nn.silu(hg) * hu * combine[:, sl].T[:, :, None].astype(t.dtype)
        y = y + jnp.einsum('etf,efd->td', a, w_ed[sl]).astype(jnp.float32)
    return y.astype(t.dtype)


def hybrid_layer(x, cos, sin, w_in, b_in, sinks, w_proj_a, w_proj_b, w_out, g_mix, g_ffn,
                 w_rg, b_rg, w_re, b_re, w_eg, w_eu, w_ed):
    b, s, d = x.shape
    h = rmsnorm(x, g_mix)
    z = h @ w_in + b_in
    za = z[..., :A_QKV_WIDTH]
    zb = z[..., A_QKV_WIDTH:A_QKV_WIDTH + B_QKV_WIDTH]
    zg = z[..., A_QKV_WIDTH + B_QKV_WIDTH:]

    za = za.reshape(b, s, 3, N_DIL_GROUPS, A_HEADS, HEAD_DIM)
    outs, lses = [], []
    for gi, (window, dil) in enumerate(DIL_GROUPS):
        qa = rope(za[:, :, 0, gi], cos, sin)
        ka = rope(za[:, :, 1, gi], cos, sin)
        o, l = dilated_group(qa, ka, za[:, :, 2, gi], window, dil)
        outs.append(o)
        lses.append(l)
    wgt = jax.nn.softmax(jnp.stack(lses), axis=0)
    ya = jnp.einsum('gbsh,gbshd->bshd', wgt.astype(x.dtype), jnp.stack(outs)).reshape(b, s, A_GROUP_WIDTH)

    qb = rope(zb[..., :B_Q_WIDTH].reshape(b, s, B_Q_HEADS, HEAD_DIM), cos, sin)
    kb = rope(zb[..., B_Q_WIDTH:B_Q_WIDTH + B_KV_WIDTH].reshape(b, s, B_KV_HEADS, HEAD_DIM), cos, sin)
    vb = zb[..., B_Q_WIDTH + B_KV_WIDTH:].reshape(b, s, B_KV_HEADS, HEAD_DIM)
    qb = qb.reshape(b, s, B_KV_HEADS, B_GROUP, HEAD_DIM).transpose(0, 2, 3, 1, 4)
    ob, _ = banded_attention(qb, kb.transpose(0, 2, 1, 3), vb.transpose(0, 2, 1, 3),
                             B_WINDOW - 1, sinks.reshape(B_KV_HEADS, B_GROUP))
    yb = ob.transpose(0, 3, 1, 2, 4).reshape(b, s, B_Q_WIDTH)

    gates = jax.nn.sigmoid(zg.reshape(b, s, N_BRANCHES, d))
    merged = gates[:, :, 0] * (ya @ w_proj_a) + gates[:, :, 1] * (yb @ w_proj_b)
    x = x + merged @ w_out

    h2 = rmsnorm(x, g_ffn)
    x = x + hier_moe(h2.reshape(b * s, d), w_rg, b_rg, w_re, b_re, w_eg, w_eu, w_ed).reshape(b, s, d)
    return x


def setup_inputs(seed: int = 0) -> dict:
    key = jax.random.key(seed)
    ks = jax.random.split(key, 18)
    f32 = jnp.float32
    nrm = lambda k, shape, scale: jax.random.normal(k, shape, f32) * scale
    return {
        "x": nrm(ks[0], (BATCH, SEQ, D_MODEL), 1.0),
        "w_in": nrm(ks[1], (DEPTH, D_MODEL, IN_WIDTH), D_MODEL ** -0.5),
        "b_in": nrm(ks[2], (DEPTH, IN_WIDTH), 0.02),
        "sinks": nrm(ks[3], (DEPTH, B_Q_HEADS), 0.5),
        "w_proj_a": nrm(ks[4], (DEPTH, A_GROUP_WIDTH, D_MODEL), A_GROUP_WIDTH ** -0.5),
        "w_proj_b": nrm(ks[5], (DEPTH, B_Q_WIDTH, D_MODEL), B_Q_WIDTH ** -0.5),
        "w_out": nrm(ks[6], (DEPTH, D_MODEL, D_MODEL), D_MODEL ** -0.5),
        "g_mix": 1.0 + nrm(ks[7], (DEPTH, D_MODEL), 0.05),
        "g_ffn": 1.0 + nrm(ks[8], (DEPTH, D_MODEL), 0.05),
        "w_router_group": nrm(ks[9], (DEPTH, D_MODEL, N_EXPERT_GROUPS), D_MODEL ** -0.5),
        "b_router_group": nrm(ks[10], (DEPTH, N_EXPERT_GROUPS), 0.01),
        "w_router_expert": nrm(ks[11], (DEPTH, D_MODEL, N_EXPERTS), D_MODEL ** -0.5),
        "b_router_expert": nrm(ks[12], (DEPTH, N_EXPERTS), 0.01),
        "w_exp_gate": nrm(ks[13], (DEPTH, N_EXPERTS, D_MODEL, D_EXPERT), D_MODEL ** -0.5),
        "w_exp_up": nrm(ks[14], (DEPTH, N_EXPERTS, D_MODEL, D_EXPERT), D_MODEL ** -0.5),
        "w_exp_down": nrm(ks[15], (DEPTH, N_EXPERTS, D_EXPERT, D_MODEL), D_EXPERT ** -0.5),
        "g_final": 1.0 + nrm(ks[16], (D_MODEL,), 0.05),
    }


def reference(x, w_in, b_in, sinks, w_proj_a, w_proj_b, w_out, g_mix, g_ffn,
              w_router_group, b_router_group, w_router_expert, b_router_expert,
              w_exp_gate, w_exp_up, w_exp_down, g_final):
    s = x.shape[1]
    pos = jnp.arange(s, dtype=jnp.float32)
    inv_freq = ROPE_THETA ** (-jnp.arange(0, HEAD_DIM, 2, dtype=jnp.float32) / HEAD_DIM)
    ang = pos[:, None] * inv_freq[None, :]
    cos, sin = jnp.cos(ang), jnp.sin(ang)
    for layer in range(DEPTH):
        x = hybrid_layer(x, cos, sin, w_in[layer], b_in[layer], sinks[layer], w_proj_a[layer],
                         w_proj_b[layer], w_out[layer], g_mix[layer], g_ffn[layer],
                         w_router_group[layer], b_router_group[layer], w_router_expert[layer],
                         b_router_expert[layer], w_exp_gate[layer], w_exp_up[layer], w_exp_down[layer])
    return rmsnorm(x, g_final)
```

```python
import os
from contextlib import ExitStack
import numpy as np
import ml_dtypes
import concourse.bass as bass
import concourse.mybir as mybir
from concourse.bass_utils import run_bass_kernel_spmd
from concourse.alu_op_type import AluOpType as ALU

F32 = mybir.dt.float32
BF16 = mybir.dt.bfloat16
AF = mybir.ActivationFunctionType
AX = mybir.AxisListType
U32 = mybir.dt.uint32
I32 = mybir.dt.int32

D = 1024
T = 4096
NCH = 8
W = 512
NW = T // W
HD = 64
EPS = 1e-6
DIL = (1, 4, 16)
NEXP = 32
DEXP = 256
A_QKV = 4608
B_QKV = 1280
N_DMA_SEMS = 48
SAME_ENGINE_SYNC = True


class _Rec:
    def __init__(self):
        self.call = None

    def __getattr__(self, name):
        def f(*args, **kwargs):
            assert self.call is None
            self.call = (name, args, kwargs)
            return self
        return f


class Prog:
    def __init__(self, nc):
        self.nc = nc
        self.ops = []

    def add(self, eng, fn, r=(), w=(), dma=False, raw=False):
        if raw:
            fn2 = fn
        else:
            rec = _Rec()
            fn(rec)
            name, args, kwargs = rec.call
            fn2 = lambda e: getattr(e, name)(*args, **kwargs)
        self.ops.append(dict(eng=eng, fn=fn2, r=tuple(r), w=tuple(w), dma=dma))
        return len(self.ops) - 1

    def barrier(self, tag):
        engs = ['pe', 'act', 'dve', 'pool', 'sp']
        allres = set()
        for o in self.ops:
            allres.update(o['r'])
            allres.update(o['w'])
        allres = tuple(allres)
        for e in engs:
            self.add(e, lambda eng: eng.nop(), r=allres, w=[('bar', tag, e)])
        for e in engs:
            self.add(e, lambda eng: eng.nop(), r=[('bar', tag, f) for f in engs], w=allres)

    def emit(self, stack):
        nc = self.nc
        ops = self.ops
        engs = ['pe', 'act', 'dve', 'pool', 'sp']
        last_w = {}
        readers = {}
        deps = []
        for i, op in enumerate(ops):
            d = set()
            for r in op['r']:
                if r in last_w:
                    d.add(last_w[r])
            for w_ in op['w']:
                if w_ in last_w:
                    d.add(last_w[w_])
                d.update(readers.get(w_, ()))
            d.discard(i)
            for r in op['r']:
                readers.setdefault(r, []).append(i)
            for w_ in op['w']:
                last_w[w_] = i
                readers[w_] = []
            deps.append(d)
        dma_sem_of = {}
        dma_val_of = {}
        sem_uses = [0] * N_DMA_SEMS
        sem_last = [None] * N_DMA_SEMS
        k = 0
        for i, op in enumerate(ops):
            if op['dma']:
                s = k % N_DMA_SEMS
                k += 1
                if sem_last[s] is not None:
                    deps[i].add(sem_last[s])
                sem_uses[s] += 1
                dma_sem_of[i] = s
                dma_val_of[i] = 16 * sem_uses[s]
                sem_last[s] = i
        red = []
        for i, op in enumerate(ops):
            best = {}
            dmas = []
            for j in deps[i]:
                oj = ops[j]
                if oj['dma']:
                    dmas.append(j)
                    continue
                if oj['eng'] == op['eng'] and not op['dma']:
                    if oj['eng'] in ('pe', 'sp') or not SAME_ENGINE_SYNC:
                        continue
                if oj['eng'] not in best or j > best[oj['eng']]:
                    best[oj['eng']] = j
            red.append((sorted(best.values()), sorted(dmas)))
        need_inc = [False] * len(ops)
        for i in range(len(ops)):
            for j in red[i][0]:
                need_inc[j] = True
        cnt = {e: 0 for e in engs}
        val_of = {}
        for i, op in enumerate(ops):
            if need_inc[i]:
                cnt[op['eng']] += 1
                val_of[i] = cnt[op['eng']]
        esem = {e: stack.enter_context(nc.semaphore('s_' + e)) for e in engs}
        dsem = [stack.enter_context(nc.semaphore('d%d' % s)) for s in range(N_DMA_SEMS)]
        waited = set()
        for i in range(len(ops)):
            for j in deps[i]:
                waited.add(j)
        tail = [i for i, op in enumerate(ops) if op['dma'] and i not in waited]
        per_eng = {e: [i for i, op in enumerate(ops) if op['eng'] == e] for e in engs}

        def body(ename, eng):
            seen = {}
            for i in per_eng[ename]:
                op = ops[i]
                need = {}
                for j in red[i][0]:
                    key = ('e', ops[j]['eng'])
                    need[key] = max(need.get(key, 0), val_of[j])
                for j in red[i][1]:
                    key = ('d', dma_sem_of[j])
                    need[key] = max(need.get(key, 0), dma_val_of[j])
                for key, val in need.items():
                    if seen.get(key, 0) >= val:
                        continue
                    seen[key] = val
                    sem = esem[key[1]] if key[0] == 'e' else dsem[key[1]]
                    eng.wait_ge(sem, val)
                inst = op['fn'](eng)
                if op['dma']:
                    inst.then_inc(dsem[dma_sem_of[i]], 16)
                elif need_inc[i]:
                    inst.then_inc(esem[ename], 1)
            if ename == 'sp':
                for j in tail:
                    eng.wait_ge(dsem[dma_sem_of[j]], dma_val_of[j])

        block = stack.enter_context(nc.Block())

        @block.tensor
        def _(e):
            body('pe', e)

        @block.scalar
        def _(e):
            body('act', e)

        @block.vector
        def _(e):
            body('dve', e)

        @block.gpsimd
        def _(e):
            body('pool', e)

        @block.sync
        def _(e):
            body('sp', e)


class Arena:
    def __init__(self, nc):
        self.nc = nc
        self.base = (nc.sbuf_base + 31) // 32 * 32
        self.top = nc.sbuf_top
        self.cur = self.base
        self.n = 0

    def alloc(self, name, shape, dt):
        esz = 2 if dt == BF16 else 4
        nbytes = int(np.prod(shape[1:])) * esz
        off = self.cur
        self.cur = (off + nbytes + 31) // 32 * 32
        assert self.cur <= self.top, ('SBUF overflow', name, self.cur, self.top)
        self.n += 1
        return self.nc.alloc_sbuf_tensor_at('%s_%d' % (name, self.n), list(shape), dt, offset=off)

    def mark(self):
        return self.cur

    def release(self, m):
        self.cur = m


def inproj_blocks():
    blocks = []
    idx = {}
    for p in range(4):
        for role, ri in (('q', 0), ('k', 1), ('v', 2)):
            for g in range(3):
                c0 = ((ri * 3 + g) * 8 + 2 * p) * 64
                idx[('A', p, role, g)] = len(blocks)
                blocks.append(np.arange(c0, c0 + 128))
    for gi in range(8):
        idx[('B', 'q', gi)] = len(blocks)
        blocks.append(np.concatenate([A_QKV + gi * 64 + np.arange(64), A_QKV + (8 + gi) * 64 + np.arange(64)]))
    idx[('B', 'k')] = len(blocks)
    blocks.append(A_QKV + 1024 + np.arange(128))
    idx[('B', 'v')] = len(blocks)
    blocks.append(A_QKV + 1024 + 128 + np.arange(128))
    for f in range(16):
        idx[('G', f)] = len(blocks)
        blocks.append(A_QKV + B_QKV + f * 128 + np.arange(128))
    return blocks, idx


BLOCKS, BIDX = inproj_blocks()
NBLK = len(BLOCKS)
VBLKS = [BIDX[('A', p, 'v', g)] for p in range(4) for g in range(3)] + [BIDX[('B', 'v')]]
VIDX = {b: i for i, b in enumerate(VBLKS)}


def perm_block_tokens(d, blk):
    L = T // d
    pos = 128 * blk
    r, j0 = pos // L, pos % L
    return j0 * d + r, d


def build(debug=None, phases='A'):
    nc = bass.Bass('TRN2', target_bir_lowering=False)
    P = Prog(nc)
    stack = ExitStack()
    ar = Arena(nc)

    def dram(name, shape, dt=F32, kind='ExternalInput'):
        return nc.dram_tensor(name, list(shape), dt, kind=kind).ap()

    xT = dram('xT', [D, T])
    gmix = dram('gmix', [128, NCH])
    win = dram('win', [NBLK, 128, NCH, 128])
    binb = dram('binb', [128, NBLK])
    bvrep = dram('bvrep', [len(VBLKS), 128, 128])
    cosd = dram('cosT', [128, T])
    sind = dram('sinT', [128, T])
    cbf = dram('cbf', [128, 256 + 512 * 6 + 128], BF16)
    wpa = dram('wpa', [8, 128, 4, 128])
    wpb = dram('wpb', [8, 128, 8, 128])
    wo = dram('wo', [8, 128, 8, 128])
    gffn = dram('gffn', [128, NCH])
    sinkrep = dram('sinkrep', [128, 16])
    wr = dram('wr', [128, NCH, 36])
    brep = dram('brep', [128, 36])
    weg = dram('weg', [NEXP, 128, NCH, DEXP])
    weu = dram('weu', [NEXP, 128, NCH, DEXP])
    wed = dram('wed', [NEXP, 128, 2, D])
    wegR = weg.rearrange('e p c f -> (e p) (c f)')
    weuR = weu.rearrange('e p c f -> (e p) (c f)')
    wedR = wed.rearrange('e p c d -> (e p) (c d)')
    identf_d = dram('identf', [128, 128])
    thr_d = dram('thr', [128, 32, 32])
    jrow_d = dram('jrow', [128, 64])
    pcol_d = dram('pcol', [128, 1])
    gfr_d = dram('gfr', [128, D])
    outd = dram('out', [T, D], kind='ExternalOutput')
    wgb = dram('wgb', [NEXP * 128, NCH * DEXP], BF16, kind='Internal')
    wub = dram('wub', [NEXP * 128, NCH * DEXP], BF16, kind='Internal')
    wdb = dram('wdb', [NEXP * 128, 2 * D], BF16, kind='Internal')
    B0 = BIDX[('B', 'q', 0)]
    NB3 = NBLK - B0
    winb = dram('winb', [NB3 * 128, NCH * 128], BF16, kind='Internal')
    wpab = dram('wpab', [8 * 128, 4 * 128], BF16, kind='Internal')
    wpbb = dram('wpbb', [8 * 128, NCH * 128], BF16, kind='Internal')
    wob = dram('wob', [8 * 128, NCH * 128], BF16, kind='Internal')
    winR = win.rearrange('b p c n -> (b p) (c n)')
    wpaR = wpa.rearrange('f p c n -> (f p) (c n)')
    wpbR = wpb.rearrange('f p c n -> (f p) (c n)')
    woR = wo.rearrange('f p c n -> (f p) (c n)')
    casts2 = []
    for r0 in range(0, NB3 * 128, 512):
        r1 = min(r0 + 512, NB3 * 128)
        casts2.append((winR[B0 * 128 + r0:B0 * 128 + r1, :], winb[r0:r1, :]))
    for (s_, d_) in ((wpaR, wpab), (wpbR, wpbb), (woR, wob)):
        for r0 in (0, 512):
            casts2.append((s_[r0:r0 + 512, :], d_[r0:r0 + 512, :]))
    X1 = dram('X1s', [T, D], F32, kind='Internal')
    H2 = dram('H2s', [T, D], BF16, kind='Internal')
    Xs = dram('Xss', [64 * 256, D], BF16, kind='Internal')
    Ys = dram('Yss', [64 * 256, D], F32, kind='Internal')
    xTv = xT.rearrange('(c p) t -> p c t', p=128)

    ones_bf = ar.alloc('ones_bf', [128, 128], BF16)
    gmix_sb = ar.alloc('gmix_sb', [128, NCH], F32)
    bin_sb = ar.alloc('bin_sb', [128, NBLK], F32)
    cbf_sb = ar.alloc('cbf_sb', [128, 256 + 512], BF16)
    rotT = cbf_sb[:, 0:128]
    ident = cbf_sb[:, 128:256]
    maskB = cbf_sb[:, 256:768]
    gffn_sb = ar.alloc('gffn_sb', [128, NCH], F32)
    esink = ar.alloc('esink', [128, 16], F32)
    selb = ar.alloc('selb', [128, 32, 32], BF16)
    Ff = ar.alloc('Ff', [128, 32, 32], BF16)
    w0 = ar.alloc('w0', [128, 32], F32)
    w1 = ar.alloc('w1', [128, 32], F32)
    identf = ar.alloc('identf', [128, 128], F32)
    m_big = ar.mark()
    hT = ar.alloc('hT', [128, NCH, T], BF16)
    yaT = ar.alloc('yaT', [128, 4, T], BF16)
    cosw = ar.alloc('cosw', [128, W], F32)
    sinw = ar.alloc('sinw', [128, W], F32)
    zT2 = [ar.alloc('zT', [128, W], BF16) for i in range(2)]
    tt2 = [ar.alloc('tt', [128, W], F32) for i in range(2)]
    uu2 = [ar.alloc('uu', [128, W], F32) for i in range(2)]
    PT = [ar.alloc('PT', [128, 1024], BF16) for i in range(2)]
    bv_sb = ar.alloc('bv_sb', [128, 128], F32)
    esinkT2 = ar.alloc('esinkT2', [128, 2, 512], F32)
    psum = [stack.enter_context(nc.psum_tensor('ps%d' % i, [128, 512], F32)) for i in range(8)]

    P.add('dve', lambda e: e.memset(ones_bf[:], 1.0), w=['ones'])
    P.add('sp', lambda e: e.dma_start(out=gmix_sb[:], in_=gmix), w=['gmix'], dma=True)
    P.add('sp', lambda e: e.dma_start(out=bin_sb[:], in_=binb), w=['bin'], dma=True)
    P.add('sp', lambda e: e.dma_start(out=cbf_sb[:], in_=cbf[:, 0:768]), w=['cbf'], dma=True)
    P.add('sp', lambda e: e.dma_start(out=gffn_sb[:], in_=gffn), w=['gffn'], dma=True)
    P.add('sp', lambda e: e.dma_start(out=esink[:], in_=sinkrep), w=['esink'], dma=True)
    P.add('act', lambda e: e.activation(out=esink[:], in_=esink[:], func=AF.Exp), r=['esink'], w=['esink'])
    for kvh in range(2):
        for half in range(2):
            for j in range(4):
                qh = kvh * 8 + 4 * half + j
                rws = slice(64 * kvh, 64 * kvh + 64)
                P.add('dve', lambda e, rws=rws, half=half, j=j, qh=qh: e.tensor_copy(
                    out=esinkT2[rws, half, j * 128:(j + 1) * 128], in_=esink[rws, qh:qh + 1].broadcast_to([64, 128])),
                    r=['esink'], w=['esinkT2'])

    m1 = ar.mark()
    xw = [ar.alloc('xw', [128, NCH, W], F32) for i in range(3)]
    sq = [ar.alloc('sq', [128, NCH, W], BF16) for i in range(3)]
    rstd = [ar.alloc('rstd', [128, W], F32) for i in range(3)]
    srt = rstd
    for w in range(NW):
        s = w % 3
        ws = slice(w * W, (w + 1) * W)
        P.add('sp', lambda e, s=s, ws=ws: e.dma_start(out=xw[s][:], in_=xTv[:, :, ws]),
              w=[('xw', s)], dma=True)
        P.add('act', lambda e, s=s: e.activation(out=sq[s][:], in_=xw[s][:], func=AF.Square),
              r=[('xw', s)], w=[('sq', s)])
        pb = psum[s]
        for c in range(NCH):
            P.add('pe', lambda e, s=s, c=c, pb=pb: e.matmul(pb[:], lhsT=ones_bf[:], rhs=sq[s][:, c, :],
                                                           start=(c == 0), stop=(c == NCH - 1)),
                  r=[('sq', s), 'ones'], w=[('ps', s)])
        P.add('act', lambda e, s=s, pb=pb: e.activation(out=srt[s][:], in_=pb[:], func=AF.Ln,
                                                        scale=1.0 / D, bias=EPS),
              r=[('ps', s)], w=[('srt', s), ('rstd', s)])
        P.add('act', lambda e, s=s: e.activation(out=rstd[s][:], in_=srt[s][:], func=AF.Exp, scale=-0.5),
              r=[('srt', s)], w=[('rstd', s)])
        for c in range(NCH):
            P.add('dve', lambda e, s=s, c=c, ws=ws: e.scalar_tensor_tensor(
                out=hT[:, c, ws], in0=xw[s][:, c, :], scalar=gmix_sb[:, c:c + 1], in1=rstd[s][:],
                op0=ALU.mult, op1=ALU.mult),
                r=[('xw', s), ('rstd', s), 'gmix'], w=[('hT', w)])
    P.barrier('p1')
    ar.release(m1)

    mA = ar.mark()
    KT = [ar.alloc('KT', [128, T], BF16) for g in range(3)]
    Vst = [ar.alloc('Vst', [128, 32, 128], BF16) for g in range(3)]
    QW = [ar.alloc('QW', [128, W], BF16) for g in range(3)]
    wkv = [ar.alloc('wkv', [128, NCH, 128], BF16) for g in range(3)]
    wq = [ar.alloc('wq', [128, NCH, 128], BF16) for g in range(3)]
    mska = ar.alloc('mska', [128, 512 * 5], BF16)
    maskA = mska[:, 0:512]
    maskA16 = [mska[:, 512 * (v + 1): 512 * (v + 2)] for v in range(4)]
    P.add('sp', lambda e: e.dma_start(out=mska[:], in_=cbf[:, 768:768 + 2560]), w=['cbf'], dma=True)
    Uacc = ar.alloc('Uacc', [128, W], F32)
    Dacc = ar.alloc('Dacc', [128, W], F32)
    rD = ar.alloc('rD', [128, W], F32)

    PS_Z, PS_R, PS_U, PS_D = 0, 1, 6, 7
    PS_S = [(4, 5), (0, 1)]
    unit_ctr = [0]

    def load_w_raw(dst, blk, res):
        load_w(dst, blk, res)

    def load_w(dst, blk, res):
        P.add('pool', lambda e: e.dma_start(out=dst[:].rearrange('p c n -> p (c n)'), in_=win[blk].rearrange('p c n -> p (c n)')), w=[res], dma=True)

    def load_cs(w):
        ws = slice(w * W, (w + 1) * W)
        P.add('sp', lambda e: e.dma_start(out=cosw[:], in_=cosd[:, ws]), w=['cosw'], dma=True)
        P.add('sp', lambda e: e.dma_start(out=sinw[:], in_=sind[:, ws]), w=['sinw'], dma=True)

    proj_ctr = [0]
    proj_pend = {'p': None}

    def proj_flush():
        if proj_pend['p'] is not None:
            proj_pend['p']()
        proj_pend['p'] = None

    def proj_rope(wt, wres, blk, w, dst_ap, dst_res, d):
        ws = slice(w * W, (w + 1) * W)
        zb = proj_ctr[0] % 2
        proj_ctr[0] += 1
        bz, br = 0 + zb, 2 + zb
        pz, pr = psum[bz], psum[br]
        zT, tt, uu = zT2[zb], tt2[zb], uu2[zb]
        for c in range(NCH):
            P.add('pe', lambda e, c=c: e.matmul(pz[:], lhsT=wt[:, c, :], rhs=hT[:, c, ws],
                                                start=(c == 0), stop=(c == NCH - 1)),
                  r=[wres, ('hT', w)], w=[('ps', bz)])
        P.add('act', lambda e: e.activation(out=zT[:], in_=pz[:], func=AF.Identity,
                                            bias=bin_sb[:, blk:blk + 1], scale=1.0),
              r=[('ps', bz), 'bin'], w=[('zT', zb)])

        def part2():
            P.add('pe', lambda e: e.matmul(pr[:], lhsT=rotT, rhs=zT[:], start=True, stop=True),
                  r=[('zT', zb), 'cbf'], w=[('ps', br)])
            P.add('dve', lambda e: e.tensor_tensor(out=uu[:], in0=pr[:], in1=sinw[:], op=ALU.mult),
                  r=[('ps', br), 'sinw'], w=[('uu', zb)])
            P.add('pool', lambda e: e.tensor_tensor(out=tt[:], in0=zT[:], in1=cosw[:], op=ALU.mult),
                  r=[('zT', zb), 'cosw'], w=[('tt', zb)])
            if d == 1:
                a, b = tt[:], uu[:]
            else:
                a = tt[:].rearrange('p (j r) -> p r j', r=d)
                b = uu[:].rearrange('p (j r) -> p r j', r=d)
            P.add('dve', lambda e: e.tensor_tensor(out=dst_ap, in0=a, in1=b, op=ALU.add),
                  r=[('tt', zb), ('uu', zb)], w=[dst_res])

        prev = proj_pend['p']
        proj_pend['p'] = part2
        if prev is not None:
            prev()

    pipe = {'pending': None}

    def pipe_push(front, back):
        front()
        if pipe['pending'] is not None:
            pipe['pending']()
        pipe['pending'] = back

    def pipe_flush():
        if pipe['pending'] is not None:
            pipe['pending']()
        pipe['pending'] = None

    def attn_unit(tiles, qsrc, qres, ksrc, kres_fn, vsrc, vres, mask_ap, hrow, evac, after=None):
        pipe_push(*attn_unit_parts(tiles, qsrc, qres, ksrc, kres_fn, vsrc, vres, mask_ap, hrow, evac, after))

    def attn_unit_parts(tiles, qsrc, qres, ksrc, kres_fn, vsrc, vres, mask_ap, hrow, evac, after):
        u = unit_ctr[0]
        unit_ctr[0] += 1
        sl = u % 2
        sb0, sb1 = PS_S[sl]
        pt = PT[sl]
        rows = slice(hrow, hrow + 64)
        PS_U, PS_D = (6, 7) if u % 2 == 0 else (2, 3)

        def front():
          for i, (qc, nq, kbp, kbc, kcp, kcc, uc) in enumerate(tiles):
            for half, kc in ((0, kcp), (1, kcc)):
                col = i * 2 * nq + half * nq
                bank = sb0 if col < 512 else sb1
                cc = col % 512
                P.add('pe', lambda e, bank=bank, cc=cc, kc=kc, qc=qc, nq=nq: e.matmul(
                    psum[bank][:, cc:cc + nq], lhsT=ksrc[rows, kc:kc + 128], rhs=qsrc[rows, qc:qc + nq],
                    start=True, stop=True),
                    r=list(qres) + kres_fn(kc), w=[('ps', bank)])
          for hb, bank in ((0, sb0), (1, sb1)):
            P.add('act', lambda e, hb=hb, bank=bank: e.activation(
                out=pt[:, hb * 512:(hb + 1) * 512], in_=psum[bank][:], func=AF.Exp, scale=0.125),
                r=[('ps', bank)], w=[('PT', sl, hb)])
            P.add('dve' if hb == 0 else 'pool', lambda e, hb=hb: e.tensor_tensor(
                out=pt[:, hb * 512:(hb + 1) * 512], in0=pt[:, hb * 512:(hb + 1) * 512], in1=mask_ap,
                op=ALU.mult),
                r=[('PT', sl, hb), 'cbf'], w=[('PT', sl, hb)])
        def back():
          first = True
          for i, (qc, nq, kbp, kbc, kcp, kcc, uc) in enumerate(tiles):
            for half, kb in ((0, kbp), (1, kbc)):
                if kb is None:
                    continue
                col = i * 2 * nq + half * nq
                hb = col // 512
                P.add('pe', lambda e, first=first, col=col, kb=kb, uc=uc, nq=nq: e.matmul(
                    psum[PS_U][:, uc:uc + nq], lhsT=vsrc[:, kb, :], rhs=pt[:, col:col + nq],
                    start=first, stop=False, skip_group_check=True),
                    r=[('PT', sl, hb)] + vres(kb), w=[('ps', PS_U)])
                P.add('pe', lambda e, first=first, col=col, kb=kb, uc=uc, nq=nq: e.matmul(
                    psum[PS_D][:, uc:uc + nq], lhsT=ones_bf[:], rhs=pt[:, col:col + nq],
                    start=first, stop=False, skip_group_check=True),
                    r=[('PT', sl, hb), 'ones'], w=[('ps', PS_D)])
                first = False
          evac(rows, PS_U, PS_D)
          if after is not None:
              after()
        return front, back

    for p in range(4):
        for g in range(3):
            load_w(wkv[g], BIDX[('A', p, 'k', g)], ('wkv', g))
        for w in range(NW):
            proj_flush()
            load_cs(w)
            for g in range(3):
                d = DIL[g]
                L = T // d
                dst = KT[g][:].rearrange('p (r j) -> p r j', r=d)[:, :, w * W // d:(w + 1) * W // d] if d > 1 \
                    else KT[g][:, w * W:(w + 1) * W]
                proj_rope(wkv[g], ('wkv', g), BIDX[('A', p, 'k', g)], w, dst, ('KT', g), d)
        proj_flush()
        for g in range(3):
            load_w(wkv[g], BIDX[('A', p, 'v', g)], ('wkv', g))
        for g in range(3):
            d = DIL[g]
            vb = BIDX[('A', p, 'v', g)]
            P.add('sp', lambda e, vb=vb: e.dma_start(out=bv_sb[:], in_=bvrep[VIDX[vb]]), w=['bv'], dma=True)
            for bg in range(8):
                bank = 4 + (bg % 2)
                for i in range(4):
                    blk = bg * 4 + i
                    t0, st = perm_block_tokens(d, blk)
                    for c in range(NCH):
                        P.add('pe', lambda e, c=c, i=i, t0=t0, st=st, g=g, bank=bank: e.matmul(
                            psum[bank][:, i * 128:(i + 1) * 128],
                            lhsT=hT[:, c, t0:t0 + 127 * st + 1:st], rhs=wkv[g][:, c, :],
                            start=(c == 0), stop=(c == NCH - 1)),
                            r=[('wkv', g)] + [('hT', ww) for ww in range(NW)], w=[('ps', bank)])
                P.add('dve', lambda e, g=g, bg=bg, bank=bank: e.tensor_tensor(
                    out=Vst[g][:, bg * 4:(bg + 1) * 4, :], in0=psum[bank][:].rearrange('p (i n) -> p i n', i=4),
                    in1=bv_sb[:].unsqueeze(1).broadcast_to([128, 4, 128]), op=ALU.add),
                    r=[('ps', bank), 'bv'], w=[('Vst', g)])
        for g in range(3):
            load_w(wq[g], BIDX[('A', p, 'q', g)], ('wq', g))
        for w in range(NW):
            it = p * NW + w
            if it < 7:
                for k2 in (2 * it, 2 * it + 1):
                    if k2 < len(casts2):
                        s_, d_ = casts2[k2]
                        P.add('pool', lambda e, s_=s_, d_=d_: e.dma_start(out=d_, in_=s_), w=[('wcast2', k2)], dma=True)
            elif it - 7 < 24:
                ee = it - 7
                src_, dst_ = ((wegR, wgb), (weuR, wub), (wedR, wdb))[ee % 3]
                rws = slice((ee // 3) * 512, (ee // 3 + 1) * 512)
                P.add('pool', lambda e, src_=src_, dst_=dst_, rws=rws: e.dma_start(out=dst_[rws, :], in_=src_[rws, :]),
                      w=[('wcast', ee)], dma=True)
            load_cs(w)
            for g in range(3):
                d = DIL[g]
                dst = QW[g][:].rearrange('p (r j) -> p r j', r=d) if d > 1 else QW[g][:]
                proj_rope(wq[g], ('wq', g), BIDX[('A', p, 'q', g)], w, dst, ('QW', g), d)
            proj_flush()
            for hh in range(2):
                for g in range(3):
                    d = DIL[g]
                    L = T // d
                    tiles = []
                    if d == 1:
                        for i in range(4):
                            qb = 4 * w + i
                            kbp = qb - 1 if qb >= 1 else None
                            tiles.append((128 * i, 128, kbp, qb, 128 * max(qb - 1, 0), 128 * qb, 128 * i))
                        mask_ap = maskA
                    elif d == 4:
                        for r in range(4):
                            qb = w
                            base = r * (L // 128)
                            kbp = base + qb - 1 if qb >= 1 else None
                            tiles.append((128 * r, 128, kbp, base + qb, 128 * (base + max(qb - 1, 0)), 128 * (base + qb), 128 * r))
                        mask_ap = maskA
                    else:
                        for r in range(16):
                            qb = w // 4
                            base = r * (L // 128)
                            kbp = base + qb - 1 if qb >= 1 else None
                            tiles.append((32 * r, 32, kbp, base + qb, 128 * (base + max(qb - 1, 0)), 128 * (base + qb), 32 * r))
                        mask_ap = maskA16[w % 4]

                    def evac(rows, PS_U, PS_D, g=g, d=d):
                        if g == 0:
                            P.add('act', lambda e: e.activation(out=Uacc[rows, :], in_=psum[PS_U][rows, :], func=AF.Copy),
                                  r=[('ps', PS_U)], w=['Uacc'])
                            P.add('act', lambda e: e.activation(out=Dacc[rows, :], in_=psum[PS_D][rows, :], func=AF.Copy),
                                  r=[('ps', PS_D)], w=['Dacc'])
                        else:
                            for acc, bank, nm in ((Uacc, PS_U, 'Uacc'), (Dacc, PS_D, 'Dacc')):
                                av = acc[rows, :].rearrange('p (j r) -> p r j', r=d)
                                pv = psum[bank][rows, :].rearrange('p (r j) -> p r j', r=d)
                                P.add('dve', lambda e, av=av, pv=pv: e.tensor_tensor(out=av, in0=av, in1=pv, op=ALU.add),
                                      r=[('ps', bank), nm], w=[nm])

                    def fin(p=p, w=w):
                        ws = slice(w * W, (w + 1) * W)
                        P.add('act', lambda e: e.activation(out=rD[:], in_=Dacc[:], func=AF.Ln), r=['Dacc'], w=['rD'])
                        P.add('act', lambda e: e.activation(out=rD[:], in_=rD[:], func=AF.Exp, scale=-1.0), r=['rD'], w=['rD'])
                        P.add('dve', lambda e: e.tensor_tensor(out=yaT[:, p, ws], in0=Uacc[:], in1=rD[:], op=ALU.mult),
                              r=['Uacc', 'rD'], w=[('yaT', p, w)])

                    attn_unit(tiles, QW[g], [('QW', g)], KT[g], lambda kc, g=g: [('KT', g)], Vst[g], lambda kb, g=g: [('Vst', g)],
                              mask_ap, 64 * hh, evac, after=(fin if (hh == 1 and g == 2) else None))
        pipe_flush()
    P.barrier('pA')
    ar.release(mA)

    m3 = ar.mark()
    acc = ar.alloc('acc', [128, NCH, W], F32)
    P.add('sp', lambda e: e.dma_start(out=identf[:], in_=identf_d), w=['identf'], dma=True)
    P.add('sp', lambda e: e.nop(), r=[('wcast2', k2) for k2 in range(len(casts2))], w=['wcast2All'])
    KBq = ar.alloc('KBq', [128, 128 + 2 * W], BF16)
    VBq = ar.alloc('VBq', [128, 9, 128], BF16)
    wsl = [ar.alloc('wsl', [128, NCH, 128], BF16) for i in range(3)]
    maskB2 = ar.alloc('maskB2', [128, 1024], BF16)
    for hb in range(2):
        P.add('dve', lambda e, hb=hb: e.tensor_copy(
            out=maskB2[:, hb * 512:(hb + 1) * 512].rearrange('p (j q) -> p j q', j=4),
            in_=maskB[:, hb * 128:(hb + 1) * 128].unsqueeze(1).broadcast_to([128, 4, 128])),
            r=['cbf'], w=['maskB2'])

    def attn_unit_B(QB, i, kvh, lb, has_prev, evac):
        u = unit_ctr[0]
        unit_ctr[0] += 1
        sl = u % 2
        sb0, sb1 = PS_S[sl]
        pt = PT[sl]
        rows = slice(64 * kvh, 64 * kvh + 64)
        PS_U, PS_D = (6, 7) if u % 2 == 0 else (2, 3)
        kcp, kcc = 128 * (lb - 1 if has_prev else lb), 128 * lb
        qv = QB[rows, :].rearrange('p (j q) -> p j q', j=4)[:, :, 128 * i:128 * (i + 1)]
        kres = [('KBq', 'prev'), ('KBq', 0), ('KBq', 1)]
        vres = [('VBq', 'prev'), ('VBq', 0), ('VBq', 1)]

        def front():
            for bank, kc in ((sb0, kcp), (sb1, kcc)):
                P.add('pe', lambda e, bank=bank, kc=kc: e.matmul(
                    psum[bank][:].rearrange('p (j q) -> p j q', j=4), lhsT=KBq[rows, kc:kc + 128], rhs=qv,
                    start=True, stop=True),
                    r=[('QB', j) for j in range(4)] + kres, w=[('ps', bank)])
            for hb, bank in ((0, sb0), (1, sb1)):
                P.add('act', lambda e, hb=hb, bank=bank: e.activation(
                    out=pt[:, hb * 512:(hb + 1) * 512], in_=psum[bank][:], func=AF.Exp, scale=0.125),
                    r=[('ps', bank)], w=[('PT', sl, hb)])
                P.add('dve' if hb == 0 else 'pool', lambda e, hb=hb: e.tensor_tensor(
                    out=pt[:, hb * 512:(hb + 1) * 512], in0=pt[:, hb * 512:(hb + 1) * 512],
                    in1=maskB2[:, hb * 512:(hb + 1) * 512], op=ALU.mult),
                    r=[('PT', sl, hb), 'maskB2'], w=[('PT', sl, hb)])

        def back():
            first = True
            for hb, kb in ((0, (lb - 1) if has_prev else None), (1, lb)):
                if kb is None:
                    continue
                P.add('pe', lambda e, first=first, hb=hb, kb=kb: e.matmul(
                    psum[PS_U][:], lhsT=VBq[:, kb, :], rhs=pt[:, hb * 512:(hb + 1) * 512],
                    start=first, stop=False, skip_group_check=True),
                    r=[('PT', sl, hb)] + vres, w=[('ps', PS_U)])
                P.add('pe', lambda e, first=first, hb=hb: e.matmul(
                    psum[PS_D][:], lhsT=ones_bf[:], rhs=pt[:, hb * 512:(hb + 1) * 512],
                    start=first, stop=False, skip_group_check=True),
                    r=[('PT', sl, hb), 'ones'], w=[('ps', PS_D)])
                first = False
            evac(rows, PS_U, PS_D)
        return front, back

    wstate = {'issued': 0, 'used': 0}
    msub = ar.mark()

    def phase3(w):
        lw = w % 2
        ws = slice(w * W, (w + 1) * W)
        lws = slice(0, W)
        ar.release(msub)
        QB = ar.alloc('QB', [128, 4 * W], BF16)
        ybW = ar.alloc('ybW', [128, NCH, W], BF16)
        wa_s = [ar.alloc('wa_s', [128, 4, 128], BF16) for i in range(3)]
        wb_s = [ar.alloc('wb_s', [128, NCH, 128], BF16) for i in range(3)]
        wga_s = [ar.alloc('wga_s', [128, NCH, 128], BF16) for i in range(3)]
        wgb_s = [ar.alloc('wgb_s', [128, NCH, 128], BF16) for i in range(3)]
        wo_s = [ar.alloc('wo_s', [128, NCH, 128], BF16) for i in range(2)]
        ga = ar.alloc('ga', [128, W], F32)
        gb = ar.alloc('gb', [128, W], F32)
        mergedT = ar.alloc('mergedT', [128, NCH, W], BF16)
        xres = [ar.alloc('xres', [128, W], F32) for i in range(2)]
        Dt2 = [ga, gb]
        dctr = [0]
        blk_seq = [BIDX[('B', 'k')], BIDX[('B', 'v')]] + [BIDX[('B', 'q', gi)] for gi in range(8)]
        NBW = len(blk_seq)

        def load_w_raw(dst, blk, res):
            r0 = (blk - B0) * 128
            P.add('sp', lambda e: e.dma_start(out=dst[:].rearrange('p c n -> p (c n)'), in_=winb[r0:r0 + 128, :]),
                  r=['wcast2All'], w=[res], dma=True)

        def prefetch_to(n):
            while wstate['issued'] < min(n, NBW * NW):
                b = wstate['issued']
                load_w_raw(wsl[b % 3], blk_seq[b % NBW], ('wsl', b % 3))
                wstate['issued'] += 1

        def next_wsl():
            b = wstate['used']
            prefetch_to(b + 3)
            wstate['used'] += 1
            return wsl[b % 3], ('wsl', b % 3)

        def load_merge(f):
            s3 = f % 3
            P.add('sp', lambda e: e.dma_start(out=wa_s[s3][:].rearrange('p c n -> p (c n)'), in_=wpab[f * 128:(f + 1) * 128, :]),
                  r=['wcast2All'], w=[('wa', s3)], dma=True)
            P.add('sp', lambda e: e.dma_start(out=wb_s[s3][:].rearrange('p c n -> p (c n)'), in_=wpbb[f * 128:(f + 1) * 128, :]),
                  r=['wcast2All'], w=[('wb', s3)], dma=True)
            load_w_raw(wga_s[s3], BIDX[('G', f)], ('wga', s3))
            load_w_raw(wgb_s[s3], BIDX[('G', 8 + f)], ('wgb', s3))

        wo3 = [wo_s[0][:].rearrange('p c n -> p (c n)'), wo_s[1][:].rearrange('p c n -> p (c n)'), QB[:, 0:NCH * 128]]
        wo3res = [[('wo', 0)], [('wo', 1)], [('QB', 0), ('QB', 1)]]

        def load_wo(f2):
            P.add('sp', lambda e: e.dma_start(out=wo3[f2 % 3], in_=wob[f2 * 128:(f2 + 1) * 128, :]),
                  r=['wcast2All'], w=wo3res[f2 % 3], dma=True)

        load_merge(0)
        load_cs(w)
        wt, wres = next_wsl()
        proj_rope(wt, wres, BIDX[('B', 'k')], w, KBq[:, 128 + lw * W:128 + (lw + 1) * W],
                  ('KBq', lw), 1)
        proj_flush()
        wt, wres = next_wsl()
        vb = BIDX[('B', 'v')]
        P.add('sp', lambda e: e.dma_start(out=bv_sb[:], in_=bvrep[VIDX[vb]]), w=['bv'], dma=True)
        bank = PS_S[0][0]
        for i in range(4):
            t0 = w * W + 128 * i
            for c in range(NCH):
                P.add('pe', lambda e, c=c, i=i, t0=t0, wt=wt: e.matmul(
                    psum[bank][:, i * 128:(i + 1) * 128], lhsT=hT[:, c, t0:t0 + 128], rhs=wt[:, c, :],
                    start=(c == 0), stop=(c == NCH - 1)),
                    r=[wres, ('hT', w)], w=[('ps', bank)])
        P.add('dve', lambda e: e.tensor_tensor(
            out=VBq[:, 1 + 4 * lw:5 + 4 * lw, :], in0=psum[bank][:].rearrange('p (i n) -> p i n', i=4),
            in1=bv_sb[:].unsqueeze(1).broadcast_to([128, 4, 128]), op=ALU.add),
            r=[('ps', bank), 'bv'], w=[('VBq', lw)])
        for half in range(2):
            for j in range(4):
                gi = 4 * half + j
                wt, wres = next_wsl()
                proj_rope(wt, wres, BIDX[('B', 'q', gi)], w, QB[:, j * W:(j + 1) * W], ('QB', j), 1)
            proj_flush()
            load_merge(1 + half)
            for i in range(4):
                lb = 1 + 4 * lw + i
                has_prev = not (w == 0 and i == 0)
                for kvh in range(2):
                    tiles = []
                    for j in range(4):
                        tiles.append((j * W + 128 * i, 128, (lb - 1) if has_prev else None, lb,
                                      128 * (lb - 1 if has_prev else lb), 128 * lb, 128 * j))

                    def evac(rows, PS_U, PS_D, kvh=kvh, i=i, half=half):
                        di = dctr[0] % 2
                        dctr[0] += 1
                        Dt = Dt2[di]
                        dn = 'ga' if di == 0 else 'gb'
                        P.add('dve', lambda e: e.tensor_tensor(out=Dt[rows, :], in0=psum[PS_D][rows, :], in1=esinkT2[rows, half, :],
                                                               op=ALU.add),
                              r=[('ps', PS_D), 'esinkT2'], w=[dn])
                        P.add('act', lambda e: e.activation(out=Dt[rows, :], in_=Dt[rows, :], func=AF.Ln), r=[dn], w=[dn])
                        P.add('act', lambda e: e.activation(out=Dt[rows, :], in_=Dt[rows, :], func=AF.Exp, scale=-1.0), r=[dn], w=[dn])
                        P.add('dve', lambda e: e.tensor_tensor(
                            out=ybW[rows, 4 * half:4 * half + 4, 128 * i:128 * (i + 1)],
                            in0=psum[PS_U][rows, :].rearrange('p (j q) -> p j q', j=4),
                            in1=Dt[rows, :].rearrange('p (j q) -> p j q', j=4), op=ALU.mult),
                            r=[('ps', PS_U), dn], w=['ybW'])

                    def kres(kc, lw=lw):
                        return [('KBq', 'prev'), ('KBq', 0), ('KBq', 1)]

                    def vres(kb, lw=lw):
                        return [('VBq', 'prev'), ('VBq', 0), ('VBq', 1)]

                    pipe_push(*attn_unit_B(QB, i, kvh, lb, has_prev, evac))
        pipe_flush()
        if lw == 1:
            P.add('dve', lambda e: e.tensor_copy(out=KBq[:, 0:128], in_=KBq[:, 2 * W:2 * W + 128]),
                  r=[('KBq', 1)], w=[('KBq', 'prev')])
            P.add('dve', lambda e: e.tensor_copy(out=VBq[:, 0, :], in_=VBq[:, 8, :]),
                  r=[('VBq', 1)], w=[('VBq', 'prev')])
        for f in range(8):
            s = f % 2
            bA, bB, bGA, bGB = (2, 3, 4, 5) if s == 0 else (6, 7, 0, 1)
            s = f % 3
            if f == 0:
                load_wo(0)
                load_wo(1)
                load_wo(2)
            for c in range(4):
                P.add('pe', lambda e, c=c, s=s: e.matmul(psum[bA][:], lhsT=wa_s[s][:, c, :], rhs=yaT[:, c, ws],
                                                         start=(c == 0), stop=(c == 3)),
                      r=[('wa', s)] + [('yaT', c, w)], w=[('ps', bA)])
            for c in range(NCH):
                P.add('pe', lambda e, c=c, s=s: e.matmul(psum[bB][:], lhsT=wb_s[s][:, c, :], rhs=ybW[:, c, :],
                                                         start=(c == 0), stop=(c == NCH - 1)),
                      r=[('wb', s), 'ybW'], w=[('ps', bB)])
            for c in range(NCH):
                P.add('pe', lambda e, c=c, s=s: e.matmul(psum[bGA][:], lhsT=wga_s[s][:, c, :], rhs=hT[:, c, ws],
                                                         start=(c == 0), stop=(c == NCH - 1)),
                      r=[('wga', s), ('hT', w)], w=[('ps', bGA)])
            for c in range(NCH):
                P.add('pe', lambda e, c=c, s=s: e.matmul(psum[bGB][:], lhsT=wgb_s[s][:, c, :], rhs=hT[:, c, ws],
                                                         start=(c == 0), stop=(c == NCH - 1)),
                      r=[('wgb', s), ('hT', w)], w=[('ps', bGB)])
            if f + 3 < 8:
                load_merge(f + 3)
            ba, bb = BIDX[('G', f)], BIDX[('G', 8 + f)]
            P.add('act', lambda e, ba=ba: e.activation(out=ga[:], in_=psum[bGA][:], func=AF.Sigmoid,
                                                       bias=bin_sb[:, ba:ba + 1], scale=1.0),
                  r=[('ps', bGA), 'bin'], w=['ga'])
            P.add('act', lambda e, bb=bb: e.activation(out=gb[:], in_=psum[bGB][:], func=AF.Sigmoid,
                                                       bias=bin_sb[:, bb:bb + 1], scale=1.0),
                  r=[('ps', bGB), 'bin'], w=['gb'])
            P.add('dve', lambda e: e.tensor_tensor(out=ga[:], in0=psum[bA][:], in1=ga[:], op=ALU.mult),
                  r=[('ps', bA), 'ga'], w=['ga'])
            P.add('dve', lambda e: e.tensor_tensor(out=gb[:], in0=psum[bB][:], in1=gb[:], op=ALU.mult),
                  r=[('ps', bB), 'gb'], w=['gb'])
            P.add('dve', lambda e, f=f: e.tensor_tensor(out=mergedT[:, f, :], in0=ga[:], in1=gb[:], op=ALU.add),
                  r=['ga', 'gb'], w=['mergedT'])
        for f2 in range(8):
            s = f2 % 2
            P.add('sp', lambda e, f2=f2, s=s: e.dma_start(out=xres[s][:], in_=xTv[:, f2, ws]), w=[('xres', s)], dma=True)
            for c in range(NCH):
                P.add('pe', lambda e, c=c, s=s, f2=f2: e.matmul(psum[s][:], lhsT=wo3[f2 % 3][:, c * 128:(c + 1) * 128], rhs=mergedT[:, c, :],
                                                         start=(c == 0), stop=(c == NCH - 1)),
                      r=wo3res[f2 % 3] + ['mergedT'], w=[('ps', s)])
            P.add('dve', lambda e, f2=f2, s=s: e.tensor_tensor(out=acc[:, f2, lws], in0=psum[s][:], in1=xres[s][:],
                                                               op=ALU.add),
                  r=[('ps', s), ('xres', s)], w=['acc'])
            if f2 + 3 < 8:
                load_wo(f2 + 3)

    def norm_stats(sqb, srtb, rstdb):
        P.add('act', lambda e: e.activation(out=sqb[:], in_=acc[:], func=AF.Square), r=['acc'], w=['sqb'])
        for c in range(NCH):
            P.add('pe', lambda e, c=c: e.matmul(psum[7][:], lhsT=ones_bf[:], rhs=sqb[:, c, :],
                                                start=(c == 0), stop=(c == NCH - 1)),
                  r=['sqb', 'ones'], w=[('ps', 7)])
        P.add('act', lambda e: e.activation(out=srtb[:], in_=psum[7][:], func=AF.Ln, scale=1.0 / D, bias=EPS),
              r=[('ps', 7)], w=['srtb'])
        P.add('act', lambda e: e.activation(out=rstdb[:], in_=srtb[:], func=AF.Exp, scale=-0.5), r=['srtb'], w=['rstdb'])

    def post_window(w):
        ar.release(msub)
        sqb = ar.alloc('sqb', [128, NCH, W], BF16)
        srtb = ar.alloc('srtb', [128, W], F32)
        rstdb = ar.alloc('rstdb', [128, W], F32)
        h2f = ar.alloc('h2f', [128, NCH, W], F32)
        h2Tw = ar.alloc('h2Tw', [128, NCH, W], BF16)
        wr_sb = ar.alloc('wr_sb', [128, NCH, 36], F32)
        brep_sb = ar.alloc('brep_sb', [128, 36], F32)
        L4 = ar.alloc('L4', [128, 4, 36], F32)
        Lg = ar.alloc('Lg', [128, 4, 4], F32)
        oh4 = ar.alloc('oh4', [128, 4, 4], F32)
        Lm4 = ar.alloc('Lm4', [128, 4, 32], F32)
        ee4 = ar.alloc('ee4', [128, 4, 32], F32)
        tm4 = ar.alloc('tm4', [128, 4, 32], F32)
        top84 = ar.alloc('top84', [128, 4, 8], F32)
        sm4 = ar.alloc('sm4', [128, 12, 4], F32)
        h2tok = [ar.alloc('h2tok', [128, D], BF16) for i in range(2)]
        x1tok = [ar.alloc('x1tok', [128, D], F32) for i in range(2)]
        P.add('sp', lambda e: e.dma_start(out=wr_sb[:], in_=wr), w=['wr'], dma=True)
        P.add('sp', lambda e: e.dma_start(out=brep_sb[:], in_=brep), w=['brep'], dma=True)
        BIG = 30000.0
        def S1(t4):
            ts_ = slice(t4 * 128, (t4 + 1) * 128)
            P.add('act', lambda e: e.activation(out=sqb[:, :, ts_], in_=acc[:, :, ts_], func=AF.Square),
                  r=['acc'], w=[('sqb', t4)])
            for c in range(NCH):
                P.add('pe', lambda e, c=c: e.matmul(psum[7][:, ts_], lhsT=ones_bf[:], rhs=sqb[:, c, ts_],
                                                    start=(c == 0), stop=(c == NCH - 1)),
                      r=[('sqb', t4), 'ones'], w=[('ps7', t4)])
            P.add('act', lambda e: e.activation(out=srtb[:, ts_], in_=psum[7][:, ts_], func=AF.Ln, scale=1.0 / D, bias=EPS),
                  r=[('ps7', t4)], w=[('srtb', t4)])
            P.add('act', lambda e: e.activation(out=rstdb[:, ts_], in_=srtb[:, ts_], func=AF.Exp, scale=-0.5),
                  r=[('srtb', t4)], w=[('rstdb', t4)])

        def S2(t4):
            ts_ = slice(t4 * 128, (t4 + 1) * 128)
            for c in range(NCH):
                P.add('dve', lambda e, c=c: e.scalar_tensor_tensor(
                    out=h2f[:, c, ts_], in0=acc[:, c, ts_], scalar=gffn_sb[:, c:c + 1], in1=rstdb[:, ts_],
                    op0=ALU.mult, op1=ALU.mult),
                    r=['acc', ('rstdb', t4), 'gffn'], w=[('h2f', t4)])
            P.add('act', lambda e: e.activation(out=h2Tw[:, :, ts_], in_=h2f[:, :, ts_], func=AF.Copy),
                  r=[('h2f', t4)], w=[('h2Tw', t4)])

        def S3a(t4):
            tg = 4 * w + t4
            s2 = t4 % 2
            ts_ = slice(t4 * 128, (t4 + 1) * 128)
            rows = slice(tg * 128, (tg + 1) * 128)
            b0 = 2 + 2 * s2
            for c in range(NCH):
                bank = b0 + c // 4
                P.add('pe', lambda e, c=c, bank=bank: e.transpose(
                    out=psum[bank][:, (c % 4) * 128:(c % 4 + 1) * 128], in_=acc[:, c, ts_], identity=identf[:]),
                    r=['acc', 'identf'], w=[('ps', bank)])
            P.add('dve', lambda e: e.tensor_copy(out=x1tok[s2][:, 0:512], in_=psum[b0][:]),
                  r=[('ps', b0)], w=[('x1tok', s2, 0)])
            P.add('dve', lambda e: e.tensor_copy(out=x1tok[s2][:, 512:1024], in_=psum[b0 + 1][:]),
                  r=[('ps', b0 + 1)], w=[('x1tok', s2, 1)])
            P.add('sp', lambda e: e.dma_start(out=X1[rows, :], in_=x1tok[s2][:]),
                  r=[('x1tok', s2, 0), ('x1tok', s2, 1)], w=[('X1', tg)], dma=True)

        def S3b(t4):
            tg = 4 * w + t4
            s2 = t4 % 2
            ts_ = slice(t4 * 128, (t4 + 1) * 128)
            rows = slice(tg * 128, (tg + 1) * 128)
            pb = psum[s2][:].bitcast(BF16)
            for c in range(NCH):
                P.add('pe', lambda e, c=c: e.transpose(out=pb[:, c * 128:(c + 1) * 128], in_=h2Tw[:, c, ts_], identity=ident),
                      r=[('h2Tw', t4), 'cbf'], w=[('ps', s2)])
            P.add('act', lambda e: e.activation(out=h2tok[s2][:], in_=pb, func=AF.Copy), r=[('ps', s2)], w=[('h2tok', s2)])
            P.add('sp', lambda e: e.dma_start(out=H2[rows, :], in_=h2tok[s2][:]), r=[('h2tok', s2)], w=[('H2', tg)], dma=True)
            pl = psum[6]
            for c in range(NCH):
                P.add('pe', lambda e, c=c: e.matmul(pl[:, t4 * 36:(t4 + 1) * 36], lhsT=h2f[:, c, ts_], rhs=wr_sb[:, c, :],
                                                    start=(c == 0), stop=(c == NCH - 1), skip_group_check=True),
                      r=[('h2f', t4), 'wr'], w=[('ps', 6)])

        for st in (lambda: S1(0), lambda: S3a(0), lambda: S1(1), lambda: S2(0), lambda: S3a(1), lambda: S1(2), lambda: S2(1),
                   lambda: S3b(0), lambda: S3a(2), lambda: S1(3), lambda: S2(2), lambda: S3b(1), lambda: S3a(3), lambda: S2(3),
                   lambda: S3b(2), lambda: S3b(3)):
            st()
        BIG = 30000.0
        tgs = slice(4 * w, 4 * w + 4)
        bc3 = lambda ap, n: ap.unsqueeze(2).broadcast_to([128, 4, n])
        P.add('dve', lambda e: e.tensor_tensor(out=L4[:], in0=psum[6][:, 0:144].rearrange('p (t n) -> p t n', t=4),
                                               in1=brep_sb[:].unsqueeze(1).broadcast_to([128, 4, 36]), op=ALU.add),
              r=[('ps', 6), 'brep'], w=['L4'])
        P.add('dve', lambda e: e.tensor_reduce(out=sm4[:, 0, :], in_=L4[:, :, 0:4], axis=AX.X, op=ALU.max), r=['L4'], w=['gmax'])
        P.add('dve', lambda e: e.tensor_tensor(out=Lg[:], in0=L4[:, :, 0:4], in1=bc3(sm4[:, 0, :], 4), op=ALU.subtract),
              r=['L4', 'gmax'], w=['Lg'])
        P.add('dve', lambda e: e.tensor_tensor(out=oh4[:], in0=L4[:, :, 0:4], in1=bc3(sm4[:, 0, :], 4), op=ALU.is_ge),
              r=['L4', 'gmax'], w=['oh4'])
        P.add('act', lambda e: e.activation(out=Lg[:], in_=Lg[:], func=AF.Exp), r=['Lg'], w=['Lg'])
        P.add('dve', lambda e: e.tensor_reduce(out=sm4[:, 1, :], in_=Lg[:], axis=AX.X, op=ALU.add), r=['Lg'], w=['sumg'])
        P.add('dve', lambda e: e.tensor_scalar(out=oh4[:], in0=oh4[:], scalar1=BIG, scalar2=-BIG, op0=ALU.mult, op1=ALU.add),
              r=['oh4'], w=['oh4'])
        P.add('dve', lambda e: e.tensor_tensor(
            out=Lm4[:].rearrange('p t (g x) -> p t g x', g=4), in0=L4[:, :, 4:36].rearrange('p t (g x) -> p t g x', g=4),
            in1=oh4[:].unsqueeze(3).broadcast_to([128, 4, 4, 8]), op=ALU.add), r=['L4', 'oh4'], w=['Lm4'])
        for t4 in range(4):
            P.add('dve', lambda e, t4=t4: e.max(out=top84[:, t4, :], in_=Lm4[:, t4, :]), r=['Lm4'], w=[('top84', t4)])
        t8 = [('top84', t4) for t4 in range(4)]
        P.add('dve', lambda e: e.tensor_tensor(out=ee4[:], in0=Lm4[:], in1=bc3(top84[:, :, 0], 32), op=ALU.subtract),
              r=['Lm4'] + t8, w=['ee4'])
        P.add('act', lambda e: e.activation(out=ee4[:], in_=ee4[:], func=AF.Exp), r=['ee4'], w=['ee4'])
        P.add('dve', lambda e: e.tensor_tensor(out=selb[:, tgs, :], in0=Lm4[:], in1=bc3(top84[:, :, 1], 32), op=ALU.is_ge),
              r=['Lm4'] + t8, w=[('selb', 4 * w + k) for k in range(4)])
        P.add('dve', lambda e: e.tensor_tensor(out=Ff[:, tgs, :], in0=Lm4[:], in1=bc3(top84[:, :, 0], 32), op=ALU.is_ge),
              r=['Lm4'] + t8, w=[('Ff', 4 * w + k) for k in range(4)])
        P.add('dve', lambda e: e.tensor_tensor(out=ee4[:], in0=ee4[:], in1=selb[:, tgs, :], op=ALU.mult),
              r=['ee4'] + [('selb', 4 * w + k) for k in range(4)], w=['ee4'])
        P.add('dve', lambda e: e.tensor_reduce(out=sm4[:, 2, :], in_=ee4[:], axis=AX.X, op=ALU.add), r=['ee4'], w=['ssum'])
        P.add('dve', lambda e: e.tensor_tensor(out=sm4[:, 3, :], in0=sm4[:, 2, :], in1=sm4[:, 1, :], op=ALU.mult),
              r=['ssum', 'sumg'], w=['den'])
        P.add('dve', lambda e: e.reciprocal(out=sm4[:, 4, :], in_=sm4[:, 3, :]), r=['den'], w=['rden'])
        P.add('dve', lambda e: e.tensor_tensor(out=tm4[:], in0=ee4[:], in1=Ff[:, tgs, :], op=ALU.mult),
              r=['ee4'] + [('Ff', 4 * w + k) for k in range(4)], w=['tm4'])
        P.add('dve', lambda e: e.tensor_reduce(out=sm4[:, 5, :], in_=tm4[:], axis=AX.X, op=ALU.add), r=['tm4'], w=['t0'])
        P.add('dve', lambda e: e.tensor_tensor(out=sm4[:, 6, :], in0=sm4[:, 2, :], in1=sm4[:, 5, :], op=ALU.subtract),
              r=['ssum', 't0'], w=['t1'])
        P.add('dve', lambda e: e.tensor_tensor(out=w0[:, tgs], in0=sm4[:, 5, :], in1=sm4[:, 4, :], op=ALU.mult),
              r=['t0', 'rden'], w=[('w0', 4 * w + k) for k in range(4)])
        P.add('dve', lambda e: e.tensor_tensor(out=w1[:, tgs], in0=sm4[:, 6, :], in1=sm4[:, 4, :], op=ALU.mult),
              r=['t1', 'rden'], w=[('w1', 4 * w + k) for k in range(4)])

    for w in range(NW if debug is None else int(os.environ.get('KNW', '2'))):
        phase3(w)
        P.barrier('p3_%d' % w)
        post_window(w)
        P.barrier('pw_%d' % w)
    NTG = 32 if debug is None else 4 * int(os.environ.get('KNW', '2'))

    ar.release(m_big)
    NT = 64
    triS = ar.alloc('triS', [128, 128], BF16)
    thr = ar.alloc('thr', [128, 32, 32], F32)
    jrow = ar.alloc('jrow', [128, NT], F32)
    pcol = ar.alloc('pcol', [128, 1], F32)
    rankf = ar.alloc('rankf', [128, 32, 32], F32)
    cmpb = ar.alloc('cmpb', [128, 32, 32], F32)
    totf = ar.alloc('totf', [128, 32], F32)
    tl = ar.alloc('tl', [128, 32], F32)
    scA = ar.alloc('scA', [128, 32], F32)
    scB = ar.alloc('scB', [128, 32], F32)
    offf = ar.alloc('offf', [128, 32], F32)
    s0f = ar.alloc('s0f', [128, 32], F32)
    s1f = ar.alloc('s1f', [128, 32], F32)
    s0i = ar.alloc('s0i', [128, 32], I32)
    s1i = ar.alloc('s1i', [128, 32], I32)
    eidf = ar.alloc('eidf', [128, NT], F32)
    widx = ar.alloc('widx', [128, NT], I32)
    P.add('sp', lambda e: e.dma_start(out=triS[:], in_=cbf[:, 768 + 2560:768 + 2560 + 128]), w=['triS'], dma=True)
    P.add('sp', lambda e: e.dma_start(out=thr[:], in_=thr_d), w=['thr'], dma=True)
    P.add('sp', lambda e: e.dma_start(out=jrow[:], in_=jrow_d), w=['jrow'], dma=True)
    P.add('sp', lambda e: e.dma_start(out=pcol[:], in_=pcol_d), w=['pcol'], dma=True)
    selres = [('selb', tg) for tg in range(NTG)]
    for tg in range(NTG):
        bank = tg // 16
        col = (tg % 16) * 32
        for t2 in range(tg):
            P.add('pe', lambda e, bank=bank, col=col, t2=t2, tg=tg: e.matmul(
                psum[bank][:, col:col + 32], lhsT=ones_bf[:], rhs=selb[:, t2, :],
                start=(tg % 16 == 0 and t2 == 0), stop=False, skip_group_check=True),
                r=selres + ['ones'], w=[('ps', bank)])
        P.add('pe', lambda e, bank=bank, col=col, tg=tg: e.matmul(
            psum[bank][:, col:col + 32], lhsT=triS[:], rhs=selb[:, tg, :],
            start=(tg == 0 or (tg % 16 == 0 and False)), stop=False, skip_group_check=True),
            r=selres + ['triS'], w=[('ps', bank)])
    for tg in range(NTG):
        P.add('pe', lambda e, tg=tg: e.matmul(psum[2][:, 0:32], lhsT=ones_bf[:], rhs=selb[:, tg, :],
                                              start=(tg == 0), stop=(tg == NTG - 1)),
              r=selres + ['ones'], w=[('ps', 2)])
    nb = (NTG + 15) // 16
    for b in range(nb):
        n16 = min(16, NTG - 16 * b)
        P.add('dve', lambda e, b=b, n16=n16: e.tensor_copy(
            out=rankf[:, 16 * b:16 * b + n16, :], in_=psum[b][:, 0:32 * n16].rearrange('p (t e) -> p t e', e=32)),
            r=[('ps', b)], w=['rankf'])
    P.add('dve', lambda e: e.tensor_copy(out=totf[:], in_=psum[2][:, 0:32]), r=[('ps', 2)], w=['totf'])
    P.add('dve', lambda e: e.tensor_tensor(out=cmpb[:], in0=totf[:].unsqueeze(2).broadcast_to([128, 32, 32]), in1=thr[:],
                                           op=ALU.is_gt), r=['totf', 'thr'], w=['cmpb'])
    P.add('dve', lambda e: e.tensor_reduce(out=tl[:], in_=cmpb[:], axis=AX.X, op=ALU.add), r=['cmpb'], w=['tl'])
    P.add('dve', lambda e: e.tensor_copy(out=scA[:], in_=tl[:]), r=['tl'], w=['scA'])
    cur, nxt, cn, nn = scA, scB, 'scA', 'scB'
    for sft in (1, 2, 4, 8, 16):
        P.add('dve', lambda e, cur=cur, nxt=nxt, sft=sft: e.tensor_copy(out=nxt[:, 0:sft], in_=cur[:, 0:sft]), r=[cn], w=[nn])
        P.add('dve', lambda e, cur=cur, nxt=nxt, sft=sft: e.tensor_tensor(out=nxt[:, sft:32], in0=cur[:, sft:32], in1=cur[:, 0:32 - sft],
                                                                          op=ALU.add), r=[cn], w=[nn])
        cur, nxt, cn, nn = nxt, cur, nn, cn
    endf, endn = cur, cn
    P.add('dve', lambda e: e.tensor_tensor(out=offf[:], in0=endf[:], in1=tl[:], op=ALU.subtract), r=[endn, 'tl'], w=['offf'])
    P.add('dve', lambda e: e.tensor_scalar(out=offf[:], in0=offf[:], scalar1=256.0, scalar2=None, op0=ALU.mult), r=['offf'], w=['offf'])
    P.add('dve', lambda e: e.tensor_tensor(out=rankf[:], in0=rankf[:], in1=offf[:].unsqueeze(1).broadcast_to([128, 32, 32]), op=ALU.add),
          r=['rankf', 'offf'], w=['rankf'])
    P.add('dve', lambda e: e.tensor_tensor(out=cmpb[:], in0=rankf[:], in1=Ff[:], op=ALU.mult),
          r=['rankf'] + [('Ff', tg) for tg in range(NTG)], w=['cmpb'])
    P.add('dve', lambda e: e.tensor_reduce(out=s0f[:], in_=cmpb[:], axis=AX.X, op=ALU.add), r=['cmpb'], w=['s0f'])
    P.add('dve', lambda e: e.tensor_tensor(out=cmpb[:], in0=rankf[:], in1=selb[:], op=ALU.mult), r=['rankf'] + selres, w=['cmpb'])
    P.add('dve', lambda e: e.tensor_reduce(out=s1f[:], in_=cmpb[:], axis=AX.X, op=ALU.add), r=['cmpb'], w=['s1f'])
    P.add('dve', lambda e: e.tensor_tensor(out=s1f[:], in0=s1f[:], in1=s0f[:], op=ALU.subtract), r=['s1f', 's0f'], w=['s1f'])
    P.add('dve', lambda e: e.tensor_copy(out=s0i[:], in_=s0f[:]), r=['s0f'], w=['s0i'])
    P.add('dve', lambda e: e.tensor_copy(out=s1i[:], in_=s1f[:]), r=['s1f'], w=['s1i'])
    P.add('dve', lambda e: e.memset(eidf[:], 0.0), w=['eidf'])
    for ex in range(NEXP):
        P.add('dve', lambda e, ex=ex: e.scalar_tensor_tensor(out=eidf[:], in0=jrow[:], scalar=endf[:, ex:ex + 1], in1=eidf[:],
                                                             op0=ALU.is_ge, op1=ALU.add), r=['jrow', endn, 'eidf'], w=['eidf'])
    P.add('dve', lambda e: e.tensor_scalar(out=eidf[:], in0=eidf[:], scalar1=31.0, scalar2=128.0, op0=ALU.min, op1=ALU.mult),
          r=['eidf'], w=['eidf'])
    P.add('dve', lambda e: e.tensor_scalar(out=eidf[:], in0=eidf[:], scalar1=pcol[:, 0:1], scalar2=None, op0=ALU.add),
          r=['eidf', 'pcol'], w=['eidf'])
    P.add('dve', lambda e: e.tensor_copy(out=widx[:], in_=eidf[:]), r=['eidf'], w=['widx'])

    mR = ar.mark()
    NH2 = 8
    h2l = [ar.alloc('h2l', [128, D], BF16) for i in range(NH2)]
    for tg in range(NTG):
        s3 = tg % NH2
        rows = slice(tg * 128, (tg + 1) * 128)
        P.add('sp', lambda e, s3=s3, rows=rows: e.dma_start(out=h2l[s3][:], in_=H2[rows, :]), r=[('H2', tg)], w=[('h2l', s3)], dma=True)
        for si, nm in ((s0i, 's0i'), (s1i, 's1i')):
            P.add('pool', lambda e, s3=s3, si=si, tg=tg: e.indirect_dma_start(
                out=Xs, out_offset=bass.IndirectOffsetOnAxis(si[:, tg:tg + 1].bitcast(U32), 0), in_=h2l[s3][:], in_offset=None),
                r=[('h2l', s3), nm], w=[('Xs', tg, nm)], dma=True)
    P.add('sp', lambda e: e.nop(), r=[('Xs', tg, nm) for tg in range(NTG) for nm in ('s0i', 's1i')], w=['XsAll'])
    P.barrier('pR')
    ar.release(mR)

    if debug == 'slots':
        dbg = dram('dbg', [128, 4, 32], F32, kind='ExternalOutput')
        P.add('sp', lambda e: e.dma_start(out=dbg[:, 0, :], in_=s0f[:]), r=['s0f'], w=['dbg0'], dma=True)
        P.add('sp', lambda e: e.dma_start(out=dbg[:, 1, :], in_=s1f[:]), r=['s1f'], w=['dbg1'], dma=True)
        P.add('sp', lambda e: e.dma_start(out=dbg[:, 2, :], in_=w0[:]), r=[('w0', tg) for tg in range(NTG)], w=['dbg2'], dma=True)
        P.add('sp', lambda e: e.dma_start(out=dbg[:, 3, :], in_=w1[:]), r=[('w1', tg) for tg in range(NTG)], w=['dbg3'], dma=True)

    NTE = NT if debug is None else int(os.environ.get('KNT', '8'))
    NWB = 4
    wg_s = [ar.alloc('wg_s', [128, NCH * DEXP], BF16) for i in range(NWB)]
    wu_s = [ar.alloc('wu_s', [128, NCH * DEXP], BF16) for i in range(NWB)]
    wd_s = [ar.alloc('wd_s', [128, 2 * D], BF16) for i in range(NWB)]
    xtok = [ar.alloc('xtok', [128, D], BF16) for i in range(4)]
    XT = [ar.alloc('XT', [128, NCH, 128], BF16) for i in range(2)]
    sgb = [ar.alloc('sgb', [128, DEXP], F32) for i in range(2)]
    atok = [ar.alloc('atok', [128, DEXP], BF16) for i in range(2)]
    aT = [ar.alloc('aT', [128, 2, 128], BF16) for i in range(2)]
    ysb = [ar.alloc('ysb', [128, D], F32) for i in range(2)]
    if debug not in ('slots',):
        P.add('pool', lambda e: e.nop(), r=[('wcast', it) for it in range(24)], w=['wcastAll'])
        stA, stB, stC = [], [], []
        for j in range(NTE):
            s = j % NWB
            for sub in range(2):
                u = 2 * j + sub
                s2 = u % 2
                rows = slice(j * 256 + sub * 128, j * 256 + sub * 128 + 128)
                pbank = 0 + s2
                gb_, ub_ = 2 + s2, 4 + s2

                def stageA(j=j, s=s, sub=sub, s2=s2, pbank=pbank, u=u):
                    if sub == 0:
                        for (dst, src, nm) in ((wg_s, wgb, 'wg'), (wu_s, wub, 'wu'), (wd_s, wdb, 'wd')):
                            P.add('pool', lambda e, dst=dst, src=src: e.indirect_dma_start(
                                out=dst[s][:], out_offset=None, in_=src,
                                in_offset=bass.IndirectOffsetOnAxis(widx[:, j:j + 1].bitcast(U32), 0)),
                                r=['widx', 'wcastAll'], w=[(nm, s)], dma=True)
                    x4 = u % 4
                    for uu_ in ([0, 1, 2] if u == 0 else [u + 2]):
                        if uu_ < 2 * NTE:
                            P.add('sp', lambda e, uu_=uu_: e.dma_start(out=xtok[uu_ % 4][:], in_=Xs[uu_ * 128:(uu_ + 1) * 128, :]),
                                  r=['XsAll'], w=[('xtok', uu_ % 4)], dma=True)
                    pb = psum[pbank][:].bitcast(BF16)
                    for c in range(NCH):
                        P.add('pe', lambda e, c=c: e.transpose(out=pb[:, c * 128:(c + 1) * 128],
                                                               in_=xtok[x4][:, c * 128:(c + 1) * 128], identity=ident),
                              r=[('xtok', x4), 'cbf'], w=[('ps', pbank)])
                    P.add('act', lambda e: e.activation(out=XT[s2][:].rearrange('p c t -> p (c t)'), in_=pb, func=AF.Copy),
                          r=[('ps', pbank)], w=[('XT', s2)])

                def stageB(s=s, s2=s2, gb_=gb_, ub_=ub_):
                    for c in range(NCH):
                        P.add('pe', lambda e, c=c: e.matmul(
                            psum[gb_][:, 0:DEXP], lhsT=XT[s2][:, c, :], rhs=wg_s[s][:, c * DEXP:(c + 1) * DEXP],
                            start=(c == 0), stop=(c == NCH - 1)),
                            r=[('XT', s2), ('wg', s)], w=[('ps', gb_)])
                    for c in range(NCH):
                        P.add('pe', lambda e, c=c: e.matmul(
                            psum[ub_][:, 0:DEXP], lhsT=XT[s2][:, c, :], rhs=wu_s[s][:, c * DEXP:(c + 1) * DEXP],
                            start=(c == 0), stop=(c == NCH - 1)),
                            r=[('XT', s2), ('wu', s)], w=[('ps', ub_)])
                    P.add('act', lambda e: e.activation(out=sgb[s2][:], in_=psum[gb_][:, 0:DEXP], func=AF.Silu),
                          r=[('ps', gb_)], w=[('sgb', s2)])
                    P.add('dve', lambda e: e.tensor_tensor(out=atok[s2][:], in0=psum[ub_][:, 0:DEXP], in1=sgb[s2][:], op=ALU.mult),
                          r=[('ps', ub_), ('sgb', s2)], w=[('atok', s2)])
                    pa = psum[gb_][:].bitcast(BF16)
                    for fc in range(2):
                        P.add('pe', lambda e, fc=fc: e.transpose(out=pa[:, fc * 128:(fc + 1) * 128],
                                                                 in_=atok[s2][:, fc * 128:(fc + 1) * 128], identity=ident),
                              r=[('atok', s2), 'cbf'], w=[('ps', gb_)])
                    P.add('dve', lambda e: e.tensor_copy(out=aT[s2][:].rearrange('p c t -> p (c t)'), in_=pa[:, 0:256]),
                          r=[('ps', gb_)], w=[('aT', s2)])

                def stageC(s=s, s2=s2, rows=rows):
                    for half in range(2):
                        yb_ = 6 + half
                        for fc in range(2):
                            P.add('pe', lambda e, fc=fc, half=half, yb_=yb_: e.matmul(
                                psum[yb_][:], lhsT=aT[s2][:, fc, :], rhs=wd_s[s][:, fc * D + half * 512:fc * D + (half + 1) * 512],
                                start=(fc == 0), stop=(fc == 1)),
                                r=[('aT', s2), ('wd', s)], w=[('ps', yb_)])
                    P.add('act', lambda e: e.activation(out=ysb[s2][:, 0:512], in_=psum[6][:], func=AF.Copy),
                          r=[('ps', 6)], w=[('ysb', s2, 0)])
                    P.add('dve', lambda e: e.tensor_copy(out=ysb[s2][:, 512:1024], in_=psum[7][:]),
                          r=[('ps', 7)], w=[('ysb', s2, 1)])
                    P.add('act', lambda e: e.dma_start(out=Ys[rows, :], in_=ysb[s2][:]),
                          r=[('ysb', s2, 0), ('ysb', s2, 1)], w=[('Ys', rows.start)], dma=True)

                stA.append(stageA)
                stB.append(stageB)
                stC.append(stageC)
        NU = len(stA)
        for t in range(NU + 2):
            if t < NU:
                stA[t]()
            if 0 <= t - 1 < NU:
                stB[t - 1]()
            if 0 <= t - 2 < NU:
                stC[t - 2]()
        P.add('pool', lambda e: e.nop(), r=[('Ys', 128 * u) for u in range(2 * NTE)], w=['YsAll'])
        P.barrier('pE')
    ar.release(mR)

    gfr = ar.alloc('gfr', [128, D], F32)
    NFB = 4
    x1l = [ar.alloc('x1l', [128, D], F32) for i in range(NFB)]
    y0l = [ar.alloc('y0l', [128, D], F32) for i in range(NFB)]
    y1l = [ar.alloc('y1l', [128, D], F32) for i in range(NFB)]
    ob = [ar.alloc('ob', [128, D], F32) for i in range(NFB)]
    junk = ar.alloc('junk', [128, D], BF16)
    fs = ar.alloc('fs', [128, 32, 4], F32)
    P.add('sp', lambda e: e.dma_start(out=gfr[:], in_=gfr_d), w=['gfr'], dma=True)
    if debug is None or debug == 'final':
        for tg in range(NTG):
            s = tg % NFB
            rows = slice(tg * 128, (tg + 1) * 128)
            P.add('sp', lambda e, s=s, rows=rows: e.dma_start(out=x1l[s][:], in_=X1[rows, :]), r=[('X1', tg)], w=[('x1l', s)], dma=True)
            P.add('pool', lambda e, s=s, tg=tg: e.indirect_dma_start(
                out=y0l[s][:], out_offset=None, in_=Ys, in_offset=bass.IndirectOffsetOnAxis(s0i[:, tg:tg + 1].bitcast(U32), 0)),
                r=['YsAll', 's0i'], w=[('y0l', s)], dma=True)
            P.add('pool', lambda e, s=s, tg=tg: e.indirect_dma_start(
                out=y1l[s][:], out_offset=None, in_=Ys, in_offset=bass.IndirectOffsetOnAxis(s1i[:, tg:tg + 1].bitcast(U32), 0)),
                r=['YsAll', 's1i'], w=[('y1l', s)], dma=True)
            P.add('dve', lambda e, s=s, tg=tg: e.scalar_tensor_tensor(out=x1l[s][:], in0=y0l[s][:], scalar=w0[:, tg:tg + 1], in1=x1l[s][:],
                                                                      op0=ALU.mult, op1=ALU.add),
                  r=[('y0l', s), ('x1l', s), ('w0', tg)], w=[('x1l', s)])
            P.add('dve', lambda e, s=s, tg=tg: e.scalar_tensor_tensor(out=x1l[s][:], in0=y1l[s][:], scalar=w1[:, tg:tg + 1], in1=x1l[s][:],
                                                                      op0=ALU.mult, op1=ALU.add),
                  r=[('y1l', s), ('x1l', s), ('w1', tg)], w=[('x1l', s)])
            P.add('act', lambda e, s=s, tg=tg: e.activation(out=junk[:], in_=x1l[s][:], func=AF.Square, accum_out=fs[:, tg, 0:1]),
                  r=[('x1l', s)], w=['junk', ('fs', tg, 0)])
            P.add('act', lambda e, tg=tg: e.activation(out=fs[:, tg, 1:2], in_=fs[:, tg, 0:1], func=AF.Sqrt, scale=1.0 / D, bias=EPS),
                  r=[('fs', tg, 0)], w=[('fs', tg, 1)])
            P.add('dve', lambda e, tg=tg: e.reciprocal(out=fs[:, tg, 2:3], in_=fs[:, tg, 1:2]), r=[('fs', tg, 1)], w=[('fs', tg, 2)])
            P.add('dve', lambda e, s=s, tg=tg: e.scalar_tensor_tensor(out=ob[s][:], in0=x1l[s][:], scalar=fs[:, tg, 2:3], in1=gfr[:],
                                                                      op0=ALU.mult, op1=ALU.mult),
                  r=[('x1l', s), ('fs', tg, 2), 'gfr'], w=[('ob', s)])
            P.add('act', lambda e, s=s, rows=rows: e.dma_start(out=outd[rows, :], in_=ob[s][:]), r=[('ob', s)], w=[('out', tg)], dma=True)


    if debug == 'yaT_disabled':
        dbg = dram('dbg', [128, 4, T], BF16, kind='ExternalOutput')
        P.add('sp', lambda e: e.dma_start(out=dbg, in_=yaT[:]), r=[('yaT', p, w) for p in range(4) for w in range(NW)],
              w=['dbg'], dma=True)
    if debug == 'hT_disabled':
        dbg = dram('dbg', [128, NCH, T], BF16, kind='ExternalOutput')
        P.add('sp', lambda e: e.dma_start(out=dbg, in_=hT[:]), r=[('hT', w) for w in range(NW)],
              w=['dbg'], dma=True)

    P.emit(stack)
    stack.close()
    return nc


def rope_tables():
    pos = np.arange(T, dtype=np.float32)
    inv_freq = (np.float32(10000.0) ** (-np.arange(0, HD, 2, dtype=np.float32) / np.float32(HD))).astype(np.float32)
    ang = (pos[:, None] * inv_freq[None, :]).astype(np.float32)
    cos = np.cos(ang).astype(np.float32).T
    sin = np.sin(ang).astype(np.float32).T
    return np.ascontiguousarray(np.tile(cos, (4, 1))), np.ascontiguousarray(np.tile(sin, (4, 1)))


def const_bf16():
    rotT = np.zeros((128, 128), np.float32)
    for m in range(128):
        if m % 64 < 32:
            rotT[m + 32, m] = -1.0
        else:
            rotT[m - 32, m] = 1.0
    kk = np.arange(128)[:, None]
    q = np.arange(128)[None, :]
    cur = (kk <= q).astype(np.float32)
    prevA = (kk >= q).astype(np.float32)
    prevB = (kk >= q + 1).astype(np.float32)
    mA = np.concatenate([prevA, cur, prevA, cur], 1)
    m16 = []
    for v in range(4):
        sl = slice(32 * v, 32 * v + 32)
        m16.append(np.concatenate([prevA[:, sl], cur[:, sl]] * 8, 1))
    mB = np.concatenate([prevB, cur, prevB, cur], 1)
    triS = (np.arange(128)[:, None] < np.arange(128)[None, :]).astype(np.float32)
    allc = np.concatenate([rotT, np.eye(128, dtype=np.float32), mB, mA] + m16 + [triS], 1)
    return allc.astype(ml_dtypes.bfloat16)


def prep_inputs(inp):
    x = np.asarray(inp['x'], dtype=np.float32)
    B = x.shape[0]
    w_in = np.asarray(inp['w_in'], np.float32)[0]
    b_in = np.asarray(inp['b_in'], np.float32)[0]
    gmix = np.ascontiguousarray(np.asarray(inp['g_mix'], np.float32)[0].reshape(NCH, 128).T)
    win = np.empty((NBLK, 128, NCH, 128), np.float32)
    binb = np.empty((128, NBLK), np.float32)
    for i, cols in enumerate(BLOCKS):
        win[i] = w_in[:, cols].reshape(NCH, 128, 128).transpose(1, 0, 2)
        binb[:, i] = b_in[cols]
    bvrep = np.empty((len(VBLKS), 128, 128), np.float32)
    for i, b in enumerate(VBLKS):
        bvrep[i] = np.tile(b_in[BLOCKS[b]][None, :], (128, 1))
    cosT, sinT = rope_tables()
    f32 = lambda k: np.asarray(inp[k], np.float32)
    wpa_ = f32('w_proj_a')[0]
    wpb_ = f32('w_proj_b')[0]
    wo_ = f32('w_out')[0]
    rows_b = np.concatenate([np.concatenate([c * 64 + np.arange(64), (8 + c) * 64 + np.arange(64)]) for c in range(8)])
    blk = lambda m, nc_: np.ascontiguousarray(m.reshape(nc_, 128, 8, 128).transpose(2, 1, 0, 3))
    wpa = blk(wpa_, 4)
    wpb = blk(wpb_[rows_b], 8)
    wo = blk(wo_, 8)
    pc = lambda v: np.ascontiguousarray(v.reshape(NCH, 128).T)
    gffn = pc(f32('g_ffn')[0])
    gfin = pc(f32('g_final'))
    sinkrep = np.ascontiguousarray(np.tile(f32('sinks')[0][None, :], (128, 1)))
    wr_ = np.concatenate([f32('w_router_group')[0], f32('w_router_expert')[0]], 1)
    wr = np.ascontiguousarray(wr_.reshape(NCH, 128, 36).transpose(1, 0, 2))
    brep = np.ascontiguousarray(np.tile(np.concatenate([f32('b_router_group')[0], f32('b_router_expert')[0]])[None, :], (128, 1)))
    weg = np.ascontiguousarray(f32('w_exp_gate')[0].reshape(NEXP, NCH, 128, DEXP).transpose(0, 2, 1, 3))
    weu = np.ascontiguousarray(f32('w_exp_up')[0].reshape(NEXP, NCH, 128, DEXP).transpose(0, 2, 1, 3))
    wed = np.ascontiguousarray(f32('w_exp_down')[0].reshape(NEXP, 2, 128, D).transpose(0, 2, 1, 3))
    shared = dict(gmix=gmix, win=win, binb=binb, bvrep=bvrep, cosT=cosT, sinT=sinT, cbf=const_bf16(),
                  wpa=wpa, wpb=wpb, wo=wo, gffn=gffn, gfin=gfin, sinkrep=sinkrep, wr=wr, brep=brep,
                  weg=weg, weu=weu, wed=wed,
                  identf=np.eye(128, dtype=np.float32),
                  thr=np.ascontiguousarray(np.tile((256.0 * np.arange(32, dtype=np.float32))[None, None, :], (128, 32, 1))),
                  jrow=np.ascontiguousarray(np.tile(np.arange(64, dtype=np.float32)[None, :], (128, 1))),
                  pcol=np.arange(128, dtype=np.float32).reshape(128, 1),
                  gfr=np.ascontiguousarray(np.tile(f32('g_final')[None, :], (128, 1))))
    shared.pop('gfin', None)
    per_core = []
    for b in range(B):
        m = dict(shared)
        m['xT'] = np.ascontiguousarray(x[b].T)
        per_core.append(m)
    return per_core


def kernel(**inputs):
    debug = os.environ.get('KDEBUG')
    nc = build(debug)
    in_maps = prep_inputs(inputs)
    res = run_bass_kernel_spmd(nc, in_maps, core_ids=list(range(8)))
    if debug:
        return [r['dbg'] for r in res.results]
    return np.ascontiguousarray(np.stack([np.asarray(r['out']) for r in res.results], 0)).astype(np.float32)
```

```python
import os
from contextlib import ExitStack
import numpy as np
import ml_dtypes
import concourse.bass as bass
import concourse.mybir as mybir
from concourse.bass_utils import run_bass_kernel_spmd
from concourse.alu_op_type import AluOpType as ALU

F32 = mybir.dt.float32
BF16 = mybir.dt.bfloat16
AF = mybir.ActivationFunctionType
AX = mybir.AxisListType
U32 = mybir.dt.uint32
I32 = mybir.dt.int32

D = 1024
T = 4096
NCH = 8
W = 512
NW = T // W
HD = 64
EPS = 1e-6
DIL = (1, 4, 16)
NEXP = 32
DEXP = 256
A_QKV = 4608
B_QKV = 1280
N_DMA_SEMS = 48
SAME_ENGINE_SYNC = True


class _Rec:
    def __init__(self):
        self.call = None

    def __getattr__(self, name):
        def f(*args, **kwargs):
            assert self.call is None
            self.call = (name, args, kwargs)
            return self
        return f


class Prog:
    def __init__(self, nc):
        self.nc = nc
        self.ops = []

    def add(self, eng, fn, r=(), w=(), dma=False, raw=False):
        if raw:
            fn2 = fn
        else:
            rec = _Rec()
            fn(rec)
            name, args, kwargs = rec.call
            fn2 = lambda e: getattr(e, name)(*args, **kwargs)
        self.ops.append(dict(eng=eng, fn=fn2, r=tuple(r), w=tuple(w), dma=dma))
        return len(self.ops) - 1

    def barrier(self, tag):
        engs = ['pe', 'act', 'dve', 'pool', 'sp']
        allres = set()
        for o in self.ops:
            allres.update(o['r'])
            allres.update(o['w'])
        allres = tuple(allres)
        for e in engs:
            self.add(e, lambda eng: eng.nop(), r=allres, w=[('bar', tag, e)])
        for e in engs:
            self.add(e, lambda eng: eng.nop(), r=[('bar', tag, f) for f in engs], w=allres)

    def emit(self, stack):
        nc = self.nc
        ops = self.ops
        engs = ['pe', 'act', 'dve', 'pool', 'sp']
        last_w = {}
        readers = {}
        deps = []
        for i, op in enumerate(ops):
            d = set()
            for r in op['r']:
                if r in last_w:
                    d.add(last_w[r])
            for w_ in op['w']:
                if w_ in last_w:
                    d.add(last_w[w_])
                d.update(readers.get(w_, ()))
            d.discard(i)
            for r in op['r']:
                readers.setdefault(r, []).append(i)
            for w_ in op['w']:
                last_w[w_] = i
                readers[w_] = []
            deps.append(d)
        dma_sem_of = {}
        dma_val_of = {}
        sem_uses = [0] * N_DMA_SEMS
        sem_last = [None] * N_DMA_SEMS
        k = 0
        for i, op in enumerate(ops):
            if op['dma']:
                s = k % N_DMA_SEMS
                k += 1
                if sem_last[s] is not None:
                    deps[i].add(sem_last[s])
                sem_uses[s] += 1
                dma_sem_of[i] = s
                dma_val_of[i] = 16 * sem_uses[s]
                sem_last[s] = i
        red = []
        for i, op in enumerate(ops):
            best = {}
            dmas = []
            for j in deps[i]:
                oj = ops[j]
                if oj['dma']:
                    dmas.append(j)
                    continue
                if oj['eng'] == op['eng'] and not op['dma']:
                    if oj['eng'] in ('pe', 'sp') or not SAME_ENGINE_SYNC:
                        continue
                if oj['eng'] not in best or j > best[oj['eng']]:
                    best[oj['eng']] = j
            red.append((sorted(best.values()), sorted(dmas)))
        need_inc = [False] * len(ops)
        for i in range(len(ops)):
            for j in red[i][0]:
                need_inc[j] = True
        cnt = {e: 0 for e in engs}
        val_of = {}
        for i, op in enumerate(ops):
            if need_inc[i]:
                cnt[op['eng']] += 1
                val_of[i] = cnt[op['eng']]
        esem = {e: stack.enter_context(nc.semaphore('s_' + e)) for e in engs}
        dsem = [stack.enter_context(nc.semaphore('d%d' % s)) for s in range(N_DMA_SEMS)]
        waited = set()
        for i in range(len(ops)):
            for j in deps[i]:
                waited.add(j)
        tail = [i for i, op in enumerate(ops) if op['dma'] and i not in waited]
        per_eng = {e: [i for i, op in enumerate(ops) if op['eng'] == e] for e in engs}

        def body(ename, eng):
            seen = {}
            for i in per_eng[ename]:
                op = ops[i]
                need = {}
                for j in red[i][0]:
                    key = ('e', ops[j]['eng'])
                    need[key] = max(need.get(key, 0), val_of[j])
                for j in red[i][1]:
                    key = ('d', dma_sem_of[j])
                    need[key] = max(need.get(key, 0), dma_val_of[j])
                for key, val in need.items():
                    if seen.get(key, 0) >= val:
                        continue
                    seen[key] = val
                    sem = esem[key[1]] if key[0] == 'e' else dsem[key[1]]
                    eng.wait_ge(sem, val)
                inst = op['fn'](eng)
                if op['dma']:
                    inst.then_inc(dsem[dma_sem_of[i]], 16)
                elif need_inc[i]:
                    inst.then_inc(esem[ename], 1)
            if ename == 'sp':
                for j in tail:
                    eng.wait_ge(dsem[dma_sem_of[j]], dma_val_of[j])

        block = stack.enter_context(nc.Block())

        @block.tensor
        def _(e):
            body('pe', e)

        @block.scalar
        def _(e):
            body('act', e)

        @block.vector
        def _(e):
            body('dve', e)

        @block.gpsimd
        def _(e):
            body('pool', e)

        @block.sync
        def _(e):
            body('sp', e)


class Arena:
    def __init__(self, nc):
        self.nc = nc
        self.base = (nc.sbuf_base + 31) // 32 * 32
        self.top = nc.sbuf_top
        self.cur = self.base
        self.n = 0

    def alloc(self, name, shape, dt):
        esz = 2 if dt == BF16 else 4
        nbytes = int(np.prod(shape[1:])) * esz
        off = self.cur
        self.cur = (off + nbytes + 31) // 32 * 32
        assert self.cur <= self.top, ('SBUF overflow', name, self.cur, self.top)
        self.n += 1
        return self.nc.alloc_sbuf_tensor_at('%s_%d' % (name, self.n), list(shape), dt, offset=off)

    def mark(self):
        return self.cur

    def release(self, m):
        self.cur = m


def inproj_blocks():
    blocks = []
    idx = {}
    for p in range(4):
        for role, ri in (('q', 0), ('k', 1), ('v', 2)):
            for g in range(3):
                c0 = ((ri * 3 + g) * 8 + 2 * p) * 64
                idx[('A', p, role, g)] = len(blocks)
                blocks.append(np.arange(c0, c0 + 128))
    for gi in range(8):
        idx[('B', 'q', gi)] = len(blocks)
        blocks.append(np.concatenate([A_QKV + gi * 64 + np.arange(64), A_QKV + (8 + gi) * 64 + np.arange(64)]))
    idx[('B', 'k')] = len(blocks)
    blocks.append(A_QKV + 1024 + np.arange(128))
    idx[('B', 'v')] = len(blocks)
    blocks.append(A_QKV + 1024 + 128 + np.arange(128))
    for f in range(16):
        idx[('G', f)] = len(blocks)
        blocks.append(A_QKV + B_QKV + f * 128 + np.arange(128))
    return blocks, idx


BLOCKS, BIDX = inproj_blocks()
NBLK = len(BLOCKS)
VBLKS = [BIDX[('A', p, 'v', g)] for p in range(4) for g in range(3)] + [BIDX[('B', 'v')]]
VIDX = {b: i for i, b in enumerate(VBLKS)}


def perm_block_tokens(d, blk):
    L = T // d
    pos = 128 * blk
    r, j0 = pos // L, pos % L
    return j0 * d + r, d


def build(debug=None, phases='A'):
    nc = bass.Bass('TRN2', target_bir_lowering=False)
    P = Prog(nc)
    stack = ExitStack()
    ar = Arena(nc)

    def dram(name, shape, dt=F32, kind='ExternalInput'):
        return nc.dram_tensor(name, list(shape), dt, kind=kind).ap()

    xT = dram('xT', [D, T])
    gmix = dram('gmix', [128, NCH])
    win = dram('win', [NBLK, 128, NCH, 128])
    binb = dram('binb', [128, NBLK])
    bvrep = dram('bvrep', [len(VBLKS), 128, 128])
    cosd = dram('cosT', [128, T])
    sind = dram('sinT', [128, T])
    cbf = dram('cbf', [128, 256 + 512 * 6 + 128], BF16)
    wpa = dram('wpa', [8, 128, 4, 128])
    wpb = dram('wpb', [8, 128, 8, 128])
    wo = dram('wo', [8, 128, 8, 128])
    gffn = dram('gffn', [128, NCH])
    sinkrep = dram('sinkrep', [128, 16])
    wr = dram('wr', [128, NCH, 36])
    brep = dram('brep', [128, 36])
    weg = dram('weg', [NEXP, 128, NCH, DEXP])
    weu = dram('weu', [NEXP, 128, NCH, DEXP])
    wed = dram('wed', [NEXP, 128, 2, D])
    wegR = weg.rearrange('e p c f -> (e p) (c f)')
    weuR = weu.rearrange('e p c f -> (e p) (c f)')
    wedR = wed.rearrange('e p c d -> (e p) (c d)')
    identf_d = dram('identf', [128, 128])
    thr_d = dram('thr', [128, 32, 32])
    jrow_d = dram('jrow', [128, 64])
    pcol_d = dram('pcol', [128, 1])
    gfr_d = dram('gfr', [128, D])
    outd = dram('out', [T, D], kind='ExternalOutput')
    wgb = dram('wgb', [NEXP * 128, NCH * DEXP], BF16, kind='Internal')
    wub = dram('wub', [NEXP * 128, NCH * DEXP], BF16, kind='Internal')
    wdb = dram('wdb', [NEXP * 128, 2 * D], BF16, kind='Internal')
    B0 = BIDX[('B', 'q', 0)]
    NB3 = NBLK - B0
    winb = dram('winb', [NB3 * 128, NCH * 128], BF16, kind='Internal')
    wpab = dram('wpab', [8 * 128, 4 * 128], BF16, kind='Internal')
    wpbb = dram('wpbb', [8 * 128, NCH * 128], BF16, kind='Internal')
    wob = dram('wob', [8 * 128, NCH * 128], BF16, kind='Internal')
    winR = win.rearrange('b p c n -> (b p) (c n)')
    wpaR = wpa.rearrange('f p c n -> (f p) (c n)')
    wpbR = wpb.rearrange('f p c n -> (f p) (c n)')
    woR = wo.rearrange('f p c n -> (f p) (c n)')
    casts2 = []
    for r0 in range(0, NB3 * 128, 512):
        r1 = min(r0 + 512, NB3 * 128)
        casts2.append((winR[B0 * 128 + r0:B0 * 128 + r1, :], winb[r0:r1, :]))
    for (s_, d_) in ((wpaR, wpab), (wpbR, wpbb), (woR, wob)):
        for r0 in (0, 512):
            casts2.append((s_[r0:r0 + 512, :], d_[r0:r0 + 512, :]))
    X1 = dram('X1s', [T, D], F32, kind='Internal')
    H2 = dram('H2s', [T, D], BF16, kind='Internal')
    Xs = dram('Xss', [64 * 256, D], BF16, kind='Internal')
    Ys = dram('Yss', [64 * 256, D], F32, kind='Internal')
    xTv = xT.rearrange('(c p) t -> p c t', p=128)

    ones_bf = ar.alloc('ones_bf', [128, 128], BF16)
    gmix_sb = ar.alloc('gmix_sb', [128, NCH], F32)
    bin_sb = ar.alloc('bin_sb', [128, NBLK], F32)
    cbf_sb = ar.alloc('cbf_sb', [128, 256 + 512], BF16)
    rotT = cbf_sb[:, 0:128]
    ident = cbf_sb[:, 128:256]
    maskB = cbf_sb[:, 256:768]
    gffn_sb = ar.alloc('gffn_sb', [128, NCH], F32)
    esink = ar.alloc('esink', [128, 16], F32)
    selb = ar.alloc('selb', [128, 32, 32], BF16)
    Ff = ar.alloc('Ff', [128, 32, 32], BF16)
    w0 = ar.alloc('w0', [128, 32], F32)
    w1 = ar.alloc('w1', [128, 32], F32)
    identf = ar.alloc('identf', [128, 128], F32)
    m_big = ar.mark()
    hT = ar.alloc('hT', [128, NCH, T], BF16)
    yaT = ar.alloc('yaT', [128, 4, T], BF16)
    cosw = ar.alloc('cosw', [128, W], F32)
    sinw = ar.alloc('sinw', [128, W], F32)
    zT2 = [ar.alloc('zT', [128, W], BF16) for i in range(2)]
    tt2 = [ar.alloc('tt', [128, W], F32) for i in range(2)]
    uu2 = [ar.alloc('uu', [128, W], F32) for i in range(2)]
    PT = [ar.alloc('PT', [128, 1024], BF16) for i in range(2)]
    bv_sb = ar.alloc('bv_sb', [128, 128], F32)
    esinkT2 = ar.alloc('esinkT2', [128, 2, 512], F32)
    psum = [stack.enter_context(nc.psum_tensor('ps%d' % i, [128, 512], F32)) for i in range(8)]

    P.add('dve', lambda e: e.memset(ones_bf[:], 1.0), w=['ones'])
    P.add('sp', lambda e: e.dma_start(out=gmix_sb[:], in_=gmix), w=['gmix'], dma=True)
    P.add('sp', lambda e: e.dma_start(out=bin_sb[:], in_=binb), w=['bin'], dma=True)
    P.add('sp', lambda e: e.dma_start(out=cbf_sb[:], in_=cbf[:, 0:768]), w=['cbf'], dma=True)
    P.add('sp', lambda e: e.dma_start(out=gffn_sb[:], in_=gffn), w=['gffn'], dma=True)
    P.add('sp', lambda e: e.dma_start(out=esink[:], in_=sinkrep), w=['esink'], dma=True)
    P.add('act', lambda e: e.activation(out=esink[:], in_=esink[:], func=AF.Exp), r=['esink'], w=['esink'])
    for kvh in range(2):
        for half in range(2):
            for j in range(4):
                qh = kvh * 8 + 4 * half + j
                rws = slice(64 * kvh, 64 * kvh + 64)
                P.add('dve', lambda e, rws=rws, half=half, j=j, qh=qh: e.tensor_copy(
                    out=esinkT2[rws, half, j * 128:(j + 1) * 128], in_=esink[rws, qh:qh + 1].broadcast_to([64, 128])),
                    r=['esink'], w=['esinkT2'])

    m1 = ar.mark()
    xw = [ar.alloc('xw', [128, NCH, W], F32) for i in range(3)]
    sq = [ar.alloc('sq', [128, NCH, W], BF16) for i in range(3)]
    rstd = [ar.alloc('rstd', [128, W], F32) for i in range(3)]
    srt = rstd
    for w in range(NW):
        s = w % 3
        ws = slice(w * W, (w + 1) * W)
        P.add('sp', lambda e, s=s, ws=ws: e.dma_start(out=xw[s][:], in_=xTv[:, :, ws]),
              w=[('xw', s)], dma=True)
        P.add('act', lambda e, s=s: e.activation(out=sq[s][:], in_=xw[s][:], func=AF.Square),
              r=[('xw', s)], w=[('sq', s)])
        pb = psum[s]
        for c in range(NCH):
            P.add('pe', lambda e, s=s, c=c, pb=pb: e.matmul(pb[:], lhsT=ones_bf[:], rhs=sq[s][:, c, :],
                                                           start=(c == 0), stop=(c == NCH - 1)),
                  r=[('sq', s), 'ones'], w=[('ps', s)])
        P.add('act', lambda e, s=s, pb=pb: e.activation(out=srt[s][:], in_=pb[:], func=AF.Ln,
                                                        scale=1.0 / D, bias=EPS),
              r=[('ps', s)], w=[('srt', s), ('rstd', s)])
        P.add('act', lambda e, s=s: e.activation(out=rstd[s][:], in_=srt[s][:], func=AF.Exp, scale=-0.5),
              r=[('srt', s)], w=[('rstd', s)])
        for c in range(NCH):
            P.add('dve', lambda e, s=s, c=c, ws=ws: e.scalar_tensor_tensor(
                out=hT[:, c, ws], in0=xw[s][:, c, :], scalar=gmix_sb[:, c:c + 1], in1=rstd[s][:],
                op0=ALU.mult, op1=ALU.mult),
                r=[('xw', s), ('rstd', s), 'gmix'], w=[('hT', w)])
    P.barrier('p1')
    ar.release(m1)

    mA = ar.mark()
    KT = [ar.alloc('KT', [128, T], BF16) for g in range(3)]
    Vst = [ar.alloc('Vst', [128, 32, 128], BF16) for g in range(3)]
    QW = [ar.alloc('QW', [128, W], BF16) for g in range(3)]
    wkv = [ar.alloc('wkv', [128, NCH, 128], BF16) for g in range(3)]
    wq = [ar.alloc('wq', [128, NCH, 128], BF16) for g in range(3)]
    mska = ar.alloc('mska', [128, 512 * 5], BF16)
    maskA = mska[:, 0:512]
    maskA16 = [mska[:, 512 * (v + 1): 512 * (v + 2)] for v in range(4)]
    P.add('sp', lambda e: e.dma_start(out=mska[:], in_=cbf[:, 768:768 + 2560]), w=['cbf'], dma=True)
    Uacc = ar.alloc('Uacc', [128, W], F32)
    Dacc = ar.alloc('Dacc', [128, W], F32)
    rD = ar.alloc('rD', [128, W], F32)

    PS_Z, PS_R, PS_U, PS_D = 0, 1, 6, 7
    PS_S = [(4, 5), (4, 5)]
    unit_ctr = [0]

    def load_w_raw(dst, blk, res):
        load_w(dst, blk, res)

    def load_w(dst, blk, res):
        P.add('pool', lambda e: e.dma_start(out=dst[:].rearrange('p c n -> p (c n)'), in_=win[blk].rearrange('p c n -> p (c n)')), w=[res], dma=True)

    def load_cs(w):
        ws = slice(w * W, (w + 1) * W)
        P.add('sp', lambda e: e.dma_start(out=cosw[:], in_=cosd[:, ws]), w=['cosw'], dma=True)
        P.add('sp', lambda e: e.dma_start(out=sinw[:], in_=sind[:, ws]), w=['sinw'], dma=True)

    proj_ctr = [0]
    proj_pend = {'p': None}

    def proj_flush():
        if proj_pend['p'] is not None:
            proj_pend['p']()
        proj_pend['p'] = None

    def proj_rope(wt, wres, blk, w, dst_ap, dst_res, d):
        ws = slice(w * W, (w + 1) * W)
        zb = proj_ctr[0] % 2
        proj_ctr[0] += 1
        bz, br = 0 + zb, 2 + zb
        pz, pr = psum[bz], psum[br]
        zT, tt, uu = zT2[zb], tt2[zb], uu2[zb]
        for c in range(NCH):
            P.add('pe', lambda e, c=c: e.matmul(pz[:], lhsT=wt[:, c, :], rhs=hT[:, c, ws],
                                                start=(c == 0), stop=(c == NCH - 1)),
                  r=[wres, ('hT', w)], w=[('ps', bz)])
        P.add('act', lambda e: e.activation(out=zT[:], in_=pz[:], func=AF.Identity,
                                            bias=bin_sb[:, blk:blk + 1], scale=1.0),
              r=[('ps', bz), 'bin'], w=[('zT', zb)])

        def part2():
            P.add('pe', lambda e: e.matmul(pr[:], lhsT=rotT, rhs=zT[:], start=True, stop=True),
                  r=[('zT', zb), 'cbf'], w=[('ps', br)])
            P.add('dve', lambda e: e.tensor_tensor(out=uu[:], in0=pr[:], in1=sinw[:], op=ALU.mult),
                  r=[('ps', br), 'sinw'], w=[('uu', zb)])
            P.add('pool', lambda e: e.tensor_tensor(out=tt[:], in0=zT[:], in1=cosw[:], op=ALU.mult),
                  r=[('zT', zb), 'cosw'], w=[('tt', zb)])
            if d == 1:
                a, b = tt[:], uu[:]
            else:
                a = tt[:].rearrange('p (j r) -> p r j', r=d)
                b = uu[:].rearrange('p (j r) -> p r j', r=d)
            P.add('dve', lambda e: e.tensor_tensor(out=dst_ap, in0=a, in1=b, op=ALU.add),
                  r=[('tt', zb), ('uu', zb)], w=[dst_res])

        prev = proj_pend['p']
        proj_pend['p'] = part2
        if prev is not None:
            prev()

    pipe = {'pending': None}

    def pipe_push(front, back):
        front()
        if pipe['pending'] is not None:
            pipe['pending']()
        pipe['pending'] = back

    def pipe_flush():
        if pipe['pending'] is not None:
            pipe['pending']()
        pipe['pending'] = None

    def attn_unit(tiles, qsrc, qres, ksrc, kres_fn, vsrc, vres, mask_ap, hrow, evac, after=None):
        pipe_push(*attn_unit_parts(tiles, qsrc, qres, ksrc, kres_fn, vsrc, vres, mask_ap, hrow, evac, after))

    def attn_unit_parts(tiles, qsrc, qres, ksrc, kres_fn, vsrc, vres, mask_ap, hrow, evac, after):
        u = unit_ctr[0]
        unit_ctr[0] += 1
        sl = u % 2
        sb0, sb1 = PS_S[sl]
        pt = PT[sl]
        rows = slice(hrow, hrow + 64)
        PS_U, PS_D = (6, 7) if u % 2 == 0 else (2, 3)

        def front():
          for i, (qc, nq, kbp, kbc, kcp, kcc, uc) in enumerate(tiles):
            for half, kc in ((0, kcp), (1, kcc)):
                col = i * 2 * nq + half * nq
                bank = sb0 if col < 512 else sb1
                cc = col % 512
                P.add('pe', lambda e, bank=bank, cc=cc, kc=kc, qc=qc, nq=nq: e.matmul(
                    psum[bank][:, cc:cc + nq], lhsT=ksrc[rows, kc:kc + 128], rhs=qsrc[rows, qc:qc + nq],
                    start=True, stop=True),
                    r=list(qres) + kres_fn(kc), w=[('ps', bank)])
          for hb, bank in ((0, sb0), (1, sb1)):
            P.add('act', lambda e, hb=hb, bank=bank: e.activation(
                out=pt[:, hb * 512:(hb + 1) * 512], in_=psum[bank][:], func=AF.Exp, scale=0.125),
                r=[('ps', bank)], w=[('PT', sl, hb)])
            P.add('dve' if hb == 0 else 'pool', lambda e, hb=hb: e.tensor_tensor(
                out=pt[:, hb * 512:(hb + 1) * 512], in0=pt[:, hb * 512:(hb + 1) * 512], in1=mask_ap,
                op=ALU.mult),
                r=[('PT', sl, hb), 'cbf'], w=[('PT', sl, hb)])
        def back():
          first = True
          for i, (qc, nq, kbp, kbc, kcp, kcc, uc) in enumerate(tiles):
            for half, kb in ((0, kbp), (1, kbc)):
                if kb is None:
                    continue
                col = i * 2 * nq + half * nq
                hb = col // 512
                P.add('pe', lambda e, first=first, col=col, kb=kb, uc=uc, nq=nq: e.matmul(
                    psum[PS_U][:, uc:uc + nq], lhsT=vsrc[:, kb, :], rhs=pt[:, col:col + nq],
                    start=first, stop=False, skip_group_check=True),
                    r=[('PT', sl, hb)] + vres(kb), w=[('ps', PS_U)])
                P.add('pe', lambda e, first=first, col=col, kb=kb, uc=uc, nq=nq: e.matmul(
                    psum[PS_D][:, uc:uc + nq], lhsT=ones_bf[:], rhs=pt[:, col:col + nq],
                    start=first, stop=False, skip_group_check=True),
                    r=[('PT', sl, hb), 'ones'], w=[('ps', PS_D)])
                first = False
          evac(rows, PS_U, PS_D)
          if after is not None:
              after()
        return front, back

    for p in range(4):
        for g in range(3):
            load_w(wkv[g], BIDX[('A', p, 'k', g)], ('wkv', g))
        for w in range(NW):
            proj_flush()
            load_cs(w)
            for g in range(3):
                d = DIL[g]
                L = T // d
                dst = KT[g][:].rearrange('p (r j) -> p r j', r=d)[:, :, w * W // d:(w + 1) * W // d] if d > 1 \
                    else KT[g][:, w * W:(w + 1) * W]
                proj_rope(wkv[g], ('wkv', g), BIDX[('A', p, 'k', g)], w, dst, ('KT', g), d)
        proj_flush()
        for g in range(3):
            load_w(wkv[g], BIDX[('A', p, 'v', g)], ('wkv', g))
        for g in range(3):
            d = DIL[g]
            vb = BIDX[('A', p, 'v', g)]
            P.add('sp', lambda e, vb=vb: e.dma_start(out=bv_sb[:], in_=bvrep[VIDX[vb]]), w=['bv'], dma=True)
            for bg in range(8):
                bank = 4 + (bg % 2)
                for i in range(4):
                    blk = bg * 4 + i
                    t0, st = perm_block_tokens(d, blk)
                    for c in range(NCH):
                        P.add('pe', lambda e, c=c, i=i, t0=t0, st=st, g=g, bank=bank: e.matmul(
                            psum[bank][:, i * 128:(i + 1) * 128],
                            lhsT=hT[:, c, t0:t0 + 127 * st + 1:st], rhs=wkv[g][:, c, :],
                            start=(c == 0), stop=(c == NCH - 1)),
                            r=[('wkv', g)] + [('hT', ww) for ww in range(NW)], w=[('ps', bank)])
                P.add('dve', lambda e, g=g, bg=bg, bank=bank: e.tensor_tensor(
                    out=Vst[g][:, bg * 4:(bg + 1) * 4, :], in0=psum[bank][:].rearrange('p (i n) -> p i n', i=4),
                    in1=bv_sb[:].unsqueeze(1).broadcast_to([128, 4, 128]), op=ALU.add),
                    r=[('ps', bank), 'bv'], w=[('Vst', g)])
        for g in range(3):
            load_w(wq[g], BIDX[('A', p, 'q', g)], ('wq', g))
        for w in range(NW):
            it = p * NW + w
            if it < 7:
                for k2 in (2 * it, 2 * it + 1):
                    if k2 < len(casts2):
                        s_, d_ = casts2[k2]
                        P.add('pool', lambda e, s_=s_, d_=d_: e.dma_start(out=d_, in_=s_), w=[('wcast2', k2)], dma=True)
            elif it - 7 < 24:
                ee = it - 7
                src_, dst_ = ((wegR, wgb), (weuR, wub), (wedR, wdb))[ee % 3]
                rws = slice((ee // 3) * 512, (ee // 3 + 1) * 512)
                P.add('pool', lambda e, src_=src_, dst_=dst_, rws=rws: e.dma_start(out=dst_[rws, :], in_=src_[rws, :]),
                      w=[('wcast', ee)], dma=True)
            load_cs(w)
            for g in range(3):
                d = DIL[g]
                dst = QW[g][:].rearrange('p (r j) -> p r j', r=d) if d > 1 else QW[g][:]
                proj_rope(wq[g], ('wq', g), BIDX[('A', p, 'q', g)], w, dst, ('QW', g), d)
            proj_flush()
            for hh in range(2):
                for g in range(3):
                    d = DIL[g]
                    L = T // d
                    tiles = []
                    if d == 1:
                        for i in range(4):
                            qb = 4 * w + i
                            kbp = qb - 1 if qb >= 1 else None
                            tiles.append((128 * i, 128, kbp, qb, 128 * max(qb - 1, 0), 128 * qb, 128 * i))
                        mask_ap = maskA
                    elif d == 4:
                        for r in range(4):
                            qb = w
                            base = r * (L // 128)
                            kbp = base + qb - 1 if qb >= 1 else None
                            tiles.append((128 * r, 128, kbp, base + qb, 128 * (base + max(qb - 1, 0)), 128 * (base + qb), 128 * r))
                        mask_ap = maskA
                    else:
                        for r in range(16):
                            qb = w // 4
                            base = r * (L // 128)
                            kbp = base + qb - 1 if qb >= 1 else None
                            tiles.append((32 * r, 32, kbp, base + qb, 128 * (base + max(qb - 1, 0)), 128 * (base + qb), 32 * r))
                        mask_ap = maskA16[w % 4]

                    def evac(rows, PS_U, PS_D, g=g, d=d):
                        if g == 0:
                            P.add('act', lambda e: e.activation(out=Uacc[rows, :], in_=psum[PS_U][rows, :], func=AF.Copy),
                                  r=[('ps', PS_U)], w=['Uacc'])
                            P.add('act', lambda e: e.activation(out=Dacc[rows, :], in_=psum[PS_D][rows, :], func=AF.Copy),
                                  r=[('ps', PS_D)], w=['Dacc'])
                        else:
                            for acc, bank, nm in ((Uacc, PS_U, 'Uacc'), (Dacc, PS_D, 'Dacc')):
                                av = acc[rows, :].rearrange('p (j r) -> p r j', r=d)
                                pv = psum[bank][rows, :].rearrange('p (r j) -> p r j', r=d)
                                P.add('dve', lambda e, av=av, pv=pv: e.tensor_tensor(out=av, in0=av, in1=pv, op=ALU.add),
                                      r=[('ps', bank), nm], w=[nm])

                    def fin(p=p, w=w):
                        ws = slice(w * W, (w + 1) * W)
                        P.add('act', lambda e: e.activation(out=rD[:], in_=Dacc[:], func=AF.Ln), r=['Dacc'], w=['rD'])
                        P.add('act', lambda e: e.activation(out=rD[:], in_=rD[:], func=AF.Exp, scale=-1.0), r=['rD'], w=['rD'])
                        P.add('dve', lambda e: e.tensor_tensor(out=yaT[:, p, ws], in0=Uacc[:], in1=rD[:], op=ALU.mult),
                              r=['Uacc', 'rD'], w=[('yaT', p, w)])

                    attn_unit(tiles, QW[g], [('QW', g)], KT[g], lambda kc, g=g: [('KT', g)], Vst[g], lambda kb, g=g: [('Vst', g)],
                              mask_ap, 64 * hh, evac, after=(fin if (hh == 1 and g == 2) else None))
        pipe_flush()
    P.barrier('pA')
    ar.release(mA)

    m3 = ar.mark()
    acc = ar.alloc('acc', [128, NCH, W], F32)
    P.add('sp', lambda e: e.dma_start(out=identf[:], in_=identf_d), w=['identf'], dma=True)
    P.add('sp', lambda e: e.nop(), r=[('wcast2', k2) for k2 in range(len(casts2))], w=['wcast2All'])
    KBq = ar.alloc('KBq', [128, 128 + 2 * W], BF16)
    VBq = ar.alloc('VBq', [128, 9, 128], BF16)
    NWSL = 4
    wsl = [ar.alloc('wsl', [128, NCH, 128], BF16) for i in range(NWSL)]

    def attn_unit_B(QB, i, kvh, lb, has_prev, evac):
        u = unit_ctr[0]
        unit_ctr[0] += 1
        sl = u % 2
        sb0, sb1 = PS_S[sl]
        pt = PT[sl]
        rows = slice(64 * kvh, 64 * kvh + 64)
        PS_U, PS_D = (6, 7) if u % 2 == 0 else (2, 3)
        kcp, kcc = 128 * (lb - 1 if has_prev else lb), 128 * lb
        qv = QB[rows, :].rearrange('p (j q) -> p j q', j=4)[:, :, 128 * i:128 * (i + 1)]
        kres = [('KBq', 'prev'), ('KBq', 0), ('KBq', 1)]
        vres = [('VBq', 'prev'), ('VBq', 0), ('VBq', 1)]

        def front():
            for bank, kc in ((sb0, kcp), (sb1, kcc)):
                P.add('pe', lambda e, bank=bank, kc=kc: e.matmul(
                    psum[bank][:].rearrange('p (j q) -> p j q', j=4), lhsT=KBq[rows, kc:kc + 128], rhs=qv,
                    start=True, stop=True),
                    r=[('QB', j) for j in range(4)] + kres, w=[('ps', bank)])
            for hb, bank in ((0, sb0), (1, sb1)):
                P.add('act', lambda e, hb=hb, bank=bank: e.activation(
                    out=pt[:, hb * 512:(hb + 1) * 512], in_=psum[bank][:], func=AF.Exp, scale=0.125),
                    r=[('ps', bank)], w=[('PT', sl, hb)])
                P.add('dve' if hb == 0 else 'pool', lambda e, hb=hb: e.tensor_tensor(
                    out=pt[:, hb * 512:(hb + 1) * 512].rearrange('p (j q) -> p j q', j=4),
                    in0=pt[:, hb * 512:(hb + 1) * 512].rearrange('p (j q) -> p j q', j=4),
                    in1=maskB[:, hb * 128:(hb + 1) * 128].unsqueeze(1).broadcast_to([128, 4, 128]), op=ALU.mult),
                    r=[('PT', sl, hb), 'cbf'], w=[('PT', sl, hb)])

        def back():
            first = True
            for hb, kb in ((0, (lb - 1) if has_prev else None), (1, lb)):
                if kb is None:
                    continue
                P.add('pe', lambda e, first=first, hb=hb, kb=kb: e.matmul(
                    psum[PS_U][:], lhsT=VBq[:, kb, :], rhs=pt[:, hb * 512:(hb + 1) * 512],
                    start=first, stop=False, skip_group_check=True),
                    r=[('PT', sl, hb)] + vres, w=[('ps', PS_U)])
                P.add('pe', lambda e, first=first, hb=hb: e.matmul(
                    psum[PS_D][:], lhsT=ones_bf[:], rhs=pt[:, hb * 512:(hb + 1) * 512],
                    start=first, stop=False, skip_group_check=True),
                    r=[('PT', sl, hb), 'ones'], w=[('ps', PS_D)])
                first = False
            evac(rows, PS_U, PS_D)
        return front, back

    wstate = {'issued': 0, 'used': 0}
    msub = ar.mark()

    def phase3(w):
        lw = w % 2
        ws = slice(w * W, (w + 1) * W)
        lws = slice(0, W)
        ar.release(msub)
        QB = ar.alloc('QB', [128, 4 * W], BF16)
        ybW = ar.alloc('ybW', [128, NCH, W], BF16)
        wa_s = [ar.alloc('wa_s', [128, 4, 128], BF16) for i in range(3)]
        wb_s = [ar.alloc('wb_s', [128, NCH, 128], BF16) for i in range(3)]
        wga_s = [ar.alloc('wga_s', [128, NCH, 128], BF16) for i in range(3)]
        wgb_s = [ar.alloc('wgb_s', [128, NCH, 128], BF16) for i in range(3)]
        wo_s = [ar.alloc('wo_s', [128, NCH, 128], BF16) for i in range(2)]
        ga = ar.alloc('ga', [128, W], F32)
        gb = ar.alloc('gb', [128, W], F32)
        mergedT = ar.alloc('mergedT', [128, NCH, W], BF16)
        xres = [ar.alloc('xres', [128, W], F32) for i in range(2)]
        Dt2 = [ga, gb]
        dctr = [0]
        blk_seq = [BIDX[('B', 'k')], BIDX[('B', 'v')]] + [BIDX[('B', 'q', gi)] for gi in range(8)]
        NBW = len(blk_seq)

        def load_w_raw(dst, blk, res):
            r0 = (blk - B0) * 128
            P.add('sp', lambda e: e.dma_start(out=dst[:].rearrange('p c n -> p (c n)'), in_=winb[r0:r0 + 128, :]),
                  r=['wcast2All'], w=[res], dma=True)

        def prefetch_to(n):
            while wstate['issued'] < min(n, NBW * NW):
                b = wstate['issued']
                load_w_raw(wsl[b % NWSL], blk_seq[b % NBW], ('wsl', b % NWSL))
                wstate['issued'] += 1

        def next_wsl():
            b = wstate['used']
            prefetch_to(b + NWSL)
            wstate['used'] += 1
            return wsl[b % NWSL], ('wsl', b % NWSL)

        def load_merge(f):
            s3 = f % 3
            P.add('sp', lambda e: e.dma_start(out=wa_s[s3][:].rearrange('p c n -> p (c n)'), in_=wpab[f * 128:(f + 1) * 128, :]),
                  r=['wcast2All'], w=[('wa', s3)], dma=True)
            P.add('sp', lambda e: e.dma_start(out=wb_s[s3][:].rearrange('p c n -> p (c n)'), in_=wpbb[f * 128:(f + 1) * 128, :]),
                  r=['wcast2All'], w=[('wb', s3)], dma=True)
            load_w_raw(wga_s[s3], BIDX[('G', f)], ('wga', s3))
            load_w_raw(wgb_s[s3], BIDX[('G', 8 + f)], ('wgb', s3))

        wo3 = [wo_s[0][:].rearrange('p c n -> p (c n)'), wo_s[1][:].rearrange('p c n -> p (c n)'), QB[:, 0:NCH * 128]]
        wo3res = [[('wo', 0)], [('wo', 1)], [('QB', 0), ('QB', 1)]]

        def load_wo(f2):
            P.add('sp', lambda e: e.dma_start(out=wo3[f2 % 3], in_=wob[f2 * 128:(f2 + 1) * 128, :]),
                  r=['wcast2All'], w=wo3res[f2 % 3], dma=True)

        load_merge(0)
        load_cs(w)
        wt, wres = next_wsl()
        proj_rope(wt, wres, BIDX[('B', 'k')], w, KBq[:, 128 + lw * W:128 + (lw + 1) * W],
                  ('KBq', lw), 1)
        proj_flush()
        wt, wres = next_wsl()
        vb = BIDX[('B', 'v')]
        P.add('sp', lambda e: e.dma_start(out=bv_sb[:], in_=bvrep[VIDX[vb]]), w=['bv'], dma=True)
        bank = PS_S[0][0]
        for i in range(4):
            t0 = w * W + 128 * i
            for c in range(NCH):
                P.add('pe', lambda e, c=c, i=i, t0=t0, wt=wt: e.matmul(
                    psum[bank][:, i * 128:(i + 1) * 128], lhsT=hT[:, c, t0:t0 + 128], rhs=wt[:, c, :],
                    start=(c == 0), stop=(c == NCH - 1)),
                    r=[wres, ('hT', w)], w=[('ps', bank)])
        P.add('dve', lambda e: e.tensor_tensor(
            out=VBq[:, 1 + 4 * lw:5 + 4 * lw, :], in0=psum[bank][:].rearrange('p (i n) -> p i n', i=4),
            in1=bv_sb[:].unsqueeze(1).broadcast_to([128, 4, 128]), op=ALU.add),
            r=[('ps', bank), 'bv'], w=[('VBq', lw)])
        for half in range(2):
            for j in range(4):
                gi = 4 * half + j
                wt, wres = next_wsl()
                proj_rope(wt, wres, BIDX[('B', 'q', gi)], w, QB[:, j * W:(j + 1) * W], ('QB', j), 1)
            proj_flush()
            load_merge(1 + half)
            for i in range(4):
                lb = 1 + 4 * lw + i
                has_prev = not (w == 0 and i == 0)
                for kvh in range(2):
                    tiles = []
                    for j in range(4):
                        tiles.append((j * W + 128 * i, 128, (lb - 1) if has_prev else None, lb,
                                      128 * (lb - 1 if has_prev else lb), 128 * lb, 128 * j))

                    def evac(rows, PS_U, PS_D, kvh=kvh, i=i, half=half):
                        di = dctr[0] % 2
                        dctr[0] += 1
                        Dt = Dt2[di]
                        dn = 'ga' if di == 0 else 'gb'
                        P.add('dve', lambda e: e.tensor_tensor(out=Dt[rows, :], in0=psum[PS_D][rows, :], in1=esinkT2[rows, half, :],
                                                               op=ALU.add),
                              r=[('ps', PS_D), 'esinkT2'], w=[dn])
                        P.add('act', lambda e: e.activation(out=Dt[rows, :], in_=Dt[rows, :], func=AF.Ln), r=[dn], w=[dn])
                        P.add('act', lambda e: e.activation(out=Dt[rows, :], in_=Dt[rows, :], func=AF.Exp, scale=-1.0), r=[dn], w=[dn])
                        P.add('dve', lambda e: e.tensor_tensor(
                            out=ybW[rows, 4 * half:4 * half + 4, 128 * i:128 * (i + 1)],
                            in0=psum[PS_U][rows, :].rearrange('p (j q) -> p j q', j=4),
                            in1=Dt[rows, :].rearrange('p (j q) -> p j q', j=4), op=ALU.mult),
                            r=[('ps', PS_U), dn], w=['ybW'])

                    def kres(kc, lw=lw):
                        return [('KBq', 'prev'), ('KBq', 0), ('KBq', 1)]

                    def vres(kb, lw=lw):
                        return [('VBq', 'prev'), ('VBq', 0), ('VBq', 1)]

                    pipe_push(*attn_unit_B(QB, i, kvh, lb, has_prev, evac))
        pipe_flush()
        if lw == 1:
            P.add('dve', lambda e: e.tensor_copy(out=KBq[:, 0:128], in_=KBq[:, 2 * W:2 * W + 128]),
                  r=[('KBq', 1)], w=[('KBq', 'prev')])
            P.add('dve', lambda e: e.tensor_copy(out=VBq[:, 0, :], in_=VBq[:, 8, :]),
                  r=[('VBq', 1)], w=[('VBq', 'prev')])
        for f in range(8):
            s = f % 2
            bA, bB, bGA, bGB = (2, 3, 4, 5) if s == 0 else (6, 7, 0, 1)
            s = f % 3
            if f == 0:
                load_wo(0)
                load_wo(1)
                load_wo(2)
            for c in range(4):
                P.add('pe', lambda e, c=c, s=s: e.matmul(psum[bA][:], lhsT=wa_s[s][:, c, :], rhs=yaT[:, c, ws],
                                                         start=(c == 0), stop=(c == 3)),
                      r=[('wa', s)] + [('yaT', c, w)], w=[('ps', bA)])
            for c in range(NCH):
                P.add('pe', lambda e, c=c, s=s: e.matmul(psum[bB][:], lhsT=wb_s[s][:, c, :], rhs=ybW[:, c, :],
                                                         start=(c == 0), stop=(c == NCH - 1)),
                      r=[('wb', s), 'ybW'], w=[('ps', bB)])
            for c in range(NCH):
                P.add('pe', lambda e, c=c, s=s: e.matmul(psum[bGA][:], lhsT=wga_s[s][:, c, :], rhs=hT[:, c, ws],
                                                         start=(c == 0), stop=(c == NCH - 1)),
                      r=[('wga', s), ('hT', w)], w=[('ps', bGA)])
            for c in range(NCH):
                P.add('pe', lambda e, c=c, s=s: e.matmul(psum[bGB][:], lhsT=wgb_s[s][:, c, :], rhs=hT[:, c, ws],
                                                         start=(c == 0), stop=(c == NCH - 1)),
                      r=[('wgb', s), ('hT', w)], w=[('ps', bGB)])
            if f + 3 < 8:
                load_merge(f + 3)
            ba, bb = BIDX[('G', f)], BIDX[('G', 8 + f)]
            P.add('act', lambda e, ba=ba: e.activation(out=ga[:], in_=psum[bGA][:], func=AF.Sigmoid,
                                                       bias=bin_sb[:, ba:ba + 1], scale=1.0),
                  r=[('ps', bGA), 'bin'], w=['ga'])
            P.add('act', lambda e, bb=bb: e.activation(out=gb[:], in_=psum[bGB][:], func=AF.Sigmoid,
                                                       bias=bin_sb[:, bb:bb + 1], scale=1.0),
                  r=[('ps', bGB), 'bin'], w=['gb'])
            P.add('dve', lambda e: e.tensor_tensor(out=ga[:], in0=psum[bA][:], in1=ga[:], op=ALU.mult),
                  r=[('ps', bA), 'ga'], w=['ga'])
            P.add('dve', lambda e: e.tensor_tensor(out=gb[:], in0=psum[bB][:], in1=gb[:], op=ALU.mult),
                  r=[('ps', bB), 'gb'], w=['gb'])
            P.add('dve', lambda e, f=f: e.tensor_tensor(out=mergedT[:, f, :], in0=ga[:], in1=gb[:], op=ALU.add),
                  r=['ga', 'gb'], w=['mergedT'])
        for f2 in range(8):
            s = f2 % 2
            P.add('sp', lambda e, f2=f2, s=s: e.dma_start(out=xres[s][:], in_=xTv[:, f2, ws]), w=[('xres', s)], dma=True)
            for c in range(NCH):
                P.add('pe', lambda e, c=c, s=s, f2=f2: e.matmul(psum[s][:], lhsT=wo3[f2 % 3][:, c * 128:(c + 1) * 128], rhs=mergedT[:, c, :],
                                                         start=(c == 0), stop=(c == NCH - 1)),
                      r=wo3res[f2 % 3] + ['mergedT'], w=[('ps', s)])
            P.add('dve', lambda e, f2=f2, s=s: e.tensor_tensor(out=acc[:, f2, lws], in0=psum[s][:], in1=xres[s][:],
                                                               op=ALU.add),
                  r=[('ps', s), ('xres', s)], w=['acc'])
            if f2 + 3 < 8:
                load_wo(f2 + 3)

    def norm_stats(sqb, srtb, rstdb):
        P.add('act', lambda e: e.activation(out=sqb[:], in_=acc[:], func=AF.Square), r=['acc'], w=['sqb'])
        for c in range(NCH):
            P.add('pe', lambda e, c=c: e.matmul(psum[7][:], lhsT=ones_bf[:], rhs=sqb[:, c, :],
                                                start=(c == 0), stop=(c == NCH - 1)),
                  r=['sqb', 'ones'], w=[('ps', 7)])
        P.add('act', lambda e: e.activation(out=srtb[:], in_=psum[7][:], func=AF.Ln, scale=1.0 / D, bias=EPS),
              r=[('ps', 7)], w=['srtb'])
        P.add('act', lambda e: e.activation(out=rstdb[:], in_=srtb[:], func=AF.Exp, scale=-0.5), r=['srtb'], w=['rstdb'])

    def post_window(w):
        ar.release(msub)
        sqb = ar.alloc('sqb', [128, NCH, W], BF16)
        srtb = ar.alloc('srtb', [128, W], F32)
        rstdb = ar.alloc('rstdb', [128, W], F32)
        h2f = ar.alloc('h2f', [128, NCH, W], F32)
        h2Tw = ar.alloc('h2Tw', [128, NCH, W], BF16)
        wr_sb = ar.alloc('wr_sb', [128, NCH, 36], F32)
        brep_sb = ar.alloc('brep_sb', [128, 36], F32)
        L4 = ar.alloc('L4', [128, 4, 36], F32)
        Lg = ar.alloc('Lg', [128, 4, 4], F32)
        oh4 = ar.alloc('oh4', [128, 4, 4], F32)
        Lm4 = ar.alloc('Lm4', [128, 4, 32], F32)
        ee4 = ar.alloc('ee4', [128, 4, 32], F32)
        tm4 = ar.alloc('tm4', [128, 4, 32], F32)
        top84 = ar.alloc('top84', [128, 4, 8], F32)
        sm4 = ar.alloc('sm4', [128, 12, 4], F32)
        h2tok = [ar.alloc('h2tok', [128, D], BF16) for i in range(2)]
        x1tok = [ar.alloc('x1tok', [128, D], F32) for i in range(2)]
        P.add('sp', lambda e: e.dma_start(out=wr_sb[:], in_=wr), w=['wr'], dma=True)
        P.add('sp', lambda e: e.dma_start(out=brep_sb[:], in_=brep), w=['brep'], dma=True)
        BIG = 30000.0
        def S1(t4):
            ts_ = slice(t4 * 128, (t4 + 1) * 128)
            P.add('act', lambda e: e.activation(out=sqb[:, :, ts_], in_=acc[:, :, ts_], func=AF.Square),
                  r=['acc'], w=[('sqb', t4)])
            for c in range(NCH):
                P.add('pe', lambda e, c=c: e.matmul(psum[7][:, ts_], lhsT=ones_bf[:], rhs=sqb[:, c, ts_],
                                                    start=(c == 0), stop=(c == NCH - 1)),
                      r=[('sqb', t4), 'ones'], w=[('ps7', t4)])
            P.add('act', lambda e: e.activation(out=srtb[:, ts_], in_=psum[7][:, ts_], func=AF.Ln, scale=1.0 / D, bias=EPS),
                  r=[('ps7', t4)], w=[('srtb', t4)])
            P.add('act', lambda e: e.activation(out=rstdb[:, ts_], in_=srtb[:, ts_], func=AF.Exp, scale=-0.5),
                  r=[('srtb', t4)], w=[('rstdb', t4)])

        def S2(t4):
            ts_ = slice(t4 * 128, (t4 + 1) * 128)
            for c in range(NCH):
                P.add('dve', lambda e, c=c: e.scalar_tensor_tensor(
                    out=h2f[:, c, ts_], in0=acc[:, c, ts_], scalar=gffn_sb[:, c:c + 1], in1=rstdb[:, ts_],
                    op0=ALU.mult, op1=ALU.mult),
                    r=['acc', ('rstdb', t4), 'gffn'], w=[('h2f', t4)])
            P.add('act', lambda e: e.activation(out=h2Tw[:, :, ts_], in_=h2f[:, :, ts_], func=AF.Copy),
                  r=[('h2f', t4)], w=[('h2Tw', t4)])

        def S3a(t4):
            tg = 4 * w + t4
            s2 = t4 % 2
            ts_ = slice(t4 * 128, (t4 + 1) * 128)
            rows = slice(tg * 128, (tg + 1) * 128)
            b0 = 2 + 2 * s2
            for c in range(NCH):
                bank = b0 + c // 4
                P.add('pe', lambda e, c=c, bank=bank: e.transpose(
                    out=psum[bank][:, (c % 4) * 128:(c % 4 + 1) * 128], in_=acc[:, c, ts_], identity=identf[:]),
                    r=['acc', 'identf'], w=[('ps', bank)])
            P.add('dve', lambda e: e.tensor_copy(out=x1tok[s2][:, 0:512], in_=psum[b0][:]),
                  r=[('ps', b0)], w=[('x1tok', s2, 0)])
            P.add('dve', lambda e: e.tensor_copy(out=x1tok[s2][:, 512:1024], in_=psum[b0 + 1][:]),
                  r=[('ps', b0 + 1)], w=[('x1tok', s2, 1)])
            P.add('sp', lambda e: e.dma_start(out=X1[rows, :], in_=x1tok[s2][:]),
                  r=[('x1tok', s2, 0), ('x1tok', s2, 1)], w=[('X1', tg)], dma=True)

        def S3b(t4):
            tg = 4 * w + t4
            s2 = t4 % 2
            ts_ = slice(t4 * 128, (t4 + 1) * 128)
            rows = slice(tg * 128, (tg + 1) * 128)
            pb = psum[s2][:].bitcast(BF16)
            for c in range(NCH):
                P.add('pe', lambda e, c=c: e.transpose(out=pb[:, c * 128:(c + 1) * 128], in_=h2Tw[:, c, ts_], identity=ident),
                      r=[('h2Tw', t4), 'cbf'], w=[('ps', s2)])
            P.add('act', lambda e: e.activation(out=h2tok[s2][:], in_=pb, func=AF.Copy), r=[('ps', s2)], w=[('h2tok', s2)])
            P.add('sp', lambda e: e.dma_start(out=H2[rows, :], in_=h2tok[s2][:]), r=[('h2tok', s2)], w=[('H2', tg)], dma=True)
            pl = psum[6]
            for c in range(NCH):
                P.add('pe', lambda e, c=c: e.matmul(pl[:, t4 * 36:(t4 + 1) * 36], lhsT=h2f[:, c, ts_], rhs=wr_sb[:, c, :],
                                                    start=(c == 0), stop=(c == NCH - 1), skip_group_check=True),
                      r=[('h2f', t4), 'wr'], w=[('ps', 6)])

        for st in (lambda: S1(0), lambda: S3a(0), lambda: S1(1), lambda: S2(0), lambda: S3a(1), lambda: S1(2), lambda: S2(1),
                   lambda: S3b(0), lambda: S3a(2), lambda: S1(3), lambda: S2(2), lambda: S3b(1), lambda: S3a(3), lambda: S2(3),
                   lambda: S3b(2), lambda: S3b(3)):
            st()
        BIG = 30000.0
        tgs = slice(4 * w, 4 * w + 4)
        bc3 = lambda ap, n: ap.unsqueeze(2).broadcast_to([128, 4, n])
        P.add('dve', lambda e: e.tensor_tensor(out=L4[:], in0=psum[6][:, 0:144].rearrange('p (t n) -> p t n', t=4),
                                               in1=brep_sb[:].unsqueeze(1).broadcast_to([128, 4, 36]), op=ALU.add),
              r=[('ps', 6), 'brep'], w=['L4'])
        P.add('dve', lambda e: e.tensor_reduce(out=sm4[:, 0, :], in_=L4[:, :, 0:4], axis=AX.X, op=ALU.max), r=['L4'], w=['gmax'])
        P.add('dve', lambda e: e.tensor_tensor(out=Lg[:], in0=L4[:, :, 0:4], in1=bc3(sm4[:, 0, :], 4), op=ALU.subtract),
              r=['L4', 'gmax'], w=['Lg'])
        P.add('dve', lambda e: e.tensor_tensor(out=oh4[:], in0=L4[:, :, 0:4], in1=bc3(sm4[:, 0, :], 4), op=ALU.is_ge),
              r=['L4', 'gmax'], w=['oh4'])
        P.add('act', lambda e: e.activation(out=Lg[:], in_=Lg[:], func=AF.Exp), r=['Lg'], w=['Lg'])
        P.add('dve', lambda e: e.tensor_reduce(out=sm4[:, 1, :], in_=Lg[:], axis=AX.X, op=ALU.add), r=['Lg'], w=['sumg'])
        P.add('dve', lambda e: e.tensor_scalar(out=oh4[:], in0=oh4[:], scalar1=BIG, scalar2=-BIG, op0=ALU.mult, op1=ALU.add),
              r=['oh4'], w=['oh4'])
        P.add('dve', lambda e: e.tensor_tensor(
            out=Lm4[:].rearrange('p t (g x) -> p t g x', g=4), in0=L4[:, :, 4:36].rearrange('p t (g x) -> p t g x', g=4),
            in1=oh4[:].unsqueeze(3).broadcast_to([128, 4, 4, 8]), op=ALU.add), r=['L4', 'oh4'], w=['Lm4'])
        for t4 in range(4):
            P.add('dve', lambda e, t4=t4: e.max(out=top84[:, t4, :], in_=Lm4[:, t4, :]), r=['Lm4'], w=[('top84', t4)])
        t8 = [('top84', t4) for t4 in range(4)]
        P.add('dve', lambda e: e.tensor_tensor(out=ee4[:], in0=Lm4[:], in1=bc3(top84[:, :, 0], 32), op=ALU.subtract),
              r=['Lm4'] + t8, w=['ee4'])
        P.add('act', lambda e: e.activation(out=ee4[:], in_=ee4[:], func=AF.Exp), r=['ee4'], w=['ee4'])
        P.add('dve', lambda e: e.tensor_tensor(out=selb[:, tgs, :], in0=Lm4[:], in1=bc3(top84[:, :, 1], 32), op=ALU.is_ge),
              r=['Lm4'] + t8, w=[('selb', 4 * w + k) for k in range(4)])
        P.add('dve', lambda e: e.tensor_tensor(out=Ff[:, tgs, :], in0=Lm4[:], in1=bc3(top84[:, :, 0], 32), op=ALU.is_ge),
              r=['Lm4'] + t8, w=[('Ff', 4 * w + k) for k in range(4)])
        P.add('dve', lambda e: e.tensor_tensor(out=ee4[:], in0=ee4[:], in1=selb[:, tgs, :], op=ALU.mult),
              r=['ee4'] + [('selb', 4 * w + k) for k in range(4)], w=['ee4'])
        P.add('dve', lambda e: e.tensor_reduce(out=sm4[:, 2, :], in_=ee4[:], axis=AX.X, op=ALU.add), r=['ee4'], w=['ssum'])
        P.add('dve', lambda e: e.tensor_tensor(out=sm4[:, 3, :], in0=sm4[:, 2, :], in1=sm4[:, 1, :], op=ALU.mult),
              r=['ssum', 'sumg'], w=['den'])
        P.add('dve', lambda e: e.reciprocal(out=sm4[:, 4, :], in_=sm4[:, 3, :]), r=['den'], w=['rden'])
        P.add('dve', lambda e: e.tensor_tensor(out=tm4[:], in0=ee4[:], in1=Ff[:, tgs, :], op=ALU.mult),
              r=['ee4'] + [('Ff', 4 * w + k) for k in range(4)], w=['tm4'])
        P.add('dve', lambda e: e.tensor_reduce(out=sm4[:, 5, :], in_=tm4[:], axis=AX.X, op=ALU.add), r=['tm4'], w=['t0'])
        P.add('dve', lambda e: e.tensor_tensor(out=sm4[:, 6, :], in0=sm4[:, 2, :], in1=sm4[:, 5, :], op=ALU.subtract),
              r=['ssum', 't0'], w=['t1'])
        P.add('dve', lambda e: e.tensor_tensor(out=w0[:, tgs], in0=sm4[:, 5, :], in1=sm4[:, 4, :], op=ALU.mult),
              r=['t0', 'rden'], w=[('w0', 4 * w + k) for k in range(4)])
        P.add('dve', lambda e: e.tensor_tensor(out=w1[:, tgs], in0=sm4[:, 6, :], in1=sm4[:, 4, :], op=ALU.mult),
              r=['t1', 'rden'], w=[('w1', 4 * w + k) for k in range(4)])

    for w in range(NW if debug is None else int(os.environ.get('KNW', '2'))):
        phase3(w)
        P.barrier('p3_%d' % w)
        post_window(w)
        P.barrier('pw_%d' % w)
    NTG = 32 if debug is None else 4 * int(os.environ.get('KNW', '2'))

    ar.release(m_big)
    NT = 64
    triS = ar.alloc('triS', [128, 128], BF16)
    thr = ar.alloc('thr', [128, 32, 32], F32)
    jrow = ar.alloc('jrow', [128, NT], F32)
    pcol = ar.alloc('pcol', [128, 1], F32)
    rankf = ar.alloc('rankf', [128, 32, 32], F32)
    cmpb = ar.alloc('cmpb', [128, 32, 32], F32)
    totf = ar.alloc('totf', [128, 32], F32)
    tl = ar.alloc('tl', [128, 32], F32)
    scA = ar.alloc('scA', [128, 32], F32)
    scB = ar.alloc('scB', [128, 32], F32)
    offf = ar.alloc('offf', [128, 32], F32)
    s0f = ar.alloc('s0f', [128, 32], F32)
    s1f = ar.alloc('s1f', [128, 32], F32)
    s0i = ar.alloc('s0i', [128, 32], I32)
    s1i = ar.alloc('s1i', [128, 32], I32)
    eidf = ar.alloc('eidf', [128, NT], F32)
    widx = ar.alloc('widx', [128, NT], I32)
    P.add('sp', lambda e: e.dma_start(out=triS[:], in_=cbf[:, 768 + 2560:768 + 2560 + 128]), w=['triS'], dma=True)
    P.add('sp', lambda e: e.dma_start(out=thr[:], in_=thr_d), w=['thr'], dma=True)
    P.add('sp', lambda e: e.dma_start(out=jrow[:], in_=jrow_d), w=['jrow'], dma=True)
    P.add('sp', lambda e: e.dma_start(out=pcol[:], in_=pcol_d), w=['pcol'], dma=True)
    selres = [('selb', tg) for tg in range(NTG)]
    for tg in range(NTG):
        bank = tg // 16
        col = (tg % 16) * 32
        for t2 in range(tg):
            P.add('pe', lambda e, bank=bank, col=col, t2=t2, tg=tg: e.matmul(
                psum[bank][:, col:col + 32], lhsT=ones_bf[:], rhs=selb[:, t2, :],
                start=(tg % 16 == 0 and t2 == 0), stop=False, skip_group_check=True),
                r=selres + ['ones'], w=[('ps', bank)])
        P.add('pe', lambda e, bank=bank, col=col, tg=tg: e.matmul(
            psum[bank][:, col:col + 32], lhsT=triS[:], rhs=selb[:, tg, :],
            start=(tg == 0 or (tg % 16 == 0 and False)), stop=False, skip_group_check=True),
            r=selres + ['triS'], w=[('ps', bank)])
    for tg in range(NTG):
        P.add('pe', lambda e, tg=tg: e.matmul(psum[2][:, 0:32], lhsT=ones_bf[:], rhs=selb[:, tg, :],
                                              start=(tg == 0), stop=(tg == NTG - 1)),
              r=selres + ['ones'], w=[('ps', 2)])
    nb = (NTG + 15) // 16
    for b in range(nb):
        n16 = min(16, NTG - 16 * b)
        P.add('dve', lambda e, b=b, n16=n16: e.tensor_copy(
            out=rankf[:, 16 * b:16 * b + n16, :], in_=psum[b][:, 0:32 * n16].rearrange('p (t e) -> p t e', e=32)),
            r=[('ps', b)], w=['rankf'])
    P.add('dve', lambda e: e.tensor_copy(out=totf[:], in_=psum[2][:, 0:32]), r=[('ps', 2)], w=['totf'])
    P.add('dve', lambda e: e.tensor_tensor(out=cmpb[:], in0=totf[:].unsqueeze(2).broadcast_to([128, 32, 32]), in1=thr[:],
                                           op=ALU.is_gt), r=['totf', 'thr'], w=['cmpb'])
    P.add('dve', lambda e: e.tensor_reduce(out=tl[:], in_=cmpb[:], axis=AX.X, op=ALU.add), r=['cmpb'], w=['tl'])
    P.add('dve', lambda e: e.tensor_copy(out=scA[:], in_=tl[:]), r=['tl'], w=['scA'])
    cur, nxt, cn, nn = scA, scB, 'scA', 'scB'
    for sft in (1, 2, 4, 8, 16):
        P.add('dve', lambda e, cur=cur, nxt=nxt, sft=sft: e.tensor_copy(out=nxt[:, 0:sft], in_=cur[:, 0:sft]), r=[cn], w=[nn])
        P.add('dve', lambda e, cur=cur, nxt=nxt, sft=sft: e.tensor_tensor(out=nxt[:, sft:32], in0=cur[:, sft:32], in1=cur[:, 0:32 - sft],
                                                                          op=ALU.add), r=[cn], w=[nn])
        cur, nxt, cn, nn = nxt, cur, nn, cn
    endf, endn = cur, cn
    P.add('dve', lambda e: e.tensor_tensor(out=offf[:], in0=endf[:], in1=tl[:], op=ALU.subtract), r=[endn, 'tl'], w=['offf'])
    P.add('dve', lambda e: e.tensor_scalar(out=offf[:], in0=offf[:], scalar1=256.0, scalar2=None, op0=ALU.mult), r=['offf'], w=['offf'])
    P.add('dve', lambda e: e.tensor_tensor(out=rankf[:], in0=rankf[:], in1=offf[:].unsqueeze(1).broadcast_to([128, 32, 32]), op=ALU.add),
          r=['rankf', 'offf'], w=['rankf'])
    P.add('dve', lambda e: e.tensor_tensor(out=cmpb[:], in0=rankf[:], in1=Ff[:], op=ALU.mult),
          r=['rankf'] + [('Ff', tg) for tg in range(NTG)], w=['cmpb'])
    P.add('dve', lambda e: e.tensor_reduce(out=s0f[:], in_=cmpb[:], axis=AX.X, op=ALU.add), r=['cmpb'], w=['s0f'])
    P.add('dve', lambda e: e.tensor_tensor(out=cmpb[:], in0=rankf[:], in1=selb[:], op=ALU.mult), r=['rankf'] + selres, w=['cmpb'])
    P.add('dve', lambda e: e.tensor_reduce(out=s1f[:], in_=cmpb[:], axis=AX.X, op=ALU.add), r=['cmpb'], w=['s1f'])
    P.add('dve', lambda e: e.tensor_tensor(out=s1f[:], in0=s1f[:], in1=s0f[:], op=ALU.subtract), r=['s1f', 's0f'], w=['s1f'])
    P.add('dve', lambda e: e.tensor_copy(out=s0i[:], in_=s0f[:]), r=['s0f'], w=['s0i'])
    P.add('dve', lambda e: e.tensor_copy(out=s1i[:], in_=s1f[:]), r=['s1f'], w=['s1i'])
    P.add('dve', lambda e: e.memset(eidf[:], 0.0), w=['eidf'])
    for ex in range(NEXP):
        P.add('dve', lambda e, ex=ex: e.scalar_tensor_tensor(out=eidf[:], in0=jrow[:], scalar=endf[:, ex:ex + 1], in1=eidf[:],
                                                             op0=ALU.is_ge, op1=ALU.add), r=['jrow', endn, 'eidf'], w=['eidf'])
    P.add('dve', lambda e: e.tensor_scalar(out=eidf[:], in0=eidf[:], scalar1=31.0, scalar2=128.0, op0=ALU.min, op1=ALU.mult),
          r=['eidf'], w=['eidf'])
    P.add('dve', lambda e: e.tensor_scalar(out=eidf[:], in0=eidf[:], scalar1=pcol[:, 0:1], scalar2=None, op0=ALU.add),
          r=['eidf', 'pcol'], w=['eidf'])
    P.add('dve', lambda e: e.tensor_copy(out=widx[:], in_=eidf[:]), r=['eidf'], w=['widx'])

    mR = ar.mark()
    NH2 = 8
    h2l = [ar.alloc('h2l', [128, D], BF16) for i in range(NH2)]
    for tg in range(NTG):
        s3 = tg % NH2
        rows = slice(tg * 128, (tg + 1) * 128)
        P.add('sp', lambda e, s3=s3, rows=rows: e.dma_start(out=h2l[s3][:], in_=H2[rows, :]), r=[('H2', tg)], w=[('h2l', s3)], dma=True)
        for si, nm in ((s0i, 's0i'), (s1i, 's1i')):
            P.add('pool', lambda e, s3=s3, si=si, tg=tg: e.indirect_dma_start(
                out=Xs, out_offset=bass.IndirectOffsetOnAxis(si[:, tg:tg + 1].bitcast(U32), 0), in_=h2l[s3][:], in_offset=None),
                r=[('h2l', s3), nm], w=[('Xs', tg, nm)], dma=True)
    P.add('sp', lambda e: e.nop(), r=[('Xs', tg, nm) for tg in range(NTG) for nm in ('s0i', 's1i')], w=['XsAll'])
    P.barrier('pR')
    ar.release(mR)

    if debug == 'slots':
        dbg = dram('dbg', [128, 4, 32], F32, kind='ExternalOutput')
        P.add('sp', lambda e: e.dma_start(out=dbg[:, 0, :], in_=s0f[:]), r=['s0f'], w=['dbg0'], dma=True)
        P.add('sp', lambda e: e.dma_start(out=dbg[:, 1, :], in_=s1f[:]), r=['s1f'], w=['dbg1'], dma=True)
        P.add('sp', lambda e: e.dma_start(out=dbg[:, 2, :], in_=w0[:]), r=[('w0', tg) for tg in range(NTG)], w=['dbg2'], dma=True)
        P.add('sp', lambda e: e.dma_start(out=dbg[:, 3, :], in_=w1[:]), r=[('w1', tg) for tg in range(NTG)], w=['dbg3'], dma=True)

    NTE = NT if debug is None else int(os.environ.get('KNT', '8'))
    NWB = 4
    wg_s = [ar.alloc('wg_s', [128, NCH * DEXP], BF16) for i in range(NWB)]
    wu_s = [ar.alloc('wu_s', [128, NCH * DEXP], BF16) for i in range(NWB)]
    wd_s = [ar.alloc('wd_s', [128, 2 * D], BF16) for i in range(NWB)]
    xtok = [ar.alloc('xtok', [128, D], BF16) for i in range(4)]
    XT = [ar.alloc('XT', [128, NCH, 128], BF16) for i in range(2)]
    sgb = [ar.alloc('sgb', [128, DEXP], F32) for i in range(2)]
    atok = [ar.alloc('atok', [128, DEXP], BF16) for i in range(2)]
    aT = [ar.alloc('aT', [128, 2, 128], BF16) for i in range(2)]
    ysb = [ar.alloc('ysb', [128, D], F32) for i in range(2)]
    if debug not in ('slots',):
        P.add('pool', lambda e: e.nop(), r=[('wcast', it) for it in range(24)], w=['wcastAll'])
        stA, stB, stC = [], [], []
        for j in range(NTE):
            s = j % NWB
            for sub in range(2):
                u = 2 * j + sub
                s2 = u % 2
                rows = slice(j * 256 + sub * 128, j * 256 + sub * 128 + 128)
                pbank = 0 + s2
                gb_, ub_ = 2 + s2, 4 + s2

                def stageA(j=j, s=s, sub=sub, s2=s2, pbank=pbank, u=u):
                    if sub == 0:
                        for (dst, src, nm) in ((wg_s, wgb, 'wg'), (wu_s, wub, 'wu'), (wd_s, wdb, 'wd')):
                            P.add('pool', lambda e, dst=dst, src=src: e.indirect_dma_start(
                                out=dst[s][:], out_offset=None, in_=src,
                                in_offset=bass.IndirectOffsetOnAxis(widx[:, j:j + 1].bitcast(U32), 0)),
                                r=['widx', 'wcastAll'], w=[(nm, s)], dma=True)
                    x4 = u % 4
                    for uu_ in ([0, 1, 2] if u == 0 else [u + 2]):
                        if uu_ < 2 * NTE:
                            P.add('sp', lambda e, uu_=uu_: e.dma_start(out=xtok[uu_ % 4][:], in_=Xs[uu_ * 128:(uu_ + 1) * 128, :]),
                                  r=['XsAll'], w=[('xtok', uu_ % 4)], dma=True)
                    pb = psum[pbank][:].bitcast(BF16)
                    for c in range(NCH):
                        P.add('pe', lambda e, c=c: e.transpose(out=pb[:, c * 128:(c + 1) * 128],
                                                               in_=xtok[x4][:, c * 128:(c + 1) * 128], identity=ident),
                              r=[('xtok', x4), 'cbf'], w=[('ps', pbank)])
                    P.add('act', lambda e: e.activation(out=XT[s2][:].rearrange('p c t -> p (c t)'), in_=pb, func=AF.Copy),
                          r=[('ps', pbank)], w=[('XT', s2)])

                def stageB(s=s, s2=s2, gb_=gb_, ub_=ub_):
                    for c in range(NCH):
                        P.add('pe', lambda e, c=c: e.matmul(
                            psum[gb_][:, 0:DEXP], lhsT=XT[s2][:, c, :], rhs=wg_s[s][:, c * DEXP:(c + 1) * DEXP],
                            start=(c == 0), stop=(c == NCH - 1)),
                            r=[('XT', s2), ('wg', s)], w=[('ps', gb_)])
                    for c in range(NCH):
                        P.add('pe', lambda e, c=c: e.matmul(
                            psum[ub_][:, 0:DEXP], lhsT=XT[s2][:, c, :], rhs=wu_s[s][:, c * DEXP:(c + 1) * DEXP],
                            start=(c == 0), stop=(c == NCH - 1)),
                            r=[('XT', s2), ('wu', s)], w=[('ps', ub_)])
                    P.add('act', lambda e: e.activation(out=sgb[s2][:], in_=psum[gb_][:, 0:DEXP], func=AF.Silu),
                          r=[('ps', gb_)], w=[('sgb', s2)])
                    P.add('dve', lambda e: e.tensor_tensor(out=atok[s2][:], in0=psum[ub_][:, 0:DEXP], in1=sgb[s2][:], op=ALU.mult),
                          r=[('ps', ub_), ('sgb', s2)], w=[('atok', s2)])
                    pa = psum[gb_][:].bitcast(BF16)
                    for fc in range(2):
                        P.add('pe', lambda e, fc=fc: e.transpose(out=pa[:, fc * 128:(fc + 1) * 128],
                                                                 in_=atok[s2][:, fc * 128:(fc + 1) * 128], identity=ident),
                              r=[('atok', s2), 'cbf'], w=[('ps', gb_)])
                    P.add('dve', lambda e: e.tensor_copy(out=aT[s2][:].rearrange('p c t -> p (c t)'), in_=pa[:, 0:256]),
                          r=[('ps', gb_)], w=[('aT', s2)])

                def stageC(s=s, s2=s2, rows=rows):
                    for half in range(2):
                        yb_ = 6 + half
                        for fc in range(2):
                            P.add('pe', lambda e, fc=fc, half=half, yb_=yb_: e.matmul(
                                psum[yb_][:], lhsT=aT[s2][:, fc, :], rhs=wd_s[s][:, fc * D + half * 512:fc * D + (half + 1) * 512],
                                start=(fc == 0), stop=(fc == 1)),
                                r=[('aT', s2), ('wd', s)], w=[('ps', yb_)])
                    P.add('act', lambda e: e.activation(out=ysb[s2][:, 0:512], in_=psum[6][:], func=AF.Copy),
                          r=[('ps', 6)], w=[('ysb', s2, 0)])
                    P.add('dve', lambda e: e.tensor_copy(out=ysb[s2][:, 512:1024], in_=psum[7][:]),
                          r=[('ps', 7)], w=[('ysb', s2, 1)])
                    P.add('act', lambda e: e.dma_start(out=Ys[rows, :], in_=ysb[s2][:]),
                          r=[('ysb', s2, 0), ('ysb', s2, 1)], w=[('Ys', rows.start)], dma=True)

                stA.append(stageA)
                stB.append(stageB)
                stC.append(stageC)
        NU = len(stA)
        for t in range(NU + 2):
            if t < NU:
                stA[t]()
            if 0 <= t - 1 < NU:
                stB[t - 1]()
            if 0 <= t - 2 < NU:
                stC[t - 2]()
        P.add('pool', lambda e: e.nop(), r=[('Ys', 128 * u) for u in range(2 * NTE)], w=['YsAll'])
        P.barrier('pE')
    ar.release(mR)

    gfr = ar.alloc('gfr', [128, D], F32)
    NFB = 4
    x1l = [ar.alloc('x1l', [128, D], F32) for i in range(NFB)]
    y0l = [ar.alloc('y0l', [128, D], F32) for i in range(NFB)]
    y1l = [ar.alloc('y1l', [128, D], F32) for i in range(NFB)]
    ob = [ar.alloc('ob', [128, D], F32) for i in range(NFB)]
    junk = ar.alloc('junk', [128, D], BF16)
    fs = ar.alloc('fs', [128, 32, 4], F32)
    P.add('sp', lambda e: e.dma_start(out=gfr[:], in_=gfr_d), w=['gfr'], dma=True)
    if debug is None or debug == 'final':
        for tg in range(NTG):
            s = tg % NFB
            rows = slice(tg * 128, (tg + 1) * 128)
            P.add('sp', lambda e, s=s, rows=rows: e.dma_start(out=x1l[s][:], in_=X1[rows, :]), r=[('X1', tg)], w=[('x1l', s)], dma=True)
            P.add('pool', lambda e, s=s, tg=tg: e.indirect_dma_start(
                out=y0l[s][:], out_offset=None, in_=Ys, in_offset=bass.IndirectOffsetOnAxis(s0i[:, tg:tg + 1].bitcast(U32), 0)),
                r=['YsAll', 's0i'], w=[('y0l', s)], dma=True)
            P.add('pool', lambda e, s=s, tg=tg: e.indirect_dma_start(
                out=y1l[s][:], out_offset=None, in_=Ys, in_offset=bass.IndirectOffsetOnAxis(s1i[:, tg:tg + 1].bitcast(U32), 0)),
                r=['YsAll', 's1i'], w=[('y1l', s)], dma=True)
            P.add('dve', lambda e, s=s, tg=tg: e.scalar_tensor_tensor(out=x1l[s][:], in0=y0l[s][:], scalar=w0[:, tg:tg + 1], in1=x1l[s][:],
                                                                      op0=ALU.mult, op1=ALU.add),
                  r=[('y0l', s), ('x1l', s), ('w0', tg)], w=[('x1l', s)])
            P.add('dve', lambda e, s=s, tg=tg: e.scalar_tensor_tensor(out=x1l[s][:], in0=y1l[s][:], scalar=w1[:, tg:tg + 1], in1=x1l[s][:],
                                                                      op0=ALU.mult, op1=ALU.add),
                  r=[('y1l', s), ('x1l', s), ('w1', tg)], w=[('x1l', s)])
            P.add('act', lambda e, s=s, tg=tg: e.activation(out=junk[:], in_=x1l[s][:], func=AF.Square, accum_out=fs[:, tg, 0:1]),
                  r=[('x1l', s)], w=['junk', ('fs', tg, 0)])
            P.add('act', lambda e, tg=tg: e.activation(out=fs[:, tg, 1:2], in_=fs[:, tg, 0:1], func=AF.Sqrt, scale=1.0 / D, bias=EPS),
                  r=[('fs', tg, 0)], w=[('fs', tg, 1)])
            P.add('dve', lambda e, tg=tg: e.reciprocal(out=fs[:, tg, 2:3], in_=fs[:, tg, 1:2]), r=[('fs', tg, 1)], w=[('fs', tg, 2)])
            P.add('dve', lambda e, s=s, tg=tg: e.scalar_tensor_tensor(out=ob[s][:], in0=x1l[s][:], scalar=fs[:, tg, 2:3], in1=gfr[:],
                                                                      op0=ALU.mult, op1=ALU.mult),
                  r=[('x1l', s), ('fs', tg, 2), 'gfr'], w=[('ob', s)])
            P.add('act', lambda e, s=s, rows=rows: e.dma_start(out=outd[rows, :], in_=ob[s][:]), r=[('ob', s)], w=[('out', tg)], dma=True)


    if debug == 'yaT_disabled':
        dbg = dram('dbg', [128, 4, T], BF16, kind='ExternalOutput')
        P.add('sp', lambda e: e.dma_start(out=dbg, in_=yaT[:]), r=[('yaT', p, w) for p in range(4) for w in range(NW)],
              w=['dbg'], dma=True)
    if debug == 'hT_disabled':
        dbg = dram('dbg', [128, NCH, T], BF16, kind='ExternalOutput')
        P.add('sp', lambda e: e.dma_start(out=dbg, in_=hT[:]), r=[('hT', w) for w in range(NW)],
              w=['dbg'], dma=True)

    P.emit(stack)
    stack.close()
    return nc


def rope_tables():
    pos = np.arange(T, dtype=np.float32)
    inv_freq = (np.float32(10000.0) ** (-np.arange(0, HD, 2, dtype=np.float32) / np.float32(HD))).astype(np.float32)
    ang = (pos[:, None] * inv_freq[None, :]).astype(np.float32)
    cos = np.cos(ang).astype(np.float32).T
    sin = np.sin(ang).astype(np.float32).T
    return np.ascontiguousarray(np.tile(cos, (4, 1))), np.ascontiguousarray(np.tile(sin, (4, 1)))


def const_bf16():
    rotT = np.zeros((128, 128), np.float32)
    for m in range(128):
        if m % 64 < 32:
            rotT[m + 32, m] = -1.0
        else:
            rotT[m - 32, m] = 1.0
    kk = np.arange(128)[:, None]
    q = np.arange(128)[None, :]
    cur = (kk <= q).astype(np.float32)
    prevA = (kk >= q).astype(np.float32)
    prevB = (kk >= q + 1).astype(np.float32)
    mA = np.concatenate([prevA, cur, prevA, cur], 1)
    m16 = []
    for v in range(4):
        sl = slice(32 * v, 32 * v + 32)
        m16.append(np.concatenate([prevA[:, sl], cur[:, sl]] * 8, 1))
    mB = np.concatenate([prevB, cur, prevB, cur], 1)
    triS = (np.arange(128)[:, None] < np.arange(128)[None, :]).astype(np.float32)
    allc = np.concatenate([rotT, np.eye(128, dtype=np.float32), mB, mA] + m16 + [triS], 1)
    return allc.astype(ml_dtypes.bfloat16)


def prep_inputs(inp):
    x = np.asarray(inp['x'], dtype=np.float32)
    B = x.shape[0]
    w_in = np.asarray(inp['w_in'], np.float32)[0]
    b_in = np.asarray(inp['b_in'], np.float32)[0]
    gmix = np.ascontiguousarray(np.asarray(inp['g_mix'], np.float32)[0].reshape(NCH, 128).T)
    win = np.empty((NBLK, 128, NCH, 128), np.float32)
    binb = np.empty((128, NBLK), np.float32)
    for i, cols in enumerate(BLOCKS):
        win[i] = w_in[:, cols].reshape(NCH, 128, 128).transpose(1, 0, 2)
        binb[:, i] = b_in[cols]
    bvrep = np.empty((len(VBLKS), 128, 128), np.float32)
    for i, b in enumerate(VBLKS):
        bvrep[i] = np.tile(b_in[BLOCKS[b]][None, :], (128, 1))
    cosT, sinT = rope_tables()
    f32 = lambda k: np.asarray(inp[k], np.float32)
    wpa_ = f32('w_proj_a')[0]
    wpb_ = f32('w_proj_b')[0]
    wo_ = f32('w_out')[0]
    rows_b = np.concatenate([np.concatenate([c * 64 + np.arange(64), (8 + c) * 64 + np.arange(64)]) for c in range(8)])
    blk = lambda m, nc_: np.ascontiguousarray(m.reshape(nc_, 128, 8, 128).transpose(2, 1, 0, 3))
    wpa = blk(wpa_, 4)
    wpb = blk(wpb_[rows_b], 8)
    wo = blk(wo_, 8)
    pc = lambda v: np.ascontiguousarray(v.reshape(NCH, 128).T)
    gffn = pc(f32('g_ffn')[0])
    gfin = pc(f32('g_final'))
    sinkrep = np.ascontiguousarray(np.tile(f32('sinks')[0][None, :], (128, 1)))
    wr_ = np.concatenate([f32('w_router_group')[0], f32('w_router_expert')[0]], 1)
    wr = np.ascontiguousarray(wr_.reshape(NCH, 128, 36).transpose(1, 0, 2))
    brep = np.ascontiguousarray(np.tile(np.concatenate([f32('b_router_group')[0], f32('b_router_expert')[0]])[None, :], (128, 1)))
    weg = np.ascontiguousarray(f32('w_exp_gate')[0].reshape(NEXP, NCH, 128, DEXP).transpose(0, 2, 1, 3))
    weu = np.ascontiguousarray(f32('w_exp_up')[0].reshape(NEXP, NCH, 128, DEXP).transpose(0, 2, 1, 3))
    wed = np.ascontiguousarray(f32('w_exp_down')[0].reshape(NEXP, 2, 128, D).transpose(0, 2, 1, 3))
    shared = dict(gmix=gmix, win=win, binb=binb, bvrep=bvrep, cosT=cosT, sinT=sinT, cbf=const_bf16(),
                  wpa=wpa, wpb=wpb, wo=wo, gffn=gffn, gfin=gfin, sinkrep=sinkrep, wr=wr, brep=brep,
                  weg=weg, weu=weu, wed=wed,
                  identf=np.eye(128, dtype=np.float32),
                  thr=np.ascontiguousarray(np.tile((256.0 * np.arange(32, dtype=np.float32))[None, None, :], (128, 32, 1))),
                  jrow=np.ascontiguousarray(np.tile(np.arange(64, dtype=np.float32)[None, :], (128, 1))),
                  pcol=np.arange(128, dtype=np.float32).reshape(128, 1),
                  gfr=np.ascontiguousarray(np.tile(f32('g_final')[None, :], (128, 1))))
    shared.pop('gfin', None)
    per_core = []
    for b in range(B):
        m = dict(shared)
        m['xT'] = np.ascontiguousarray(x[b].T)
        per_core.append(m)
    return per_core


def kernel(**inputs):
    debug = os.environ.get('KDEBUG')
    nc = build(debug)
    in_maps = prep_inputs(inputs)
    res = run_bass_kernel_spmd(nc, in_maps, core_ids=list(range(8)))
    if debug:
        return [r['dbg'] for r in res.results]
    return np.ascontiguousarray(np.stack([np.asarray(r['out']) for r in res.results], 0)).astype(np.float32)
```

```python
import os
from contextlib import ExitStack
import numpy as np
import ml_dtypes
import concourse.bass as bass
import concourse.mybir as mybir
from concourse.bass_utils import run_bass_kernel_spmd
from concourse.alu_op_type import AluOpType as ALU

F32 = mybir.dt.float32
BF16 = mybir.dt.bfloat16
AF = mybir.ActivationFunctionType
AX = mybir.AxisListType
U32 = mybir.dt.uint32
I32 = mybir.dt.int32

D = 1024
T = 4096
NCH = 8
W = 512
NW = T // W
HD = 64
EPS = 1e-6
DIL = (1, 4, 16)
NEXP = 32
DEXP = 256
A_QKV = 4608
B_QKV = 1280
N_DMA_SEMS = 48
SAME_ENGINE_SYNC = True


class _Rec:
    def __init__(self):
        self.call = None

    def __getattr__(self, name):
        def f(*args, **kwargs):
            assert self.call is None
            self.call = (name, args, kwargs)
            return self
        return f


class Prog:
    def __init__(self, nc):
        self.nc = nc
        self.ops = []

    def add(self, eng, fn, r=(), w=(), dma=False, raw=False):
        if raw:
            fn2 = fn
        else:
            rec = _Rec()
            fn(rec)
            name, args, kwargs = rec.call
            fn2 = lambda e: getattr(e, name)(*args, **kwargs)
        self.ops.append(dict(eng=eng, fn=fn2, r=tuple(r), w=tuple(w), dma=dma))
        return len(self.ops) - 1

    def barrier(self, tag):
        engs = ['pe', 'act', 'dve', 'pool', 'sp']
        allres = set()
        for o in self.ops:
            allres.update(o['r'])
            allres.update(o['w'])
        allres = tuple(allres)
        for e in engs:
            self.add(e, lambda eng: eng.nop(), r=allres, w=[('bar', tag, e)])
        for e in engs:
            self.add(e, lambda eng: eng.nop(), r=[('bar', tag, f) for f in engs], w=allres)

    def emit(self, stack):
        nc = self.nc
        ops = self.ops
        engs = ['pe', 'act', 'dve', 'pool', 'sp']
        last_w = {}
        readers = {}
        deps = []
        for i, op in enumerate(ops):
            d = set()
            for r in op['r']:
                if r in last_w:
                    d.add(last_w[r])
            for w_ in op['w']:
                if w_ in last_w:
                    d.add(last_w[w_])
                d.update(readers.get(w_, ()))
            d.discard(i)
            for r in op['r']:
                readers.setdefault(r, []).append(i)
            for w_ in op['w']:
                last_w[w_] = i
                readers[w_] = []
            deps.append(d)
        dma_sem_of = {}
        dma_val_of = {}
        sem_uses = [0] * N_DMA_SEMS
        sem_last = [None] * N_DMA_SEMS
        k = 0
        for i, op in enumerate(ops):
            if op['dma']:
                s = k % N_DMA_SEMS
                k += 1
                if sem_last[s] is not None:
                    deps[i].add(sem_last[s])
                sem_uses[s] += 1
                dma_sem_of[i] = s
                dma_val_of[i] = 16 * sem_uses[s]
                sem_last[s] = i
        red = []
        for i, op in enumerate(ops):
            best = {}
            dmas = []
            for j in deps[i]:
                oj = ops[j]
                if oj['dma']:
                    dmas.append(j)
                    continue
                if oj['eng'] == op['eng'] and not op['dma']:
                    if oj['eng'] in ('pe', 'sp') or not SAME_ENGINE_SYNC:
                        continue
                if oj['eng'] not in best or j > best[oj['eng']]:
                    best[oj['eng']] = j
            red.append((sorted(best.values()), sorted(dmas)))
        need_inc = [False] * len(ops)
        for i in range(len(ops)):
            for j in red[i][0]:
                need_inc[j] = True
        cnt = {e: 0 for e in engs}
        val_of = {}
        for i, op in enumerate(ops):
            if need_inc[i]:
                cnt[op['eng']] += 1
                val_of[i] = cnt[op['eng']]
        esem = {e: stack.enter_context(nc.semaphore('s_' + e)) for e in engs}
        dsem = [stack.enter_context(nc.semaphore('d%d' % s)) for s in range(N_DMA_SEMS)]
        waited = set()
        for i in range(len(ops)):
            for j in deps[i]:
                waited.add(j)
        tail = [i for i, op in enumerate(ops) if op['dma'] and i not in waited]
        per_eng = {e: [i for i, op in enumerate(ops) if op['eng'] == e] for e in engs}

        def body(ename, eng):
            seen = {}
            for i in per_eng[ename]:
                op = ops[i]
                need = {}
                for j in red[i][0]:
                    key = ('e', ops[j]['eng'])
                    need[key] = max(need.get(key, 0), val_of[j])
                for j in red[i][1]:
                    key = ('d', dma_sem_of[j])
                    need[key] = max(need.get(key, 0), dma_val_of[j])
                for key, val in need.items():
                    if seen.get(key, 0) >= val:
                        continue
                    seen[key] = val
                    sem = esem[key[1]] if key[0] == 'e' else dsem[key[1]]
                    eng.wait_ge(sem, val)
                inst = op['fn'](eng)
                if op['dma']:
                    inst.then_inc(dsem[dma_sem_of[i]], 16)
                elif need_inc[i]:
                    inst.then_inc(esem[ename], 1)
            if ename == 'sp':
                for j in tail:
                    eng.wait_ge(dsem[dma_sem_of[j]], dma_val_of[j])

        block = stack.enter_context(nc.Block())

        @block.tensor
        def _(e):
            body('pe', e)

        @block.scalar
        def _(e):
            body('act', e)

        @block.vector
        def _(e):
            body('dve', e)

        @block.gpsimd
        def _(e):
            body('pool', e)

        @block.sync
        def _(e):
            body('sp', e)


class Arena:
    def __init__(self, nc):
        self.nc = nc
        self.base = (nc.sbuf_base + 31) // 32 * 32
        self.top = nc.sbuf_top
        self.cur = self.base
        self.n = 0

    def alloc(self, name, shape, dt):
        esz = 2 if dt == BF16 else 4
        nbytes = int(np.prod(shape[1:])) * esz
        off = self.cur
        self.cur = (off + nbytes + 31) // 32 * 32
        assert self.cur <= self.top, ('SBUF overflow', name, self.cur, self.top)
        self.n += 1
        return self.nc.alloc_sbuf_tensor_at('%s_%d' % (name, self.n), list(shape), dt, offset=off)

    def mark(self):
        return self.cur

    def release(self, m):
        self.cur = m


def inproj_blocks():
    blocks = []
    idx = {}
    for p in range(4):
        for role, ri in (('q', 0), ('k', 1), ('v', 2)):
            for g in range(3):
                c0 = ((ri * 3 + g) * 8 + 2 * p) * 64
                idx[('A', p, role, g)] = len(blocks)
                blocks.append(np.arange(c0, c0 + 128))
    for gi in range(8):
        idx[('B', 'q', gi)] = len(blocks)
        blocks.append(np.concatenate([A_QKV + gi * 64 + np.arange(64), A_QKV + (8 + gi) * 64 + np.arange(64)]))
    idx[('B', 'k')] = len(blocks)
    blocks.append(A_QKV + 1024 + np.arange(128))
    idx[('B', 'v')] = len(blocks)
    blocks.append(A_QKV + 1024 + 128 + np.arange(128))
    for f in range(16):
        idx[('G', f)] = len(blocks)
        blocks.append(A_QKV + B_QKV + f * 128 + np.arange(128))
    return blocks, idx


BLOCKS, BIDX = inproj_blocks()
NBLK = len(BLOCKS)
VBLKS = [BIDX[('A', p, 'v', g)] for p in range(4) for g in range(3)] + [BIDX[('B', 'v')]]
VIDX = {b: i for i, b in enumerate(VBLKS)}


def perm_block_tokens(d, blk):
    L = T // d
    pos = 128 * blk
    r, j0 = pos // L, pos % L
    return j0 * d + r, d


def build(debug=None, phases='A'):
    nc = bass.Bass('TRN2', target_bir_lowering=False)
    P = Prog(nc)
    stack = ExitStack()
    ar = Arena(nc)

    def dram(name, shape, dt=F32, kind='ExternalInput'):
        return nc.dram_tensor(name, list(shape), dt, kind=kind).ap()

    xT = dram('xT', [D, T])
    gmix = dram('gmix', [128, NCH])
    win = dram('win', [NBLK, 128, NCH, 128])
    binb = dram('binb', [128, NBLK])
    bvrep = dram('bvrep', [len(VBLKS), 128, 128])
    cosd = dram('cosT', [128, T])
    sind = dram('sinT', [128, T])
    cbf = dram('cbf', [128, 256 + 512 * 6 + 128], BF16)
    wpa = dram('wpa', [8, 128, 4, 128])
    wpb = dram('wpb', [8, 128, 8, 128])
    wo = dram('wo', [8, 128, 8, 128])
    gffn = dram('gffn', [128, NCH])
    sinkrep = dram('sinkrep', [128, 16])
    wr = dram('wr', [128, NCH, 36])
    brep = dram('brep', [128, 36])
    weg = dram('weg', [NEXP, 128, NCH, DEXP])
    weu = dram('weu', [NEXP, 128, NCH, DEXP])
    wed = dram('wed', [NEXP, 128, 2, D])
    wegR = weg.rearrange('e p c f -> (e p) (c f)')
    weuR = weu.rearrange('e p c f -> (e p) (c f)')
    wedR = wed.rearrange('e p c d -> (e p) (c d)')
    identf_d = dram('identf', [128, 128])
    thr_d = dram('thr', [128, 32, 32])
    jrow_d = dram('jrow', [128, 64])
    pcol_d = dram('pcol', [128, 1])
    gfr_d = dram('gfr', [128, D])
    outd = dram('out', [T, D], kind='ExternalOutput')
    wgb = dram('wgb', [NEXP * 128, NCH * DEXP], BF16, kind='Internal')
    wub = dram('wub', [NEXP * 128, NCH * DEXP], BF16, kind='Internal')
    wdb = dram('wdb', [NEXP * 128, 2 * D], BF16, kind='Internal')
    B0 = BIDX[('B', 'q', 0)]
    NB3 = NBLK - B0
    winb = dram('winb', [NB3 * 128, NCH * 128], BF16, kind='Internal')
    wpab = dram('wpab', [8 * 128, 4 * 128], BF16, kind='Internal')
    wpbb = dram('wpbb', [8 * 128, NCH * 128], BF16, kind='Internal')
    wob = dram('wob', [8 * 128, NCH * 128], BF16, kind='Internal')
    winR = win.rearrange('b p c n -> (b p) (c n)')
    wpaR = wpa.rearrange('f p c n -> (f p) (c n)')
    wpbR = wpb.rearrange('f p c n -> (f p) (c n)')
    woR = wo.rearrange('f p c n -> (f p) (c n)')
    casts2 = []
    for r0 in range(0, NB3 * 128, 512):
        r1 = min(r0 + 512, NB3 * 128)
        casts2.append((winR[B0 * 128 + r0:B0 * 128 + r1, :], winb[r0:r1, :]))
    for (s_, d_) in ((wpaR, wpab), (wpbR, wpbb), (woR, wob)):
        for r0 in (0, 512):
            casts2.append((s_[r0:r0 + 512, :], d_[r0:r0 + 512, :]))
    X1 = dram('X1s', [T, D], F32, kind='Internal')
    H2 = dram('H2s', [T, D], BF16, kind='Internal')
    Xs = dram('Xss', [64 * 256, D], BF16, kind='Internal')
    Ys = dram('Yss', [64 * 256, D], F32, kind='Internal')
    xTv = xT.rearrange('(c p) t -> p c t', p=128)

    ones_bf = ar.alloc('ones_bf', [128, 128], BF16)
    gmix_sb = ar.alloc('gmix_sb', [128, NCH], F32)
    bin_sb = ar.alloc('bin_sb', [128, NBLK], F32)
    cbf_sb = ar.alloc('cbf_sb', [128, 256 + 512], BF16)
    rotT = cbf_sb[:, 0:128]
    ident = cbf_sb[:, 128:256]
    maskB = cbf_sb[:, 256:768]
    gffn_sb = ar.alloc('gffn_sb', [128, NCH], F32)
    esink = ar.alloc('esink', [128, 16], F32)
    selb = ar.alloc('selb', [128, 32, 32], BF16)
    Ff = ar.alloc('Ff', [128, 32, 32], BF16)
    w0 = ar.alloc('w0', [128, 32], F32)
    w1 = ar.alloc('w1', [128, 32], F32)
    identf = ar.alloc('identf', [128, 128], F32)
    m_big = ar.mark()
    hT = ar.alloc('hT', [128, NCH, T], BF16)
    yaT = ar.alloc('yaT', [128, 4, T], BF16)
    cosw = ar.alloc('cosw', [128, W], F32)
    sinw = ar.alloc('sinw', [128, W], F32)
    zT2 = [ar.alloc('zT', [128, W], BF16) for i in range(2)]
    tt2 = [ar.alloc('tt', [128, W], F32) for i in range(2)]
    uu2 = [ar.alloc('uu', [128, W], F32) for i in range(2)]
    PT = [ar.alloc('PT', [128, 1024], BF16) for i in range(2)]
    bv_sb = ar.alloc('bv_sb', [128, 128], F32)
    esinkT2 = ar.alloc('esinkT2', [128, 2, 512], F32)
    psum = [stack.enter_context(nc.psum_tensor('ps%d' % i, [128, 512], F32)) for i in range(8)]

    P.add('dve', lambda e: e.memset(ones_bf[:], 1.0), w=['ones'])
    P.add('sp', lambda e: e.dma_start(out=gmix_sb[:], in_=gmix), w=['gmix'], dma=True)
    P.add('sp', lambda e: e.dma_start(out=bin_sb[:], in_=binb), w=['bin'], dma=True)
    P.add('sp', lambda e: e.dma_start(out=cbf_sb[:], in_=cbf[:, 0:768]), w=['cbf'], dma=True)
    P.add('sp', lambda e: e.dma_start(out=gffn_sb[:], in_=gffn), w=['gffn'], dma=True)
    P.add('sp', lambda e: e.dma_start(out=esink[:], in_=sinkrep), w=['esink'], dma=True)
    P.add('act', lambda e: e.activation(out=esink[:], in_=esink[:], func=AF.Exp), r=['esink'], w=['esink'])
    for kvh in range(2):
        for half in range(2):
            for j in range(4):
                qh = kvh * 8 + 4 * half + j
                rws = slice(64 * kvh, 64 * kvh + 64)
                P.add('dve', lambda e, rws=rws, half=half, j=j, qh=qh: e.tensor_copy(
                    out=esinkT2[rws, half, j * 128:(j + 1) * 128], in_=esink[rws, qh:qh + 1].broadcast_to([64, 128])),
                    r=['esink'], w=['esinkT2'])

    m1 = ar.mark()
    xw = [ar.alloc('xw', [128, NCH, W], F32) for i in range(3)]
    sq = [ar.alloc('sq', [128, NCH, W], BF16) for i in range(3)]
    rstd = [ar.alloc('rstd', [128, W], F32) for i in range(3)]
    srt = rstd
    for w in range(NW):
        s = w % 3
        ws = slice(w * W, (w + 1) * W)
        P.add('sp', lambda e, s=s, ws=ws: e.dma_start(out=xw[s][:], in_=xTv[:, :, ws]),
              w=[('xw', s)], dma=True)
        P.add('act', lambda e, s=s: e.activation(out=sq[s][:], in_=xw[s][:], func=AF.Square),
              r=[('xw', s)], w=[('sq', s)])
        pb = psum[s]
        for c in range(NCH):
            P.add('pe', lambda e, s=s, c=c, pb=pb: e.matmul(pb[:], lhsT=ones_bf[:], rhs=sq[s][:, c, :],
                                                           start=(c == 0), stop=(c == NCH - 1)),
                  r=[('sq', s), 'ones'], w=[('ps', s)])
        P.add('act', lambda e, s=s, pb=pb: e.activation(out=srt[s][:], in_=pb[:], func=AF.Ln,
                                                        scale=1.0 / D, bias=EPS),
              r=[('ps', s)], w=[('srt', s), ('rstd', s)])
        P.add('act', lambda e, s=s: e.activation(out=rstd[s][:], in_=srt[s][:], func=AF.Exp, scale=-0.5),
              r=[('srt', s)], w=[('rstd', s)])
        for c in range(NCH):
            P.add('dve', lambda e, s=s, c=c, ws=ws: e.scalar_tensor_tensor(
                out=hT[:, c, ws], in0=xw[s][:, c, :], scalar=gmix_sb[:, c:c + 1], in1=rstd[s][:],
                op0=ALU.mult, op1=ALU.mult),
                r=[('xw', s), ('rstd', s), 'gmix'], w=[('hT', w)])
    P.barrier('p1')
    ar.release(m1)

    mA = ar.mark()
    KT = [ar.alloc('KT', [128, T], BF16) for g in range(3)]
    Vst = [ar.alloc('Vst', [128, 32, 128], BF16) for g in range(3)]
    QW = [ar.alloc('QW', [128, W], BF16) for g in range(3)]
    wkv = [ar.alloc('wkv', [128, NCH, 128], BF16) for g in range(3)]
    wq = [ar.alloc('wq', [128, NCH, 128], BF16) for g in range(3)]
    mska = ar.alloc('mska', [128, 512 * 5], BF16)
    maskA = mska[:, 0:512]
    maskA16 = [mska[:, 512 * (v + 1): 512 * (v + 2)] for v in range(4)]
    P.add('sp', lambda e: e.dma_start(out=mska[:], in_=cbf[:, 768:768 + 2560]), w=['cbf'], dma=True)
    Uacc = ar.alloc('Uacc', [128, W], F32)
    Dacc = ar.alloc('Dacc', [128, W], F32)
    rD = ar.alloc('rD', [128, W], F32)
    cosw2 = ar.alloc('cosw2', [128, W], F32)
    sinw2 = ar.alloc('sinw2', [128, W], F32)
    cs_bufs = [(cosw, sinw, 'cosw', 'sinw'), (cosw2, sinw2, 'cosw2', 'sinw2')]
    cs_state = {'i': 0, 'n': 2}

    PS_Z, PS_R, PS_U, PS_D = 0, 1, 6, 7
    PS_S = [(4, 5), (4, 5)]
    unit_ctr = [0]

    def load_w_raw(dst, blk, res):
        load_w(dst, blk, res)

    def load_w(dst, blk, res):
        P.add('pool', lambda e: e.dma_start(out=dst[:].rearrange('p c n -> p (c n)'), in_=win[blk].rearrange('p c n -> p (c n)')), w=[res], dma=True)

    def load_cs(w):
        ws = slice(w * W, (w + 1) * W)
        cs_state['i'] = (cs_state['i'] + 1) % cs_state['n']
        cb, sb, cn, sn = cs_bufs[cs_state['i']]
        P.add('sp', lambda e: e.dma_start(out=cb[:], in_=cosd[:, ws]), w=[cn], dma=True)
        P.add('sp', lambda e: e.dma_start(out=sb[:], in_=sind[:, ws]), w=[sn], dma=True)

    proj_ctr = [0]
    proj_pend = {'p': None}

    def proj_flush():
        if proj_pend['p'] is not None:
            proj_pend['p']()
        proj_pend['p'] = None

    def proj_rope(wt, wres, blk, w, dst_ap, dst_res, d):
        ws = slice(w * W, (w + 1) * W)
        zb = proj_ctr[0] % 2
        proj_ctr[0] += 1
        bz, br = 0 + zb, 2 + zb
        pz, pr = psum[bz], psum[br]
        zT, tt, uu = zT2[zb], tt2[zb], uu2[zb]
        cb, sb, cn, sn = cs_bufs[cs_state['i']]
        for c in range(NCH):
            P.add('pe', lambda e, c=c: e.matmul(pz[:], lhsT=wt[:, c, :], rhs=hT[:, c, ws],
                                                start=(c == 0), stop=(c == NCH - 1)),
                  r=[wres, ('hT', w)], w=[('ps', bz)])
        P.add('act', lambda e: e.activation(out=zT[:], in_=pz[:], func=AF.Identity,
                                            bias=bin_sb[:, blk:blk + 1], scale=1.0),
              r=[('ps', bz), 'bin'], w=[('zT', zb)])

        def part2():
            P.add('pe', lambda e: e.matmul(pr[:], lhsT=rotT, rhs=zT[:], start=True, stop=True),
                  r=[('zT', zb), 'cbf'], w=[('ps', br)])
            P.add('dve', lambda e: e.tensor_tensor(out=uu[:], in0=pr[:], in1=sb[:], op=ALU.mult),
                  r=[('ps', br), sn], w=[('uu', zb)])
            P.add('pool', lambda e: e.tensor_tensor(out=tt[:], in0=zT[:], in1=cb[:], op=ALU.mult),
                  r=[('zT', zb), cn], w=[('tt', zb)])
            if d == 1:
                a, b = tt[:], uu[:]
            else:
                a = tt[:].rearrange('p (j r) -> p r j', r=d)
                b = uu[:].rearrange('p (j r) -> p r j', r=d)
            P.add('dve', lambda e: e.tensor_tensor(out=dst_ap, in0=a, in1=b, op=ALU.add),
                  r=[('tt', zb), ('uu', zb)], w=[dst_res])

        prev = proj_pend['p']
        proj_pend['p'] = part2
        if prev is not None:
            prev()

    pipe = {'pending': None}

    def pipe_push(front, back):
        front()
        if pipe['pending'] is not None:
            pipe['pending']()
        pipe['pending'] = back

    def pipe_flush():
        if pipe['pending'] is not None:
            pipe['pending']()
        pipe['pending'] = None

    def attn_unit(tiles, qsrc, qres, ksrc, kres_fn, vsrc, vres, mask_ap, hrow, evac, after=None):
        pipe_push(*attn_unit_parts(tiles, qsrc, qres, ksrc, kres_fn, vsrc, vres, mask_ap, hrow, evac, after))

    def attn_unit_parts(tiles, qsrc, qres, ksrc, kres_fn, vsrc, vres, mask_ap, hrow, evac, after):
        u = unit_ctr[0]
        unit_ctr[0] += 1
        sl = u % 2
        sb0, sb1 = PS_S[sl]
        pt = PT[sl]
        rows = slice(hrow, hrow + 64)
        PS_U, PS_D = (6, 7) if u % 2 == 0 else (2, 3)

        def front():
          for i, (qc, nq, kbp, kbc, kcp, kcc, uc) in enumerate(tiles):
            for half, kc in ((0, kcp), (1, kcc)):
                col = i * 2 * nq + half * nq
                bank = sb0 if col < 512 else sb1
                cc = col % 512
                P.add('pe', lambda e, bank=bank, cc=cc, kc=kc, qc=qc, nq=nq: e.matmul(
                    psum[bank][:, cc:cc + nq], lhsT=ksrc[rows, kc:kc + 128], rhs=qsrc[rows, qc:qc + nq],
                    start=True, stop=True),
                    r=list(qres) + kres_fn(kc), w=[('ps', bank)])
          for hb, bank in ((0, sb0), (1, sb1)):
            P.add('act', lambda e, hb=hb, bank=bank: e.activation(
                out=pt[:, hb * 512:(hb + 1) * 512], in_=psum[bank][:], func=AF.Exp, scale=0.125),
                r=[('ps', bank)], w=[('PT', sl, hb)])
            P.add('dve' if hb == 0 else 'pool', lambda e, hb=hb: e.tensor_tensor(
                out=pt[:, hb * 512:(hb + 1) * 512], in0=pt[:, hb * 512:(hb + 1) * 512], in1=mask_ap,
                op=ALU.mult),
                r=[('PT', sl, hb), 'cbf'], w=[('PT', sl, hb)])
        def back():
          first = True
          for i, (qc, nq, kbp, kbc, kcp, kcc, uc) in enumerate(tiles):
            for half, kb in ((0, kbp), (1, kbc)):
                if kb is None:
                    continue
                col = i * 2 * nq + half * nq
                hb = col // 512
                P.add('pe', lambda e, first=first, col=col, kb=kb, uc=uc, nq=nq: e.matmul(
                    psum[PS_U][:, uc:uc + nq], lhsT=vsrc[:, kb, :], rhs=pt[:, col:col + nq],
                    start=first, stop=False, skip_group_check=True),
                    r=[('PT', sl, hb)] + vres(kb), w=[('ps', PS_U)])
                P.add('pe', lambda e, first=first, col=col, kb=kb, uc=uc, nq=nq: e.matmul(
                    psum[PS_D][:, uc:uc + nq], lhsT=ones_bf[:], rhs=pt[:, col:col + nq],
                    start=first, stop=False, skip_group_check=True),
                    r=[('PT', sl, hb), 'ones'], w=[('ps', PS_D)])
                first = False
          evac(rows, PS_U, PS_D)
          if after is not None:
              after()
        return front, back

    for p in range(4):
        for g in range(3):
            load_w(wkv[g], BIDX[('A', p, 'k', g)], ('wkv', g))
        for w in range(NW):
            proj_flush()
            load_cs(w)
            for g in range(3):
                d = DIL[g]
                L = T // d
                dst = KT[g][:].rearrange('p (r j) -> p r j', r=d)[:, :, w * W // d:(w + 1) * W // d] if d > 1 \
                    else KT[g][:, w * W:(w + 1) * W]
                proj_rope(wkv[g], ('wkv', g), BIDX[('A', p, 'k', g)], w, dst, ('KT', g), d)
        proj_flush()
        for g in range(3):
            load_w(wkv[g], BIDX[('A', p, 'v', g)], ('wkv', g))
        for g in range(3):
            d = DIL[g]
            vb = BIDX[('A', p, 'v', g)]
            P.add('sp', lambda e, vb=vb: e.dma_start(out=bv_sb[:], in_=bvrep[VIDX[vb]]), w=['bv'], dma=True)
            for bg in range(8):
                bank = 4 + (bg % 2)
                for i in range(4):
                    blk = bg * 4 + i
                    t0, st = perm_block_tokens(d, blk)
                    for c in range(NCH):
                        P.add('pe', lambda e, c=c, i=i, t0=t0, st=st, g=g, bank=bank: e.matmul(
                            psum[bank][:, i * 128:(i + 1) * 128],
                            lhsT=hT[:, c, t0:t0 + 127 * st + 1:st], rhs=wkv[g][:, c, :],
                            start=(c == 0), stop=(c == NCH - 1)),
                            r=[('wkv', g)] + [('hT', ww) for ww in range(NW)], w=[('ps', bank)])
                P.add('dve', lambda e, g=g, bg=bg, bank=bank: e.tensor_tensor(
                    out=Vst[g][:, bg * 4:(bg + 1) * 4, :], in0=psum[bank][:].rearrange('p (i n) -> p i n', i=4),
                    in1=bv_sb[:].unsqueeze(1).broadcast_to([128, 4, 128]), op=ALU.add),
                    r=[('ps', bank), 'bv'], w=[('Vst', g)])
        for g in range(3):
            load_w(wq[g], BIDX[('A', p, 'q', g)], ('wq', g))
        for w in range(NW):
            it = p * NW + w
            if it < 7:
                for k2 in (2 * it, 2 * it + 1):
                    if k2 < len(casts2):
                        s_, d_ = casts2[k2]
                        P.add('pool', lambda e, s_=s_, d_=d_: e.dma_start(out=d_, in_=s_), w=[('wcast2', k2)], dma=True)
            elif it - 7 < 24:
                ee = it - 7
                src_, dst_ = ((wegR, wgb), (weuR, wub), (wedR, wdb))[ee % 3]
                rws = slice((ee // 3) * 512, (ee // 3 + 1) * 512)
                P.add('pool', lambda e, src_=src_, dst_=dst_, rws=rws: e.dma_start(out=dst_[rws, :], in_=src_[rws, :]),
                      w=[('wcast', ee)], dma=True)
            load_cs(w)
            for g in range(3):
                d = DIL[g]
                dst = QW[g][:].rearrange('p (r j) -> p r j', r=d) if d > 1 else QW[g][:]
                proj_rope(wq[g], ('wq', g), BIDX[('A', p, 'q', g)], w, dst, ('QW', g), d)
            proj_flush()
            for hh in range(2):
                for g in range(3):
                    d = DIL[g]
                    L = T // d
                    tiles = []
                    if d == 1:
                        for i in range(4):
                            qb = 4 * w + i
                            kbp = qb - 1 if qb >= 1 else None
                            tiles.append((128 * i, 128, kbp, qb, 128 * max(qb - 1, 0), 128 * qb, 128 * i))
                        mask_ap = maskA
                    elif d == 4:
                        for r in range(4):
                            qb = w
                            base = r * (L // 128)
                            kbp = base + qb - 1 if qb >= 1 else None
                            tiles.append((128 * r, 128, kbp, base + qb, 128 * (base + max(qb - 1, 0)), 128 * (base + qb), 128 * r))
                        mask_ap = maskA
                    else:
                        for r in range(16):
                            qb = w // 4
                            base = r * (L // 128)
                            kbp = base + qb - 1 if qb >= 1 else None
                            tiles.append((32 * r, 32, kbp, base + qb, 128 * (base + max(qb - 1, 0)), 128 * (base + qb), 32 * r))
                        mask_ap = maskA16[w % 4]

                    def evac(rows, PS_U, PS_D, g=g, d=d):
                        if g == 0:
                            P.add('act', lambda e: e.activation(out=Uacc[rows, :], in_=psum[PS_U][rows, :], func=AF.Copy),
                                  r=[('ps', PS_U)], w=['Uacc'])
                            P.add('act', lambda e: e.activation(out=Dacc[rows, :], in_=psum[PS_D][rows, :], func=AF.Copy),
                                  r=[('ps', PS_D)], w=['Dacc'])
                        else:
                            for acc, bank, nm in ((Uacc, PS_U, 'Uacc'), (Dacc, PS_D, 'Dacc')):
                                av = acc[rows, :].rearrange('p (j r) -> p r j', r=d)
                                pv = psum[bank][rows, :].rearrange('p (r j) -> p r j', r=d)
                                P.add('dve', lambda e, av=av, pv=pv: e.tensor_tensor(out=av, in0=av, in1=pv, op=ALU.add),
                                      r=[('ps', bank), nm], w=[nm])

                    def fin(p=p, w=w):
                        ws = slice(w * W, (w + 1) * W)
                        P.add('act', lambda e: e.activation(out=rD[:], in_=Dacc[:], func=AF.Ln), r=['Dacc'], w=['rD'])
                        P.add('act', lambda e: e.activation(out=rD[:], in_=rD[:], func=AF.Exp, scale=-1.0), r=['rD'], w=['rD'])
                        P.add('dve', lambda e: e.tensor_tensor(out=yaT[:, p, ws], in0=Uacc[:], in1=rD[:], op=ALU.mult),
                              r=['Uacc', 'rD'], w=[('yaT', p, w)])

                    attn_unit(tiles, QW[g], [('QW', g)], KT[g], lambda kc, g=g: [('KT', g)], Vst[g], lambda kb, g=g: [('Vst', g)],
                              mask_ap, 64 * hh, evac, after=(fin if (hh == 1 and g == 2) else None))
        pipe_flush()
    P.barrier('pA')
    ar.release(mA)
    cs_state['n'] = 1
    cs_state['i'] = 0

    m3 = ar.mark()
    acc = ar.alloc('acc', [128, NCH, W], F32)
    P.add('sp', lambda e: e.dma_start(out=identf[:], in_=identf_d), w=['identf'], dma=True)
    P.add('sp', lambda e: e.nop(), r=[('wcast2', k2) for k2 in range(len(casts2))], w=['wcast2All'])
    KBq = ar.alloc('KBq', [128, 128 + 2 * W], BF16)
    VBq = ar.alloc('VBq', [128, 9, 128], BF16)
    NWSL = 4
    wsl = [ar.alloc('wsl', [128, NCH, 128], BF16) for i in range(NWSL)]

    def attn_unit_B(QB, i, kvh, lb, has_prev, evac):
        u = unit_ctr[0]
        unit_ctr[0] += 1
        sl = u % 2
        sb0, sb1 = PS_S[sl]
        pt = PT[sl]
        rows = slice(64 * kvh, 64 * kvh + 64)
        PS_U, PS_D = (6, 7) if u % 2 == 0 else (2, 3)
        kcp, kcc = 128 * (lb - 1 if has_prev else lb), 128 * lb
        qv = QB[rows, :].rearrange('p (j q) -> p j q', j=4)[:, :, 128 * i:128 * (i + 1)]
        kres = [('KBq', 'prev'), ('KBq', 0), ('KBq', 1)]
        vres = [('VBq', 'prev'), ('VBq', 0), ('VBq', 1)]

        def front():
            for bank, kc in ((sb0, kcp), (sb1, kcc)):
                P.add('pe', lambda e, bank=bank, kc=kc: e.matmul(
                    psum[bank][:].rearrange('p (j q) -> p j q', j=4), lhsT=KBq[rows, kc:kc + 128], rhs=qv,
                    start=True, stop=True),
                    r=[('QB', j) for j in range(4)] + kres, w=[('ps', bank)])
            for hb, bank in ((0, sb0), (1, sb1)):
                P.add('act', lambda e, hb=hb, bank=bank: e.activation(
                    out=pt[:, hb * 512:(hb + 1) * 512], in_=psum[bank][:], func=AF.Exp, scale=0.125),
                    r=[('ps', bank)], w=[('PT', sl, hb)])
                P.add('dve' if hb == 0 else 'pool', lambda e, hb=hb: e.tensor_tensor(
                    out=pt[:, hb * 512:(hb + 1) * 512].rearrange('p (j q) -> p j q', j=4),
                    in0=pt[:, hb * 512:(hb + 1) * 512].rearrange('p (j q) -> p j q', j=4),
                    in1=maskB[:, hb * 128:(hb + 1) * 128].unsqueeze(1).broadcast_to([128, 4, 128]), op=ALU.mult),
                    r=[('PT', sl, hb), 'cbf'], w=[('PT', sl, hb)])

        def back():
            first = True
            for hb, kb in ((0, (lb - 1) if has_prev else None), (1, lb)):
                if kb is None:
                    continue
                P.add('pe', lambda e, first=first, hb=hb, kb=kb: e.matmul(
                    psum[PS_U][:], lhsT=VBq[:, kb, :], rhs=pt[:, hb * 512:(hb + 1) * 512],
                    start=first, stop=False, skip_group_check=True),
                    r=[('PT', sl, hb)] + vres, w=[('ps', PS_U)])
                P.add('pe', lambda e, first=first, hb=hb: e.matmul(
                    psum[PS_D][:], lhsT=ones_bf[:], rhs=pt[:, hb * 512:(hb + 1) * 512],
                    start=first, stop=False, skip_group_check=True),
                    r=[('PT', sl, hb), 'ones'], w=[('ps', PS_D)])
                first = False
            evac(rows, PS_U, PS_D)
        return front, back

    wstate = {'issued': 0, 'used': 0}
    msub = ar.mark()

    def phase3(w):
        lw = w % 2
        ws = slice(w * W, (w + 1) * W)
        lws = slice(0, W)
        ar.release(msub)
        QB = ar.alloc('QB', [128, 4 * W], BF16)
        ybW = ar.alloc('ybW', [128, NCH, W], BF16)
        wa_s = [ar.alloc('wa_s', [128, 4, 128], BF16) for i in range(3)]
        wb_s = [ar.alloc('wb_s', [128, NCH, 128], BF16) for i in range(3)]
        wga_s = [ar.alloc('wga_s', [128, NCH, 128], BF16) for i in range(3)]
        wgb_s = [ar.alloc('wgb_s', [128, NCH, 128], BF16) for i in range(3)]
        wo_s = [ar.alloc('wo_s', [128, NCH, 128], BF16) for i in range(2)]
        ga = ar.alloc('ga', [128, W], F32)
        gb = ar.alloc('gb', [128, W], F32)
        mergedT = ar.alloc('mergedT', [128, NCH, W], BF16)
        xres = [ar.alloc('xres', [128, W], F32) for i in range(2)]
        Dt2 = [ga, gb]
        dctr = [0]
        blk_seq = [BIDX[('B', 'k')], BIDX[('B', 'v')]] + [BIDX[('B', 'q', gi)] for gi in range(8)]
        NBW = len(blk_seq)

        def load_w_raw(dst, blk, res):
            r0 = (blk - B0) * 128
            P.add('sp', lambda e: e.dma_start(out=dst[:].rearrange('p c n -> p (c n)'), in_=winb[r0:r0 + 128, :]),
                  r=['wcast2All'], w=[res], dma=True)

        def prefetch_to(n):
            while wstate['issued'] < min(n, NBW * NW):
                b = wstate['issued']
                load_w_raw(wsl[b % NWSL], blk_seq[b % NBW], ('wsl', b % NWSL))
                wstate['issued'] += 1

        def next_wsl():
            b = wstate['used']
            prefetch_to(b + NWSL)
            wstate['used'] += 1
            return wsl[b % NWSL], ('wsl', b % NWSL)

        def load_merge(f):
            s3 = f % 3
            P.add('sp', lambda e: e.dma_start(out=wa_s[s3][:].rearrange('p c n -> p (c n)'), in_=wpab[f * 128:(f + 1) * 128, :]),
                  r=['wcast2All'], w=[('wa', s3)], dma=True)
            P.add('sp', lambda e: e.dma_start(out=wb_s[s3][:].rearrange('p c n -> p (c n)'), in_=wpbb[f * 128:(f + 1) * 128, :]),
                  r=['wcast2All'], w=[('wb', s3)], dma=True)
            load_w_raw(wga_s[s3], BIDX[('G', f)], ('wga', s3))
            load_w_raw(wgb_s[s3], BIDX[('G', 8 + f)], ('wgb', s3))

        wo3 = [wo_s[0][:].rearrange('p c n -> p (c n)'), wo_s[1][:].rearrange('p c n -> p (c n)'), QB[:, 0:NCH * 128]]
        wo3res = [[('wo', 0)], [('wo', 1)], [('QB', 0), ('QB', 1)]]

        def load_wo(f2):
            P.add('sp', lambda e: e.dma_start(out=wo3[f2 % 3], in_=wob[f2 * 128:(f2 + 1) * 128, :]),
                  r=['wcast2All'], w=wo3res[f2 % 3], dma=True)

        load_merge(0)
        load_cs(w)
        wt, wres = next_wsl()
        proj_rope(wt, wres, BIDX[('B', 'k')], w, KBq[:, 128 + lw * W:128 + (lw + 1) * W],
                  ('KBq', lw), 1)
        proj_flush()
        wt, wres = next_wsl()
        vb = BIDX[('B', 'v')]
        P.add('sp', lambda e: e.dma_start(out=bv_sb[:], in_=bvrep[VIDX[vb]]), w=['bv'], dma=True)
        bank = PS_S[0][0]
        for i in range(4):
            t0 = w * W + 128 * i
            for c in range(NCH):
                P.add('pe', lambda e, c=c, i=i, t0=t0, wt=wt: e.matmul(
                    psum[bank][:, i * 128:(i + 1) * 128], lhsT=hT[:, c, t0:t0 + 128], rhs=wt[:, c, :],
                    start=(c == 0), stop=(c == NCH - 1)),
                    r=[wres, ('hT', w)], w=[('ps', bank)])
        P.add('dve', lambda e: e.tensor_tensor(
            out=VBq[:, 1 + 4 * lw:5 + 4 * lw, :], in0=psum[bank][:].rearrange('p (i n) -> p i n', i=4),
            in1=bv_sb[:].unsqueeze(1).broadcast_to([128, 4, 128]), op=ALU.add),
            r=[('ps', bank), 'bv'], w=[('VBq', lw)])
        for half in range(2):
            for j in range(4):
                gi = 4 * half + j
                wt, wres = next_wsl()
                proj_rope(wt, wres, BIDX[('B', 'q', gi)], w, QB[:, j * W:(j + 1) * W], ('QB', j), 1)
            proj_flush()
            load_merge(1 + half)
            for i in range(4):
                lb = 1 + 4 * lw + i
                has_prev = not (w == 0 and i == 0)
                for kvh in range(2):
                    tiles = []
                    for j in range(4):
                        tiles.append((j * W + 128 * i, 128, (lb - 1) if has_prev else None, lb,
                                      128 * (lb - 1 if has_prev else lb), 128 * lb, 128 * j))

                    def evac(rows, PS_U, PS_D, kvh=kvh, i=i, half=half):
                        di = dctr[0] % 2
                        dctr[0] += 1
                        Dt = Dt2[di]
                        dn = 'ga' if di == 0 else 'gb'
                        P.add('dve', lambda e: e.tensor_tensor(out=Dt[rows, :], in0=psum[PS_D][rows, :], in1=esinkT2[rows, half, :],
                                                               op=ALU.add),
                              r=[('ps', PS_D), 'esinkT2'], w=[dn])
                        P.add('act', lambda e: e.activation(out=Dt[rows, :], in_=Dt[rows, :], func=AF.Ln), r=[dn], w=[dn])
                        P.add('act', lambda e: e.activation(out=Dt[rows, :], in_=Dt[rows, :], func=AF.Exp, scale=-1.0), r=[dn], w=[dn])
                        P.add('dve', lambda e: e.tensor_tensor(
                            out=ybW[rows, 4 * half:4 * half + 4, 128 * i:128 * (i + 1)],
                            in0=psum[PS_U][rows, :].rearrange('p (j q) -> p j q', j=4),
                            in1=Dt[rows, :].rearrange('p (j q) -> p j q', j=4), op=ALU.mult),
                            r=[('ps', PS_U), dn], w=['ybW'])

                    def kres(kc, lw=lw):
                        return [('KBq', 'prev'), ('KBq', 0), ('KBq', 1)]

                    def vres(kb, lw=lw):
                        return [('VBq', 'prev'), ('VBq', 0), ('VBq', 1)]

                    pipe_push(*attn_unit_B(QB, i, kvh, lb, has_prev, evac))
        pipe_flush()
        if lw == 1:
            P.add('dve', lambda e: e.tensor_copy(out=KBq[:, 0:128], in_=KBq[:, 2 * W:2 * W + 128]),
                  r=[('KBq', 1)], w=[('KBq', 'prev')])
            P.add('dve', lambda e: e.tensor_copy(out=VBq[:, 0, :], in_=VBq[:, 8, :]),
                  r=[('VBq', 1)], w=[('VBq', 'prev')])
        for f in range(8):
            s = f % 2
            bA, bB, bGA, bGB = (2, 3, 4, 5) if s == 0 else (6, 7, 0, 1)
            s = f % 3
            if f == 0:
                load_wo(0)
                load_wo(1)
                load_wo(2)
            for c in range(4):
                P.add('pe', lambda e, c=c, s=s: e.matmul(psum[bA][:], lhsT=wa_s[s][:, c, :], rhs=yaT[:, c, ws],
                                                         start=(c == 0), stop=(c == 3)),
                      r=[('wa', s)] + [('yaT', c, w)], w=[('ps', bA)])
            for c in range(NCH):
                P.add('pe', lambda e, c=c, s=s: e.matmul(psum[bB][:], lhsT=wb_s[s][:, c, :], rhs=ybW[:, c, :],
                                                         start=(c == 0), stop=(c == NCH - 1)),
                      r=[('wb', s), 'ybW'], w=[('ps', bB)])
            for c in range(NCH):
                P.add('pe', lambda e, c=c, s=s: e.matmul(psum[bGA][:], lhsT=wga_s[s][:, c, :], rhs=hT[:, c, ws],
                                                         start=(c == 0), stop=(c == NCH - 1)),
                      r=[('wga', s), ('hT', w)], w=[('ps', bGA)])
            for c in range(NCH):
                P.add('pe', lambda e, c=c, s=s: e.matmul(psum[bGB][:], lhsT=wgb_s[s][:, c, :], rhs=hT[:, c, ws],
                                                         start=(c == 0), stop=(c == NCH - 1)),
                      r=[('wgb', s), ('hT', w)], w=[('ps', bGB)])
            if f + 3 < 8:
                load_merge(f + 3)
            ba, bb = BIDX[('G', f)], BIDX[('G', 8 + f)]
            P.add('act', lambda e, ba=ba: e.activation(out=ga[:], in_=psum[bGA][:], func=AF.Sigmoid,
                                                       bias=bin_sb[:, ba:ba + 1], scale=1.0),
                  r=[('ps', bGA), 'bin'], w=['ga'])
            P.add('act', lambda e, bb=bb: e.activation(out=gb[:], in_=psum[bGB][:], func=AF.Sigmoid,
                                                       bias=bin_sb[:, bb:bb + 1], scale=1.0),
                  r=[('ps', bGB), 'bin'], w=['gb'])
            P.add('dve', lambda e: e.tensor_tensor(out=ga[:], in0=psum[bA][:], in1=ga[:], op=ALU.mult),
                  r=[('ps', bA), 'ga'], w=['ga'])
            P.add('dve', lambda e: e.tensor_tensor(out=gb[:], in0=psum[bB][:], in1=gb[:], op=ALU.mult),
                  r=[('ps', bB), 'gb'], w=['gb'])
            P.add('dve', lambda e, f=f: e.tensor_tensor(out=mergedT[:, f, :], in0=ga[:], in1=gb[:], op=ALU.add),
                  r=['ga', 'gb'], w=['mergedT'])
        for f2 in range(8):
            s = f2 % 2
            P.add('sp', lambda e, f2=f2, s=s: e.dma_start(out=xres[s][:], in_=xTv[:, f2, ws]), w=[('xres', s)], dma=True)
            for c in range(NCH):
                P.add('pe', lambda e, c=c, s=s, f2=f2: e.matmul(psum[s][:], lhsT=wo3[f2 % 3][:, c * 128:(c + 1) * 128], rhs=mergedT[:, c, :],
                                                         start=(c == 0), stop=(c == NCH - 1)),
                      r=wo3res[f2 % 3] + ['mergedT'], w=[('ps', s)])
            P.add('dve', lambda e, f2=f2, s=s: e.tensor_tensor(out=acc[:, f2, lws], in0=psum[s][:], in1=xres[s][:],
                                                               op=ALU.add),
                  r=[('ps', s), ('xres', s)], w=['acc'])
            if f2 + 3 < 8:
                load_wo(f2 + 3)

    def norm_stats(sqb, srtb, rstdb):
        P.add('act', lambda e: e.activation(out=sqb[:], in_=acc[:], func=AF.Square), r=['acc'], w=['sqb'])
        for c in range(NCH):
            P.add('pe', lambda e, c=c: e.matmul(psum[7][:], lhsT=ones_bf[:], rhs=sqb[:, c, :],
                                                start=(c == 0), stop=(c == NCH - 1)),
                  r=['sqb', 'ones'], w=[('ps', 7)])
        P.add('act', lambda e: e.activation(out=srtb[:], in_=psum[7][:], func=AF.Ln, scale=1.0 / D, bias=EPS),
              r=[('ps', 7)], w=['srtb'])
        P.add('act', lambda e: e.activation(out=rstdb[:], in_=srtb[:], func=AF.Exp, scale=-0.5), r=['srtb'], w=['rstdb'])

    def post_window(w):
        ar.release(msub)
        sqb = ar.alloc('sqb', [128, NCH, W], BF16)
        srtb = ar.alloc('srtb', [128, W], F32)
        rstdb = ar.alloc('rstdb', [128, W], F32)
        h2f = ar.alloc('h2f', [128, NCH, W], F32)
        h2Tw = ar.alloc('h2Tw', [128, NCH, W], BF16)
        wr_sb = ar.alloc('wr_sb', [128, NCH, 36], F32)
        brep_sb = ar.alloc('brep_sb', [128, 36], F32)
        L4 = ar.alloc('L4', [128, 4, 36], F32)
        Lg = ar.alloc('Lg', [128, 4, 4], F32)
        oh4 = ar.alloc('oh4', [128, 4, 4], F32)
        Lm4 = ar.alloc('Lm4', [128, 4, 32], F32)
        ee4 = ar.alloc('ee4', [128, 4, 32], F32)
        tm4 = ar.alloc('tm4', [128, 4, 32], F32)
        top84 = ar.alloc('top84', [128, 4, 8], F32)
        sm4 = ar.alloc('sm4', [128, 12, 4], F32)
        h2tok = [ar.alloc('h2tok', [128, D], BF16) for i in range(2)]
        x1tok = [ar.alloc('x1tok', [128, D], F32) for i in range(2)]
        P.add('sp', lambda e: e.dma_start(out=wr_sb[:], in_=wr), w=['wr'], dma=True)
        P.add('sp', lambda e: e.dma_start(out=brep_sb[:], in_=brep), w=['brep'], dma=True)
        BIG = 30000.0
        def S1(t4):
            ts_ = slice(t4 * 128, (t4 + 1) * 128)
            P.add('act', lambda e: e.activation(out=sqb[:, :, ts_], in_=acc[:, :, ts_], func=AF.Square),
                  r=['acc'], w=[('sqb', t4)])
            for c in range(NCH):
                P.add('pe', lambda e, c=c: e.matmul(psum[7][:, ts_], lhsT=ones_bf[:], rhs=sqb[:, c, ts_],
                                                    start=(c == 0), stop=(c == NCH - 1)),
                      r=[('sqb', t4), 'ones'], w=[('ps7', t4)])
            P.add('act', lambda e: e.activation(out=srtb[:, ts_], in_=psum[7][:, ts_], func=AF.Ln, scale=1.0 / D, bias=EPS),
                  r=[('ps7', t4)], w=[('srtb', t4)])
            P.add('act', lambda e: e.activation(out=rstdb[:, ts_], in_=srtb[:, ts_], func=AF.Exp, scale=-0.5),
                  r=[('srtb', t4)], w=[('rstdb', t4)])

        def S2(t4):
            ts_ = slice(t4 * 128, (t4 + 1) * 128)
            for c in range(NCH):
                P.add('dve', lambda e, c=c: e.scalar_tensor_tensor(
                    out=h2f[:, c, ts_], in0=acc[:, c, ts_], scalar=gffn_sb[:, c:c + 1], in1=rstdb[:, ts_],
                    op0=ALU.mult, op1=ALU.mult),
                    r=['acc', ('rstdb', t4), 'gffn'], w=[('h2f', t4)])
            P.add('act', lambda e: e.activation(out=h2Tw[:, :, ts_], in_=h2f[:, :, ts_], func=AF.Copy),
                  r=[('h2f', t4)], w=[('h2Tw', t4)])

        def S3a(t4):
            tg = 4 * w + t4
            s2 = t4 % 2
            ts_ = slice(t4 * 128, (t4 + 1) * 128)
            rows = slice(tg * 128, (tg + 1) * 128)
            b0 = 2 + 2 * s2
            for c in range(NCH):
                bank = b0 + c // 4
                P.add('pe', lambda e, c=c, bank=bank: e.transpose(
                    out=psum[bank][:, (c % 4) * 128:(c % 4 + 1) * 128], in_=acc[:, c, ts_], identity=identf[:]),
                    r=['acc', 'identf'], w=[('ps', bank)])
            P.add('dve', lambda e: e.tensor_copy(out=x1tok[s2][:, 0:512], in_=psum[b0][:]),
                  r=[('ps', b0)], w=[('x1tok', s2, 0)])
            P.add('dve', lambda e: e.tensor_copy(out=x1tok[s2][:, 512:1024], in_=psum[b0 + 1][:]),
                  r=[('ps', b0 + 1)], w=[('x1tok', s2, 1)])
            P.add('sp', lambda e: e.dma_start(out=X1[rows, :], in_=x1tok[s2][:]),
                  r=[('x1tok', s2, 0), ('x1tok', s2, 1)], w=[('X1', tg)], dma=True)

        def S3b(t4):
            tg = 4 * w + t4
            s2 = t4 % 2
            ts_ = slice(t4 * 128, (t4 + 1) * 128)
            rows = slice(tg * 128, (tg + 1) * 128)
            pb = psum[s2][:].bitcast(BF16)
            for c in range(NCH):
                P.add('pe', lambda e, c=c: e.transpose(out=pb[:, c * 128:(c + 1) * 128], in_=h2Tw[:, c, ts_], identity=ident),
                      r=[('h2Tw', t4), 'cbf'], w=[('ps', s2)])
            P.add('act', lambda e: e.activation(out=h2tok[s2][:], in_=pb, func=AF.Copy), r=[('ps', s2)], w=[('h2tok', s2)])
            P.add('sp', lambda e: e.dma_start(out=H2[rows, :], in_=h2tok[s2][:]), r=[('h2tok', s2)], w=[('H2', tg)], dma=True)
            pl = psum[6]
            for c in range(NCH):
                P.add('pe', lambda e, c=c: e.matmul(pl[:, t4 * 36:(t4 + 1) * 36], lhsT=h2f[:, c, ts_], rhs=wr_sb[:, c, :],
                                                    start=(c == 0), stop=(c == NCH - 1), skip_group_check=True),
                      r=[('h2f', t4), 'wr'], w=[('ps', 6)])

        for st in (lambda: S1(0), lambda: S3a(0), lambda: S1(1), lambda: S2(0), lambda: S3a(1), lambda: S1(2), lambda: S2(1),
                   lambda: S3b(0), lambda: S3a(2), lambda: S1(3), lambda: S2(2), lambda: S3b(1), lambda: S3a(3), lambda: S2(3),
                   lambda: S3b(2), lambda: S3b(3)):
            st()
        BIG = 30000.0
        tgs = slice(4 * w, 4 * w + 4)
        bc3 = lambda ap, n: ap.unsqueeze(2).broadcast_to([128, 4, n])
        P.add('dve', lambda e: e.tensor_tensor(out=L4[:], in0=psum[6][:, 0:144].rearrange('p (t n) -> p t n', t=4),
                                               in1=brep_sb[:].unsqueeze(1).broadcast_to([128, 4, 36]), op=ALU.add),
              r=[('ps', 6), 'brep'], w=['L4'])
        P.add('dve', lambda e: e.tensor_reduce(out=sm4[:, 0, :], in_=L4[:, :, 0:4], axis=AX.X, op=ALU.max), r=['L4'], w=['gmax'])
        P.add('dve', lambda e: e.tensor_tensor(out=Lg[:], in0=L4[:, :, 0:4], in1=bc3(sm4[:, 0, :], 4), op=ALU.subtract),
              r=['L4', 'gmax'], w=['Lg'])
        P.add('dve', lambda e: e.tensor_tensor(out=oh4[:], in0=L4[:, :, 0:4], in1=bc3(sm4[:, 0, :], 4), op=ALU.is_ge),
              r=['L4', 'gmax'], w=['oh4'])
        P.add('act', lambda e: e.activation(out=Lg[:], in_=Lg[:], func=AF.Exp), r=['Lg'], w=['Lg'])
        P.add('dve', lambda e: e.tensor_reduce(out=sm4[:, 1, :], in_=Lg[:], axis=AX.X, op=ALU.add), r=['Lg'], w=['sumg'])
        P.add('dve', lambda e: e.tensor_scalar(out=oh4[:], in0=oh4[:], scalar1=BIG, scalar2=-BIG, op0=ALU.mult, op1=ALU.add),
              r=['oh4'], w=['oh4'])
        P.add('dve', lambda e: e.tensor_tensor(
            out=Lm4[:].rearrange('p t (g x) -> p t g x', g=4), in0=L4[:, :, 4:36].rearrange('p t (g x) -> p t g x', g=4),
            in1=oh4[:].unsqueeze(3).broadcast_to([128, 4, 4, 8]), op=ALU.add), r=['L4', 'oh4'], w=['Lm4'])
        for t4 in range(4):
            P.add('dve', lambda e, t4=t4: e.max(out=top84[:, t4, :], in_=Lm4[:, t4, :]), r=['Lm4'], w=[('top84', t4)])
        t8 = [('top84', t4) for t4 in range(4)]
        P.add('dve', lambda e: e.tensor_tensor(out=ee4[:], in0=Lm4[:], in1=bc3(top84[:, :, 0], 32), op=ALU.subtract),
              r=['Lm4'] + t8, w=['ee4'])
        P.add('act', lambda e: e.activation(out=ee4[:], in_=ee4[:], func=AF.Exp), r=['ee4'], w=['ee4'])
        P.add('dve', lambda e: e.tensor_tensor(out=selb[:, tgs, :], in0=Lm4[:], in1=bc3(top84[:, :, 1], 32), op=ALU.is_ge),
              r=['Lm4'] + t8, w=[('selb', 4 * w + k) for k in range(4)])
        P.add('dve', lambda e: e.tensor_tensor(out=Ff[:, tgs, :], in0=Lm4[:], in1=bc3(top84[:, :, 0], 32), op=ALU.is_ge),
              r=['Lm4'] + t8, w=[('Ff', 4 * w + k) for k in range(4)])
        P.add('dve', lambda e: e.tensor_tensor(out=ee4[:], in0=ee4[:], in1=selb[:, tgs, :], op=ALU.mult),
              r=['ee4'] + [('selb', 4 * w + k) for k in range(4)], w=['ee4'])
        P.add('dve', lambda e: e.tensor_reduce(out=sm4[:, 2, :], in_=ee4[:], axis=AX.X, op=ALU.add), r=['ee4'], w=['ssum'])
        P.add('dve', lambda e: e.tensor_tensor(out=sm4[:, 3, :], in0=sm4[:, 2, :], in1=sm4[:, 1, :], op=ALU.mult),
              r=['ssum', 'sumg'], w=['den'])
        P.add('dve', lambda e: e.reciprocal(out=sm4[:, 4, :], in_=sm4[:, 3, :]), r=['den'], w=['rden'])
        P.add('dve', lambda e: e.tensor_tensor(out=tm4[:], in0=ee4[:], in1=Ff[:, tgs, :], op=ALU.mult),
              r=['ee4'] + [('Ff', 4 * w + k) for k in range(4)], w=['tm4'])
        P.add('dve', lambda e: e.tensor_reduce(out=sm4[:, 5, :], in_=tm4[:], axis=AX.X, op=ALU.add), r=['tm4'], w=['t0'])
        P.add('dve', lambda e: e.tensor_tensor(out=sm4[:, 6, :], in0=sm4[:, 2, :], in1=sm4[:, 5, :], op=ALU.subtract),
              r=['ssum', 't0'], w=['t1'])
        P.add('dve', lambda e: e.tensor_tensor(out=w0[:, tgs], in0=sm4[:, 5, :], in1=sm4[:, 4, :], op=ALU.mult),
              r=['t0', 'rden'], w=[('w0', 4 * w + k) for k in range(4)])
        P.add('dve', lambda e: e.tensor_tensor(out=w1[:, tgs], in0=sm4[:, 6, :], in1=sm4[:, 4, :], op=ALU.mult),
              r=['t1', 'rden'], w=[('w1', 4 * w + k) for k in range(4)])

    for w in range(NW if debug is None else int(os.environ.get('KNW', '2'))):
        phase3(w)
        P.barrier('p3_%d' % w)
        post_window(w)
        P.barrier('pw_%d' % w)
    NTG = 32 if debug is None else 4 * int(os.environ.get('KNW', '2'))

    ar.release(m_big)
    NT = 64
    triS = ar.alloc('triS', [128, 128], BF16)
    thr = ar.alloc('thr', [128, 32, 32], F32)
    jrow = ar.alloc('jrow', [128, NT], F32)
    pcol = ar.alloc('pcol', [128, 1], F32)
    rankf = ar.alloc('rankf', [128, 32, 32], F32)
    cmpb = ar.alloc('cmpb', [128, 32, 32], F32)
    totf = ar.alloc('totf', [128, 32], F32)
    tl = ar.alloc('tl', [128, 32], F32)
    scA = ar.alloc('scA', [128, 32], F32)
    scB = ar.alloc('scB', [128, 32], F32)
    offf = ar.alloc('offf', [128, 32], F32)
    s0f = ar.alloc('s0f', [128, 32], F32)
    s1f = ar.alloc('s1f', [128, 32], F32)
    s0i = ar.alloc('s0i', [128, 32], I32)
    s1i = ar.alloc('s1i', [128, 32], I32)
    eidf = ar.alloc('eidf', [128, NT], F32)
    widx = ar.alloc('widx', [128, NT], I32)
    P.add('sp', lambda e: e.dma_start(out=triS[:], in_=cbf[:, 768 + 2560:768 + 2560 + 128]), w=['triS'], dma=True)
    P.add('sp', lambda e: e.dma_start(out=thr[:], in_=thr_d), w=['thr'], dma=True)
    P.add('sp', lambda e: e.dma_start(out=jrow[:], in_=jrow_d), w=['jrow'], dma=True)
    P.add('sp', lambda e: e.dma_start(out=pcol[:], in_=pcol_d), w=['pcol'], dma=True)
    selres = [('selb', tg) for tg in range(NTG)]
    for tg in range(NTG):
        bank = tg // 16
        col = (tg % 16) * 32
        for t2 in range(tg):
            P.add('pe', lambda e, bank=bank, col=col, t2=t2, tg=tg: e.matmul(
                psum[bank][:, col:col + 32], lhsT=ones_bf[:], rhs=selb[:, t2, :],
                start=(tg % 16 == 0 and t2 == 0), stop=False, skip_group_check=True),
                r=selres + ['ones'], w=[('ps', bank)])
        P.add('pe', lambda e, bank=bank, col=col, tg=tg: e.matmul(
            psum[bank][:, col:col + 32], lhsT=triS[:], rhs=selb[:, tg, :],
            start=(tg == 0 or (tg % 16 == 0 and False)), stop=False, skip_group_check=True),
            r=selres + ['triS'], w=[('ps', bank)])
    for tg in range(NTG):
        P.add('pe', lambda e, tg=tg: e.matmul(psum[2][:, 0:32], lhsT=ones_bf[:], rhs=selb[:, tg, :],
                                              start=(tg == 0), stop=(tg == NTG - 1)),
              r=selres + ['ones'], w=[('ps', 2)])
    nb = (NTG + 15) // 16
    for b in range(nb):
        n16 = min(16, NTG - 16 * b)
        P.add('dve', lambda e, b=b, n16=n16: e.tensor_copy(
            out=rankf[:, 16 * b:16 * b + n16, :], in_=psum[b][:, 0:32 * n16].rearrange('p (t e) -> p t e', e=32)),
            r=[('ps', b)], w=['rankf'])
    P.add('dve', lambda e: e.tensor_copy(out=totf[:], in_=psum[2][:, 0:32]), r=[('ps', 2)], w=['totf'])
    P.add('dve', lambda e: e.tensor_tensor(out=cmpb[:], in0=totf[:].unsqueeze(2).broadcast_to([128, 32, 32]), in1=thr[:],
                                           op=ALU.is_gt), r=['totf', 'thr'], w=['cmpb'])
    P.add('dve', lambda e: e.tensor_reduce(out=tl[:], in_=cmpb[:], axis=AX.X, op=ALU.add), r=['cmpb'], w=['tl'])
    P.add('dve', lambda e: e.tensor_copy(out=scA[:], in_=tl[:]), r=['tl'], w=['scA'])
    cur, nxt, cn, nn = scA, scB, 'scA', 'scB'
    for sft in (1, 2, 4, 8, 16):
        P.add('dve', lambda e, cur=cur, nxt=nxt, sft=sft: e.tensor_copy(out=nxt[:, 0:sft], in_=cur[:, 0:sft]), r=[cn], w=[nn])
        P.add('dve', lambda e, cur=cur, nxt=nxt, sft=sft: e.tensor_tensor(out=nxt[:, sft:32], in0=cur[:, sft:32], in1=cur[:, 0:32 - sft],
                                                                          op=ALU.add), r=[cn], w=[nn])
        cur, nxt, cn, nn = nxt, cur, nn, cn
    endf, endn = cur, cn
    P.add('dve', lambda e: e.tensor_tensor(out=offf[:], in0=endf[:], in1=tl[:], op=ALU.subtract), r=[endn, 'tl'], w=['offf'])
    P.add('dve', lambda e: e.tensor_scalar(out=offf[:], in0=offf[:], scalar1=256.0, scalar2=None, op0=ALU.mult), r=['offf'], w=['offf'])
    P.add('dve', lambda e: e.tensor_tensor(out=rankf[:], in0=rankf[:], in1=offf[:].unsqueeze(1).broadcast_to([128, 32, 32]), op=ALU.add),
          r=['rankf', 'offf'], w=['rankf'])
    P.add('dve', lambda e: e.tensor_tensor(out=cmpb[:], in0=rankf[:], in1=Ff[:], op=ALU.mult),
          r=['rankf'] + [('Ff', tg) for tg in range(NTG)], w=['cmpb'])
    P.add('dve', lambda e: e.tensor_reduce(out=s0f[:], in_=cmpb[:], axis=AX.X, op=ALU.add), r=['cmpb'], w=['s0f'])
    P.add('dve', lambda e: e.tensor_tensor(out=cmpb[:], in0=rankf[:], in1=selb[:], op=ALU.mult), r=['rankf'] + selres, w=['cmpb'])
    P.add('dve', lambda e: e.tensor_reduce(out=s1f[:], in_=cmpb[:], axis=AX.X, op=ALU.add), r=['cmpb'], w=['s1f'])
    P.add('dve', lambda e: e.tensor_tensor(out=s1f[:], in0=s1f[:], in1=s0f[:], op=ALU.subtract), r=['s1f', 's0f'], w=['s1f'])
    P.add('dve', lambda e: e.tensor_copy(out=s0i[:], in_=s0f[:]), r=['s0f'], w=['s0i'])
    P.add('dve', lambda e: e.tensor_copy(out=s1i[:], in_=s1f[:]), r=['s1f'], w=['s1i'])
    P.add('dve', lambda e: e.memset(eidf[:], 0.0), w=['eidf'])
    for ex in range(NEXP):
        P.add('dve', lambda e, ex=ex: e.scalar_tensor_tensor(out=eidf[:], in0=jrow[:], scalar=endf[:, ex:ex + 1], in1=eidf[:],
                                                             op0=ALU.is_ge, op1=ALU.add), r=['jrow', endn, 'eidf'], w=['eidf'])
    P.add('dve', lambda e: e.tensor_scalar(out=eidf[:], in0=eidf[:], scalar1=31.0, scalar2=128.0, op0=ALU.min, op1=ALU.mult),
          r=['eidf'], w=['eidf'])
    P.add('dve', lambda e: e.tensor_scalar(out=eidf[:], in0=eidf[:], scalar1=pcol[:, 0:1], scalar2=None, op0=ALU.add),
          r=['eidf', 'pcol'], w=['eidf'])
    P.add('dve', lambda e: e.tensor_copy(out=widx[:], in_=eidf[:]), r=['eidf'], w=['widx'])

    mR = ar.mark()
    NH2 = 8
    h2l = [ar.alloc('h2l', [128, D], BF16) for i in range(NH2)]
    for tg in range(NTG):
        s3 = tg % NH2
        rows = slice(tg * 128, (tg + 1) * 128)
        P.add('sp', lambda e, s3=s3, rows=rows: e.dma_start(out=h2l[s3][:], in_=H2[rows, :]), r=[('H2', tg)], w=[('h2l', s3)], dma=True)
        for si, nm in ((s0i, 's0i'), (s1i, 's1i')):
            P.add('pool', lambda e, s3=s3, si=si, tg=tg: e.indirect_dma_start(
                out=Xs, out_offset=bass.IndirectOffsetOnAxis(si[:, tg:tg + 1].bitcast(U32), 0), in_=h2l[s3][:], in_offset=None),
                r=[('h2l', s3), nm], w=[('Xs', tg, nm)], dma=True)
    P.add('sp', lambda e: e.nop(), r=[('Xs', tg, nm) for tg in range(NTG) for nm in ('s0i', 's1i')], w=['XsAll'])
    P.barrier('pR')
    ar.release(mR)

    if debug == 'slots':
        dbg = dram('dbg', [128, 4, 32], F32, kind='ExternalOutput')
        P.add('sp', lambda e: e.dma_start(out=dbg[:, 0, :], in_=s0f[:]), r=['s0f'], w=['dbg0'], dma=True)
        P.add('sp', lambda e: e.dma_start(out=dbg[:, 1, :], in_=s1f[:]), r=['s1f'], w=['dbg1'], dma=True)
        P.add('sp', lambda e: e.dma_start(out=dbg[:, 2, :], in_=w0[:]), r=[('w0', tg) for tg in range(NTG)], w=['dbg2'], dma=True)
        P.add('sp', lambda e: e.dma_start(out=dbg[:, 3, :], in_=w1[:]), r=[('w1', tg) for tg in range(NTG)], w=['dbg3'], dma=True)

    NTE = NT if debug is None else int(os.environ.get('KNT', '8'))
    NWB = 4
    wg_s = [ar.alloc('wg_s', [128, NCH * DEXP], BF16) for i in range(NWB)]
    wu_s = [ar.alloc('wu_s', [128, NCH * DEXP], BF16) for i in range(NWB)]
    wd_s = [ar.alloc('wd_s', [128, 2 * D], BF16) for i in range(NWB)]
    xtok = [ar.alloc('xtok', [128, D], BF16) for i in range(4)]
    XT = [ar.alloc('XT', [128, NCH, 128], BF16) for i in range(2)]
    sgb = [ar.alloc('sgb', [128, DEXP], F32) for i in range(2)]
    atok = [ar.alloc('atok', [128, DEXP], BF16) for i in range(2)]
    aT = [ar.alloc('aT', [128, 2, 128], BF16) for i in range(2)]
    ysb = [ar.alloc('ysb', [128, D], F32) for i in range(2)]
    if debug not in ('slots',):
        P.add('pool', lambda e: e.nop(), r=[('wcast', it) for it in range(24)], w=['wcastAll'])
        stA, stB, stC = [], [], []
        for j in range(NTE):
            s = j % NWB
            for sub in range(2):
                u = 2 * j + sub
                s2 = u % 2
                rows = slice(j * 256 + sub * 128, j * 256 + sub * 128 + 128)
                pbank = 0 + s2
                gb_, ub_ = 2 + s2, 4 + s2

                def stageA(j=j, s=s, sub=sub, s2=s2, pbank=pbank, u=u):
                    if sub == 0:
                        for (dst, src, nm) in ((wg_s, wgb, 'wg'), (wu_s, wub, 'wu'), (wd_s, wdb, 'wd')):
                            P.add('pool', lambda e, dst=dst, src=src: e.indirect_dma_start(
                                out=dst[s][:], out_offset=None, in_=src,
                                in_offset=bass.IndirectOffsetOnAxis(widx[:, j:j + 1].bitcast(U32), 0)),
                                r=['widx', 'wcastAll'], w=[(nm, s)], dma=True)
                    x4 = u % 4
                    for uu_ in ([0, 1, 2] if u == 0 else [u + 2]):
                        if uu_ < 2 * NTE:
                            P.add('sp', lambda e, uu_=uu_: e.dma_start(out=xtok[uu_ % 4][:], in_=Xs[uu_ * 128:(uu_ + 1) * 128, :]),
                                  r=['XsAll'], w=[('xtok', uu_ % 4)], dma=True)
                    pb = psum[pbank][:].bitcast(BF16)
                    for c in range(NCH):
                        P.add('pe', lambda e, c=c: e.transpose(out=pb[:, c * 128:(c + 1) * 128],
                                                               in_=xtok[x4][:, c * 128:(c + 1) * 128], identity=ident),
                              r=[('xtok', x4), 'cbf'], w=[('ps', pbank)])
                    P.add('act', lambda e: e.activation(out=XT[s2][:].rearrange('p c t -> p (c t)'), in_=pb, func=AF.Copy),
                          r=[('ps', pbank)], w=[('XT', s2)])

                def stageB(s=s, s2=s2, gb_=gb_, ub_=ub_):
                    for c in range(NCH):
                        P.add('pe', lambda e, c=c: e.matmul(
                            psum[gb_][:, 0:DEXP], lhsT=XT[s2][:, c, :], rhs=wg_s[s][:, c * DEXP:(c + 1) * DEXP],
                            start=(c == 0), stop=(c == NCH - 1)),
                            r=[('XT', s2), ('wg', s)], w=[('ps', gb_)])
                    for c in range(NCH):
                        P.add('pe', lambda e, c=c: e.matmul(
                            psum[ub_][:, 0:DEXP], lhsT=XT[s2][:, c, :], rhs=wu_s[s][:, c * DEXP:(c + 1) * DEXP],
                            start=(c == 0), stop=(c == NCH - 1)),
                            r=[('XT', s2), ('wu', s)], w=[('ps', ub_)])
                    P.add('act', lambda e: e.activation(out=sgb[s2][:], in_=psum[gb_][:, 0:DEXP], func=AF.Silu),
                          r=[('ps', gb_)], w=[('sgb', s2)])
                    P.add('dve', lambda e: e.tensor_tensor(out=atok[s2][:], in0=psum[ub_][:, 0:DEXP], in1=sgb[s2][:], op=ALU.mult),
                          r=[('ps', ub_), ('sgb', s2)], w=[('atok', s2)])
                    pa = psum[gb_][:].bitcast(BF16)
                    for fc in range(2):
                        P.add('pe', lambda e, fc=fc: e.transpose(out=pa[:, fc * 128:(fc + 1) * 128],
                                                                 in_=atok[s2][:, fc * 128:(fc + 1) * 128], identity=ident),
                              r=[('atok', s2), 'cbf'], w=[('ps', gb_)])
                    P.add('dve', lambda e: e.tensor_copy(out=aT[s2][:].rearrange('p c t -> p (c t)'), in_=pa[:, 0:256]),
                          r=[('ps', gb_)], w=[('aT', s2)])

                def stageC(s=s, s2=s2, rows=rows):
                    for half in range(2):
                        yb_ = 6 + half
                        for fc in range(2):
                            P.add('pe', lambda e, fc=fc, half=half, yb_=yb_: e.matmul(
                                psum[yb_][:], lhsT=aT[s2][:, fc, :], rhs=wd_s[s][:, fc * D + half * 512:fc * D + (half + 1) * 512],
                                start=(fc == 0), stop=(fc == 1)),
                                r=[('aT', s2), ('wd', s)], w=[('ps', yb_)])
                    P.add('act', lambda e: e.activation(out=ysb[s2][:, 0:512], in_=psum[6][:], func=AF.Copy),
                          r=[('ps', 6)], w=[('ysb', s2, 0)])
                    P.add('dve', lambda e: e.tensor_copy(out=ysb[s2][:, 512:1024], in_=psum[7][:]),
                          r=[('ps', 7)], w=[('ysb', s2, 1)])
                    P.add('act', lambda e: e.dma_start(out=Ys[rows, :], in_=ysb[s2][:]),
                          r=[('ysb', s2, 0), ('ysb', s2, 1)], w=[('Ys', rows.start)], dma=True)

                stA.append(stageA)
                stB.append(stageB)
                stC.append(stageC)
        NU = len(stA)
        for t in range(NU + 2):
            if t < NU:
                stA[t]()
            if 0 <= t - 1 < NU:
                stB[t - 1]()
            if 0 <= t - 2 < NU:
                stC[t - 2]()
        P.add('pool', lambda e: e.nop(), r=[('Ys', 128 * u) for u in range(2 * NTE)], w=['YsAll'])
        P.barrier('pE')
    ar.release(mR)

    gfr = ar.alloc('gfr', [128, D], F32)
    NFB = 4
    x1l = [ar.alloc('x1l', [128, D], F32) for i in range(NFB)]
    y0l = [ar.alloc('y0l', [128, D], F32) for i in range(NFB)]
    y1l = [ar.alloc('y1l', [128, D], F32) for i in range(NFB)]
    ob = [ar.alloc('ob', [128, D], F32) for i in range(NFB)]
    junk = ar.alloc('junk', [128, D], BF16)
    fs = ar.alloc('fs', [128, 32, 4], F32)
    P.add('sp', lambda e: e.dma_start(out=gfr[:], in_=gfr_d), w=['gfr'], dma=True)
    if debug is None or debug == 'final':
        for tg in range(NTG):
            s = tg % NFB
            rows = slice(tg * 128, (tg + 1) * 128)
            P.add('sp', lambda e, s=s, rows=rows: e.dma_start(out=x1l[s][:], in_=X1[rows, :]), r=[('X1', tg)], w=[('x1l', s)], dma=True)
            P.add('pool', lambda e, s=s, tg=tg: e.indirect_dma_start(
                out=y0l[s][:], out_offset=None, in_=Ys, in_offset=bass.IndirectOffsetOnAxis(s0i[:, tg:tg + 1].bitcast(U32), 0)),
                r=['YsAll', 's0i'], w=[('y0l', s)], dma=True)
            P.add('pool', lambda e, s=s, tg=tg: e.indirect_dma_start(
                out=y1l[s][:], out_offset=None, in_=Ys, in_offset=bass.IndirectOffsetOnAxis(s1i[:, tg:tg + 1].bitcast(U32), 0)),
                r=['YsAll', 's1i'], w=[('y1l', s)], dma=True)
            P.add('dve', lambda e, s=s, tg=tg: e.scalar_tensor_tensor(out=x1l[s][:], in0=y0l[s][:], scalar=w0[:, tg:tg + 1], in1=x1l[s][:],
                                                                      op0=ALU.mult, op1=ALU.add),
                  r=[('y0l', s), ('x1l', s), ('w0', tg)], w=[('x1l', s)])
            P.add('dve', lambda e, s=s, tg=tg: e.scalar_tensor_tensor(out=x1l[s][:], in0=y1l[s][:], scalar=w1[:, tg:tg + 1], in1=x1l[s][:],
                                                                      op0=ALU.mult, op1=ALU.add),
                  r=[('y1l', s), ('x1l', s), ('w1', tg)], w=[('x1l', s)])
            P.add('act', lambda e, s=s, tg=tg: e.activation(out=junk[:], in_=x1l[s][:], func=AF.Square, accum_out=fs[:, tg, 0:1]),
                  r=[('x1l', s)], w=['junk', ('fs', tg, 0)])
            P.add('act', lambda e, tg=tg: e.activation(out=fs[:, tg, 1:2], in_=fs[:, tg, 0:1], func=AF.Sqrt, scale=1.0 / D, bias=EPS),
                  r=[('fs', tg, 0)], w=[('fs', tg, 1)])
            P.add('dve', lambda e, tg=tg: e.reciprocal(out=fs[:, tg, 2:3], in_=fs[:, tg, 1:2]), r=[('fs', tg, 1)], w=[('fs', tg, 2)])
            P.add('dve', lambda e, s=s, tg=tg: e.scalar_tensor_tensor(out=ob[s][:], in0=x1l[s][:], scalar=fs[:, tg, 2:3], in1=gfr[:],
                                                                      op0=ALU.mult, op1=ALU.mult),
                  r=[('x1l', s), ('fs', tg, 2), 'gfr'], w=[('ob', s)])
            P.add('act', lambda e, s=s, rows=rows: e.dma_start(out=outd[rows, :], in_=ob[s][:]), r=[('ob', s)], w=[('out', tg)], dma=True)


    if debug == 'yaT_disabled':
        dbg = dram('dbg', [128, 4, T], BF16, kind='ExternalOutput')
        P.add('sp', lambda e: e.dma_start(out=dbg, in_=yaT[:]), r=[('yaT', p, w) for p in range(4) for w in range(NW)],
              w=['dbg'], dma=True)
    if debug == 'hT_disabled':
        dbg = dram('dbg', [128, NCH, T], BF16, kind='ExternalOutput')
        P.add('sp', lambda e: e.dma_start(out=dbg, in_=hT[:]), r=[('hT', w) for w in range(NW)],
              w=['dbg'], dma=True)

    P.emit(stack)
    stack.close()
    return nc


def rope_tables():
    pos = np.arange(T, dtype=np.float32)
    inv_freq = (np.float32(10000.0) ** (-np.arange(0, HD, 2, dtype=np.float32) / np.float32(HD))).astype(np.float32)
    ang = (pos[:, None] * inv_freq[None, :]).astype(np.float32)
    cos = np.cos(ang).astype(np.float32).T
    sin = np.sin(ang).astype(np.float32).T
    return np.ascontiguousarray(np.tile(cos, (4, 1))), np.ascontiguousarray(np.tile(sin, (4, 1)))


def const_bf16():
    rotT = np.zeros((128, 128), np.float32)
    for m in range(128):
        if m % 64 < 32:
            rotT[m + 32, m] = -1.0
        else:
            rotT[m - 32, m] = 1.0
    kk = np.arange(128)[:, None]
    q = np.arange(128)[None, :]
    cur = (kk <= q).astype(np.float32)
    prevA = (kk >= q).astype(np.float32)
    prevB = (kk >= q + 1).astype(np.float32)
    mA = np.concatenate([prevA, cur, prevA, cur], 1)
    m16 = []
    for v in range(4):
        sl = slice(32 * v, 32 * v + 32)
        m16.append(np.concatenate([prevA[:, sl], cur[:, sl]] * 8, 1))
    mB = np.concatenate([prevB, cur, prevB, cur], 1)
    triS = (np.arange(128)[:, None] < np.arange(128)[None, :]).astype(np.float32)
    allc = np.concatenate([rotT, np.eye(128, dtype=np.float32), mB, mA] + m16 + [triS], 1)
    return allc.astype(ml_dtypes.bfloat16)


def prep_inputs(inp):
    x = np.asarray(inp['x'], dtype=np.float32)
    B = x.shape[0]
    w_in = np.asarray(inp['w_in'], np.float32)[0]
    b_in = np.asarray(inp['b_in'], np.float32)[0]
    gmix = np.ascontiguousarray(np.asarray(inp['g_mix'], np.float32)[0].reshape(NCH, 128).T)
    win = np.empty((NBLK, 128, NCH, 128), np.float32)
    binb = np.empty((128, NBLK), np.float32)
    for i, cols in enumerate(BLOCKS):
        win[i] = w_in[:, cols].reshape(NCH, 128, 128).transpose(1, 0, 2)
        binb[:, i] = b_in[cols]
    bvrep = np.empty((len(VBLKS), 128, 128), np.float32)
    for i, b in enumerate(VBLKS):
        bvrep[i] = np.tile(b_in[BLOCKS[b]][None, :], (128, 1))
    cosT, sinT = rope_tables()
    f32 = lambda k: np.asarray(inp[k], np.float32)
    wpa_ = f32('w_proj_a')[0]
    wpb_ = f32('w_proj_b')[0]
    wo_ = f32('w_out')[0]
    rows_b = np.concatenate([np.concatenate([c * 64 + np.arange(64), (8 + c) * 64 + np.arange(64)]) for c in range(8)])
    blk = lambda m, nc_: np.ascontiguousarray(m.reshape(nc_, 128, 8, 128).transpose(2, 1, 0, 3))
    wpa = blk(wpa_, 4)
    wpb = blk(wpb_[rows_b], 8)
    wo = blk(wo_, 8)
    pc = lambda v: np.ascontiguousarray(v.reshape(NCH, 128).T)
    gffn = pc(f32('g_ffn')[0])
    gfin = pc(f32('g_final'))
    sinkrep = np.ascontiguousarray(np.tile(f32('sinks')[0][None, :], (128, 1)))
    wr_ = np.concatenate([f32('w_router_group')[0], f32('w_router_expert')[0]], 1)
    wr = np.ascontiguousarray(wr_.reshape(NCH, 128, 36).transpose(1, 0, 2))
    brep = np.ascontiguousarray(np.tile(np.concatenate([f32('b_router_group')[0], f32('b_router_expert')[0]])[None, :], (128, 1)))
    weg = np.ascontiguousarray(f32('w_exp_gate')[0].reshape(NEXP, NCH, 128, DEXP).transpose(0, 2, 1, 3))
    weu = np.ascontiguousarray(f32('w_exp_up')[0].reshape(NEXP, NCH, 128, DEXP).transpose(0, 2, 1, 3))
    wed = np.ascontiguousarray(f32('w_exp_down')[0].reshape(NEXP, 2, 128, D).transpose(0, 2, 1, 3))
    shared = dict(gmix=gmix, win=win, binb=binb, bvrep=bvrep, cosT=cosT, sinT=sinT, cbf=const_bf16(),
                  wpa=wpa, wpb=wpb, wo=wo, gffn=gffn, gfin=gfin, sinkrep=sinkrep, wr=wr, brep=brep,
                  weg=weg, weu=weu, wed=wed,
                  identf=np.eye(128, dtype=np.float32),
                  thr=np.ascontiguousarray(np.tile((256.0 * np.arange(32, dtype=np.float32))[None, None, :], (128, 32, 1))),
                  jrow=np.ascontiguousarray(np.tile(np.arange(64, dtype=np.float32)[None, :], (128, 1))),
                  pcol=np.arange(128, dtype=np.float32).reshape(128, 1),
                  gfr=np.ascontiguousarray(np.tile(f32('g_final')[None, :], (128, 1))))
    shared.pop('gfin', None)
    per_core = []
    for b in range(B):
        m = dict(shared)
        m['xT'] = np.ascontiguousarray(x[b].T)
        per_core.append(m)
    return per_core


def kernel(**inputs):
    debug = os.environ.get('KDEBUG')
    nc = build(debug)
    in_maps = prep_inputs(inputs)
    res = run_bass_kernel_spmd(nc, in_maps, core_ids=list(range(8)))
    if debug:
        return [r['dbg'] for r in res.results]
    return np.ascontiguousarray(np.stack([np.asarray(r['out']) for r in res.results], 0)).astype(np.float32)
```
